# Optimizing a Trainium2 kernel written in Bass

```python
import jax, jax.numpy as jnp
from jax import lax
import numpy as np

D_MODEL = 1024
BATCH = 16
SEQ = 2048
DEPTH = 2

PLE_DIM = 256
ATT_HEADS = 8
ATT_KV_HEADS = 2
HEAD_DIM = 64
Q_RANK = 256
IDX_HEADS = 8
IDX_DIM = 64
TOPK_MAX = 256
Q_BLOCK = 128
ML_HEADS = 4
ML_DIM = 128
CONV_W = 4
CHUNK = 64
D_FF = 4 * D_MODEL
ROPE_THETA = 500000.0
ROT_DIM = HEAD_DIM // 4
EPS = 1e-6

ATT_W = ATT_HEADS * HEAD_DIM
KV_W = ATT_KV_HEADS * HEAD_DIM
ML_W = ML_HEADS * ML_DIM
MIX_W = ATT_W + ML_W
IN_SIZES = (Q_RANK, KV_W, KV_W, IDX_DIM, IDX_HEADS, ML_W, ML_W, ML_W, ML_W, ML_HEADS, ML_HEADS)
IN_W = sum(IN_SIZES)
IDX_SCALE = (IDX_HEADS ** -0.5) * (IDX_DIM ** -0.5)

kernel_name = "hybrid_dsa_mlstm_parallel_heads"


def rms_norm(x, g):
    xf = x.astype(jnp.float32)
    y = xf * lax.rsqrt(jnp.mean(xf * xf, axis=-1, keepdims=True) + EPS)
    return (y * g.astype(jnp.float32)).astype(x.dtype)


def rope_tables(positions):
    half = ROT_DIM // 2
    inv = ROPE_THETA ** (-(jnp.arange(half, dtype=jnp.float32) * 2.0) / ROT_DIM)
    ang = positions.astype(jnp.float32)[..., None] * inv
    return jnp.cos(ang), jnp.sin(ang)


def apply_partial_rope(x, cos, sin):
    half = ROT_DIM // 2
    c = cos[:, :, None, :].astype(x.dtype)
    s = sin[:, :, None, :].astype(x.dtype)
    x1 = x[..., :half]
    x2 = x[..., half:ROT_DIM]
    return jnp.concatenate([x1 * c - x2 * s, x2 * c + x1 * s, x[..., ROT_DIM:]], axis=-1)


def causal_dwconv(x, w, b):
    S = x.shape[1]
    xp = jnp.pad(x, ((0, 0), (CONV_W - 1, 0), (0, 0)))
    acc = xp[:, 0:S] * w[0]
    for j in range(1, CONV_W):
        acc = acc + xp[:, j:j + S] * w[j]
    return acc + b


def dsa_attention(q, k, v, q_idx, k_idx, w_idx):
    B, S, H, D = q.shape
    G = H // ATT_KV_HEADS
    n_sel = min(TOPK_MAX, S // 4)
    nb = S // Q_BLOCK
    key_pos = jnp.arange(S)
    scale = D ** -0.5

    def blk(a):
        return a.reshape(B, nb, Q_BLOCK, *a.shape[2:]).swapaxes(0, 1)

    def one_block(args):
        qb, qib, wb, qpos = args
        logits = jnp.einsum('bqhd,bsd->bqhs', qib, k_idx)
        score = jnp.einsum('bqh,bqhs->bqs', wb, jax.nn.relu(logits)).astype(jnp.float32)
        causal = key_pos[None, :] <= qpos[:, None]
        score = jnp.where(causal[None], score, -jnp.inf)
        _, sel = lax.top_k(score, n_sel)
        k_sel = jax.vmap(lambda kb, ib: kb[ib])(k, sel)
        v_sel = jax.vmap(lambda vb, ib: vb[ib])(v, sel)
        valid = sel <= qpos[None, :, None]
        qg = qb.reshape(B, Q_BLOCK, ATT_KV_HEADS, G, D)
        s = jnp.einsum('bqgrd,bqkgd->bqgrk', qg, k_sel).astype(jnp.float32) * scale
        s = jnp.where(valid[:, :, None, None, :], s, -jnp.inf)
        pr = jax.nn.softmax(s, axis=-1).astype(v.dtype)
        o = jnp.einsum('bqgrk,bqkgd->bqgrd', pr, v_sel)
        return o.reshape(B, Q_BLOCK, H, D)

    out = lax.map(one_block, (blk(q), blk(q_idx), blk(w_idx), key_pos.reshape(nb, Q_BLOCK)))
    return out.swapaxes(0, 1).reshape(B, S, H, D)


def mlstm_chunkwise(q, k, v, i_pre, f_pre):
    B, S, H, D = q.shape
    nc = S // CHUNK
    f32 = jnp.float32
    qf = q.astype(f32) * (D ** -0.5)
    kf = k.astype(f32)
    vf = v.astype(f32)
    log_f = jax.nn.log_sigmoid(f_pre.astype(f32))
    log_i = i_pre.astype(f32)

    def chunks(a):
        a = a.reshape(B, nc, CHUNK, H, *a.shape[3:])
        return jnp.moveaxis(a, (1, 3), (0, 2))

    tri = jnp.tril(jnp.ones((CHUNK, CHUNK), dtype=bool))

    def step(carry, xs):
        C, n, m = carry
        qc, kc, vc, lic, lfc = xs
        b = jnp.cumsum(lfc, axis=-1)
        log_d = b[..., :, None] - b[..., None, :] + lic[..., None, :]
        log_d = jnp.where(tri, log_d, -jnp.inf)
        inter = b + m[..., None]
        m_t = jnp.maximum(inter, jnp.max(log_d, axis=-1))
        dmat = jnp.exp(log_d - m_t[..., None])
        inter_w = jnp.exp(inter - m_t)
        qk = jnp.einsum('bhtd,bhsd->bhts', qc, kc) * dmat
        num = inter_w[..., None] * jnp.einsum('bhtd,bhde->bhte', qc, C) + jnp.einsum('bhts,bhse->bhte', qk, vc)
        den = inter_w * jnp.einsum('bhtd,bhd->bht', qc, n) + jnp.sum(qk, axis=-1)
        h = num / jnp.maximum(jnp.abs(den), jnp.exp(-m_t))[..., None]
        b_last = b[..., -1]
        log_g = b_last[..., None] - b + lic
        m_new = jnp.maximum(b_last + m, jnp.max(log_g, axis=-1))
        g = jnp.exp(log_g - m_new[..., None])
        decay = jnp.exp(b_last + m - m_new)
        C_new = decay[..., None, None] * C + jnp.einsum('bhs,bhsd,bhse->bhde', g, kc, vc)
        n_new = decay[..., None] * n + jnp.einsum('bhs,bhsd->bhd', g, kc)
        return (C_new, n_new, m_new), h

    init = (jnp.zeros((B, H, D, D), f32), jnp.zeros((B, H, D), f32), jnp.zeros((B, H), f32))
    _, hs = lax.scan(step, init, (chunks(qf), chunks(kf), chunks(vf), chunks(log_i), chunks(log_f)))
    hs = jnp.moveaxis(hs, (0, 2), (1, 3)).reshape(B, S, H, D)
    return hs.astype(q.dtype)


def hybrid_layer(h, p_i, cos, sin, g_mix, w_in, g_cq, w_q_up, w_iq_up, g_qn, g_kn, g_ik,
                 conv_w, conv_b, i_bias, f_bias, g_mh, w_out, g_mlp, w_ff1, w_ff2,
                 g_ple, w_ple_gate, b_ple_gate, w_ple):
    B, S, _ = h.shape
    xn = rms_norm(h, g_mix)
    proj = xn @ w_in
    split_at = np.cumsum(IN_SIZES)[:-1].tolist()
    c_q, a_k, a_v, i_k, i_w, m_q, m_k, m_v, m_o, m_i, m_f = jnp.split(proj, split_at, axis=-1)

    c_q = rms_norm(c_q, g_cq)
    a_q = (c_q @ w_q_up).reshape(B, S, ATT_HEADS, HEAD_DIM)
    i_q = (c_q @ w_iq_up).reshape(B, S, IDX_HEADS, IDX_DIM)
    a_q = apply_partial_rope(rms_norm(a_q, g_qn), cos, sin)
    a_k = apply_partial_rope(rms_norm(a_k.reshape(B, S, ATT_KV_HEADS, HEAD_DIM), g_kn), cos, sin)
    a_v = a_v.reshape(B, S, ATT_KV_HEADS, HEAD_DIM)
    i_q = apply_partial_rope(i_q, cos, sin)
    i_k = apply_partial_rope(rms_norm(i_k, g_ik)[:, :, None, :], cos, sin)[:, :, 0, :]
    i_w = i_w * IDX_SCALE
    att = dsa_attention(a_q, a_k, a_v, i_q, i_k, i_w).reshape(B, S, ATT_W)

    qk = jax.nn.silu(causal_dwconv(jnp.concatenate([m_q, m_k], axis=-1), conv_w, conv_b))
    m_q, m_k = qk[..., :ML_W], qk[..., ML_W:]
    hm = mlstm_chunkwise(m_q.reshape(B, S, ML_HEADS, ML_DIM), m_k.reshape(B, S, ML_HEADS, ML_DIM),
                         m_v.reshape(B, S, ML_HEADS, ML_DIM), m_i + i_bias, m_f + f_bias)
    hm = (jax.nn.sigmoid(m_o) * rms_norm(hm, g_mh).reshape(B, S, ML_W))

    h = h + jnp.concatenate([att, hm], axis=-1) @ w_out

    u = rms_norm(h, g_mlp) @ w_ff1
    h = h + jnp.square(jax.nn.relu(u)) @ w_ff2

    gate = jax.nn.sigmoid(rms_norm(h, g_ple) @ w_ple_gate + b_ple_gate)
    return h + gate * (p_i @ w_ple)


def setup_inputs(seed: int = 0) -> dict:
    key = jax.random.key(seed)
    ks = iter(jax.random.split(key, 32))
    f32 = jnp.float32

    def nrm(shape, scale):
        return jax.random.normal(next(ks), shape, f32) * scale

    def gain(shape):
        return 1.0 + nrm(shape, 0.02)

    L = DEPTH
    x = nrm((BATCH, SEQ, D_MODEL), 1.0)
    p = nrm((L, BATCH, SEQ, PLE_DIM), 1.0)
    offs = jax.random.randint(next(ks), (BATCH, 1), 0, 4096, dtype=jnp.int32)
    positions = jnp.arange(SEQ, dtype=jnp.int32)[None, :] + offs
    return {
        "x": x,
        "p": p,
        "positions": positions,
        "g_mix": gain((L, D_MODEL)),
        "w_in": nrm((L, D_MODEL, IN_W), D_MODEL ** -0.5),
        "g_cq": gain((L, Q_RANK)),
        "w_q_up": nrm((L, Q_RANK, ATT_W), Q_RANK ** -0.5),
        "w_iq_up": nrm((L, Q_RANK, IDX_HEADS * IDX_DIM), Q_RANK ** -0.5),
        "g_qn": gain((L, HEAD_DIM)),
        "g_kn": gain((L, HEAD_DIM)),
        "g_ik": gain((L, IDX_DIM)),
        "conv_w": nrm((L, CONV_W, 2 * ML_W), CONV_W ** -0.5),
        "conv_b": nrm((L, 2 * ML_W), 0.01),
        "i_bias": nrm((L, ML_HEADS), 0.1),
        "f_bias": jnp.linspace(3.0, 6.0, ML_HEADS, dtype=f32)[None, :] + nrm((L, ML_HEADS), 0.1),
        "g_mh": gain((L, ML_HEADS, ML_DIM)),
        "w_out": nrm((L, MIX_W, D_MODEL), MIX_W ** -0.5),
        "g_mlp": gain((L, D_MODEL)),
        "w_ff1": nrm((L, D_MODEL, D_FF), D_MODEL ** -0.5),
        "w_ff2": nrm((L, D_FF, D_MODEL), D_FF ** -0.5),
        "g_ple": gain((L, D_MODEL)),
        "w_ple_gate": nrm((L, D_MODEL, D_MODEL), D_MODEL ** -0.5),
        "b_ple_gate": nrm((L, D_MODEL), 0.01),
        "w_ple": nrm((L, PLE_DIM, D_MODEL), PLE_DIM ** -0.5),
    }


def reference(x, p, positions, g_mix, w_in, g_cq, w_q_up, w_iq_up, g_qn, g_kn, g_ik,
              conv_w, conv_b, i_bias, f_bias, g_mh, w_out, g_mlp, w_ff1, w_ff2,
              g_ple, w_ple_gate, b_ple_gate, w_ple):
    cos, sin = rope_tables(positions)
    h = x
    for i in range(DEPTH):
        h = hybrid_layer(h, p[i], cos, sin, g_mix[i], w_in[i], g_cq[i], w_q_up[i], w_iq_up[i],
                         g_qn[i], g_kn[i], g_ik[i], conv_w[i], conv_b[i], i_bias[i], f_bias[i],
                         g_mh[i], w_out[i], g_mlp[i], w_ff1[i], w_ff2[i], g_ple[i],
                         w_ple_gate[i], b_ple_gate[i], w_ple[i])
    return h
```

```python
import contextlib
import os
import numpy as np
import concourse.bass as bass
import concourse.mybir as mybir
from concourse.bass_utils import run_bass_kernel_spmd

F32 = mybir.dt.float32
BF16 = mybir.dt.bfloat16
I32 = mybir.dt.int32
ALU = mybir.AluOpType
AF = mybir.ActivationFunctionType
AX = mybir.AxisListType

D = 1024
S = 2048
NL = 2
NCORES = 8
TB = 512
NTB = S // TB
EPS = 1e-6
IN_W = 2640
DFF = 4096

ENGS = ("pe", "act", "dve", "pool", "sp")
N_DMA_SEMS = 16


def _region(ap):
    if type(ap.tensor).__name__.startswith("PSum"):
        return (ap.tensor.name, 0, 128, 0, 1 << 30)
    pat = ap.ap
    row = pat[0][0]
    npart = pat[0][1]
    off = ap.offset
    if row <= 0:
        p0, f0 = 0, off
    else:
        p0 = off // row
        f0 = off - p0 * row
    ext = 1
    for st, cnt in pat[1:]:
        ext += (cnt - 1) * abs(st)
    sz = mybir.dt.size(ap.dtype)
    return (ap.tensor.name, p0, p0 + npart, f0 * sz, (f0 + ext) * sz)


def _overlap(a, b):
    return a[1] < b[2] and b[1] < a[2] and a[3] < b[4] and b[3] < a[4]


class Op:
    __slots__ = ("eng", "fn", "reads", "writes", "dma", "deps", "sig", "idx", "dmak")


class Prog:
    def __init__(self, nc):
        self.nc = nc
        self.ops = []
        self.hist = {}
        self.last_op = {}
        self.bar_id = 0
        self.bar_deps = set()
        self.eng_bar = {}
        self.dma_pending = []
        self.pending = {}

    def barrier(self):
        deps = set(self.last_op.values()) | set(self.dma_pending)
        self.dma_pending = []
        for e in ENGS:
            self.pending.setdefault(e, set()).update(deps)

    def add(self, eng, fn, reads=(), writes=(), dma=False):
        op = Op()
        op.eng, op.fn, op.dma = eng, fn, dma
        op.reads = [_region(a) for a in reads if a is not None]
        op.writes = [_region(a) for a in writes if a is not None]
        op.deps = set()
        op.sig = None
        op.dmak = None
        op.idx = len(self.ops)
        self.ops.append(op)
        ops = self.ops
        for r in op.reads:
            for (reg, oi, isw) in self.hist.setdefault(r[0], []):
                if isw and _overlap(reg, r):
                    op.deps.add(oi)
        for w in op.writes:
            keep = []
            for rec in self.hist.setdefault(w[0], []):
                reg, oi, isw = rec
                if _overlap(reg, w):
                    op.deps.add(oi)
                    if w[1] <= reg[1] and reg[2] <= w[2] and w[3] <= reg[3] and reg[4] <= w[4]:
                        continue
                keep.append(rec)
            self.hist[w[0]] = keep
        for r in op.reads:
            h = self.hist[r[0]]
            if not dma:
                h[:] = [rec for rec in h if not ((not rec[2]) and rec[0] == r
                                                 and ops[rec[1]].eng == eng and not ops[rec[1]].dma)]
            h.append((r, op.idx, False))
        for w in op.writes:
            self.hist[w[0]].append((w, op.idx, True))
        if self.pending.get(eng):
            op.deps |= self.pending[eng]
            self.pending[eng] = set()
        if not dma:
            self.last_op[eng] = op.idx
        else:
            self.dma_pending.append(op.idx)
        op.deps.discard(op.idx)
        return op

    def emit(self, final_wait_ops=()):
        nc = self.nc
        ops = self.ops
        for op in ops:
            if op.eng == "pe" and not op.dma:
                op.deps = {d for d in op.deps if not (ops[d].eng == "pe" and not ops[d].dma)}
        needed = set()
        for op in ops:
            needed |= op.deps
        for o in final_wait_ops:
            needed.add(o.idx)
        cnt = {e: 0 for e in ENGS}
        dmak = {"sp": 0, "pool": 0, "act": 0}
        dma_ops = {"sp": [], "pool": [], "act": []}
        for op in ops:
            if op.dma:
                k = dmak[op.eng]
                op.dmak = k
                op.sig = ("dma", (op.eng, k % N_DMA_SEMS), 16 * (k // N_DMA_SEMS + 1))
                dmak[op.eng] += 1
                dma_ops[op.eng].append(op)
            elif op.idx in needed:
                cnt[op.eng] += 1
                op.sig = (op.eng, cnt[op.eng])
        with contextlib.ExitStack() as st:
            sems = {e: st.enter_context(nc.semaphore("s_" + e)) for e in ENGS}
            dsems = {(q, i): st.enter_context(nc.semaphore("d_%s_%d" % (q, i)))
                     for q in ("sp", "pool") for i in range(N_DMA_SEMS)}
            block = st.enter_context(nc.Block())
            per_eng = {e: [op for op in ops if op.eng == e] for e in ENGS}

            def run(engname, eng):
                known = {}
                for op in per_eng[engname]:
                    waits = {}
                    deps = set(op.deps)
                    if op.dma and op.dmak >= N_DMA_SEMS:
                        deps.add(dma_ops[op.eng][op.dmak - N_DMA_SEMS].idx)
                    for d in deps:
                        s = ops[d].sig
                        if s[0] == "dma":
                            key, v = ("dma", s[1]), s[2]
                        else:
                            key, v = ("c", s[0]), s[1]
                        if known.get(key, 0) >= v:
                            continue
                        if waits.get(key, 0) < v:
                            waits[key] = v
                    for key, v in waits.items():
                        sem = dsems[key[1]] if key[0] == "dma" else sems[key[1]]
                        eng.wait_ge(sem, v)
                        known[key] = v
                    ins = op.fn(eng)
                    if op.sig is not None:
                        if op.sig[0] == "dma":
                            ins.then_inc(dsems[op.sig[1]], 16)
                        else:
                            ins.then_inc(sems[op.sig[0]], 1)
                if engname == "sp":
                    for o in final_wait_ops:
                        s = o.sig
                        if s[0] == "dma":
                            eng.wait_ge(dsems[s[1]], s[2])
                        else:
                            eng.wait_ge(sems[s[0]], s[1])

            @block.tensor
            def _(e):
                run("pe", e)

            @block.scalar
            def _(e):
                run("act", e)

            @block.vector
            def _(e):
                run("dve", e)

            @block.gpsimd
            def _(e):
                run("pool", e)

            @block.sync
            def _(e):
                run("sp", e)


IDX_SCALE = (8 ** -0.5) * (64 ** -0.5)
KSTOP = int(os.environ.get('K_STOP', '99'))
RUN_STAGES = ("mix", "attn", "mlstm", "ffn", "ple")
KSUB = int(os.environ.get('K_SUB', '99'))
KSUB2 = int(os.environ.get('K_SUB2', '99'))
NEG_BIG = -1.0e30
N_BISECT = 24
TWO_PI = 2.0 * np.pi


def _fm_cols(v, p=128):
    v = np.asarray(v, np.float32)
    return np.ascontiguousarray(v.reshape(-1, p).T)


class CstLayout:
    def __init__(self):
        self.off = {}
        self.n = 0

    def add(self, name, ncols):
        self.off[name] = self.n
        self.n += ncols


def make_cst_layout():
    L = CstLayout()
    for l in range(NL):
        for nm in ("g_mix", "g_mlp", "g_ple", "b_ple", "conv_b"):
            L.add((nm, l), 8)
        L.add(("g_cq", l), 2)
        for nm in ("g_qn", "g_kn", "g_ik", "i_bias", "f_bias"):
            L.add((nm, l), 1)
        L.add(("conv_w", l), 32)
        L.add(("g_mh", l), 4)
        L.add(("g_qn_row", l), 64)
        L.add(("g_kn_row", l), 64)
    return L


CL = make_cst_layout()


def build_cst(inp):
    c = np.zeros((128, CL.n), np.float32)
    for l in range(NL):
        for nm, key in (("g_mix", "g_mix"), ("g_mlp", "g_mlp"), ("g_ple", "g_ple"),
                        ("b_ple", "b_ple_gate"), ("conv_b", "conv_b")):
            o = CL.off[(nm, l)]
            c[:, o:o + 8] = _fm_cols(inp[key][l])
        o = CL.off[("g_cq", l)]
        c[:, o:o + 2] = _fm_cols(inp["g_cq"][l])
        for nm in ("g_qn", "g_kn", "g_ik"):
            o = CL.off[(nm, l)]
            c[0:64, o] = inp[nm][l]
        for nm in ("i_bias", "f_bias"):
            o = CL.off[(nm, l)]
            c[0:4, o] = inp[nm][l]
        o = CL.off[("conv_w", l)]
        for j in range(4):
            c[:, o + j * 8:o + j * 8 + 8] = _fm_cols(inp["conv_w"][l, j])
        o = CL.off[("g_mh", l)]
        c[:, o:o + 4] = np.ascontiguousarray(inp["g_mh"][l].T)
        o = CL.off[("g_qn_row", l)]
        c[:, o:o + 64] = np.broadcast_to(inp["g_qn"][l][None, :], (128, 64))
        o = CL.off[("g_kn_row", l)]
        c[:, o:o + 64] = np.broadcast_to(inp["g_kn"][l][None, :], (128, 64))
    return c


CM = {}
_o = 0
for _nm, _w in (("MEAN1024", 128), ("MEAN256", 128), ("MEAN128", 128), ("MEAN64", 64), ("ONES", 128),
                ("IDENT", 128), ("RT", 64), ("TRIU", 64), ("I4", 4), ("SEL", 512)):
    CM[_nm] = _o
    _o += _w
CM_N = _o

CF = {}
_o = 0
for _nm, _w in (("I4", 4), ("SEL", 512), ("RESET", 512), ("INV", 1), ("CAUS", 128), ("POW2", N_BISECT), ("TRIU", 64)):
    CF[_nm] = _o
    _o += _w
CF_N = _o


def build_cm():
    import ml_dtypes
    m = np.zeros((128, CM_N), np.float32)
    m[:, CM["MEAN1024"]:CM["MEAN1024"] + 128] = 1.0 / 1024.0
    m[:, CM["MEAN256"]:CM["MEAN256"] + 128] = 1.0 / 256.0
    m[:, CM["MEAN128"]:CM["MEAN128"] + 128] = 1.0 / 128.0
    m[0:64, CM["MEAN64"]:CM["MEAN64"] + 64] = 1.0 / 64.0
    m[:, CM["ONES"]:CM["ONES"] + 128] = 1.0
    m[:, CM["IDENT"]:CM["IDENT"] + 128] = np.eye(128)
    rt = np.zeros((64, 64), np.float32)
    for i in range(8):
        rt[i + 8, i] = -1.0
        rt[i, i + 8] = 1.0
    m[0:64, CM["RT"]:CM["RT"] + 64] = rt
    m[0:64, CM["TRIU"]:CM["TRIU"] + 64] = np.triu(np.ones((64, 64)))
    m[0:4, CM["I4"]:CM["I4"] + 4] = np.eye(4)
    for hd in range(4):
        m[hd, CM["SEL"] + hd * 128:CM["SEL"] + (hd + 1) * 128] = 1.0
    return m.astype(ml_dtypes.bfloat16)


def build_cf():
    f = np.zeros((128, CF_N), np.float32)
    f[0:4, CF["I4"]:CF["I4"] + 4] = np.eye(4)
    for hd in range(4):
        f[hd, CF["SEL"] + hd * 128:CF["SEL"] + (hd + 1) * 128] = 1.0
    r = np.ones(512, np.float32)
    r[0::64] = 0.0
    f[0:4, CF["RESET"]:CF["RESET"] + 512] = r[None, :]
    inv = (500000.0 ** (-(np.arange(8, dtype=np.float32) * 2.0) / 16.0)).astype(np.float32)
    f[0:8, CF["INV"]] = inv
    f[8:16, CF["INV"]] = inv
    caus = np.where(np.arange(128)[None, :] <= np.arange(128)[:, None], 0.0, NEG_BIG)
    f[:, CF["CAUS"]:CF["CAUS"] + 128] = caus
    f[:, CF["POW2"]:CF["POW2"] + N_BISECT] = (0.5 ** np.arange(1, N_BISECT + 1))[None, :]
    f[0:64, CF["TRIU"]:CF["TRIU"] + 64] = np.triu(np.ones((64, 64)))
    return f


def build_nc(nb=2, nl=NL, dbg=(), stages=("mix", "attn", "mlstm", "ffn", "ple")):
    nc = bass.Bass("TRN2", target_bir_lowering=False)

    def din(name, shape, dt=F32):
        return nc.dram_tensor(name, shape, dt, kind="ExternalInput").ap()

    x_d = din("x", [nb, D, S])
    p_d = din("p", [NL, nb, 256, S])
    pos_d = din("pos", [nb, 64, S], I32)
    cst_d = din("cst", [128, CL.n])
    cm_d = din("cm", [128, CM_N], BF16)
    cf_d = din("cf", [128, CF_N])
    w_in_d = din("w_in", [NL, D, IN_W])
    w_qup_d = din("w_q_up", [NL, 256, 512])
    w_iqup_d = din("w_iq_up", [NL, 256, 512])
    w_out_d = din("w_out", [NL, D, D])
    w_ff1_d = din("w_ff1", [NL, D, DFF])
    w_ff2_d = din("w_ff2", [NL, DFF, D])
    w_pg_d = din("w_ple_gate", [NL, D, D])
    w_pl_d = din("w_ple", [NL, 256, D])
    out_d = nc.dram_tensor("out", [nb, D, S], F32, kind="ExternalOutput").ap()

    P = Prog(nc)
    es = contextlib.ExitStack()
    uid = [0]

    def sbt(stack, name, shape, dt):
        uid[0] += 1
        return stack.enter_context(nc.sbuf_tensor("%s_%d" % (name, uid[0]), shape, dt))

    def sb(name, shape, dt):
        return sbt(es, name, shape, dt)

    h = sb("h", [128, 8, S], F32)
    cst = sb("cst", [128, CL.n], F32)
    cm = sb("cm", [128, CM_N], BF16)
    cf = sb("cf", [128, CF_N], F32)
    wb = [sb("wb%d" % i, [128, 4096], BF16) for i in range(3)]
    wS = sb("wS", [128, 8, 8], BF16)
    sq = [sb("sq%d" % i, [128, TB], BF16) for i in range(2)]
    rstd = [sb("rstd%d" % i, [128, TB], F32) for i in range(2)]
    KA = sb("KA", [64, 2, S], BF16)
    IK = sb("IK", [64, S], BF16)
    V = sb("V", [128, 16, 128], BF16)
    cosF = sb("cosF", [64, S], BF16)
    sinF = sb("sinF", [64, S], BF16)
    Cst = sb("Cst", [128, 4, 256], F32)
    Cbf = sb("Cbf", [128, 4, 256], BF16)
    halo = sb("halo", [128, 8, 4], F32)
    mixA = sb("mixA", [64, 8, TB], BF16)
    mixM = sb("mixM", [128, 4, TB], BF16)
    small = sb("small", [128, 16], F32)
    psum = [es.enter_context(nc.psum_tensor("ps%d" % i, [128, 512], F32)) for i in range(8)]
    ps_ctr = [0]
    wb_ctr = [0]

    def bank():
        b = psum[ps_ctr[0] % 4]
        ps_ctr[0] += 1
        return b

    def wbuf():
        b = wb[wb_ctr[0] % 3]
        wb_ctr[0] += 1
        return b

    def isap(v):
        return v is not None and not isinstance(v, (int, float))

    def mm(out, lhsT, rhs, start=True, stop=True):
        return P.add("pe", lambda e: e.matmul(out, lhsT, rhs, start=start, stop=stop),
                     reads=[lhsT, rhs], writes=[out])

    def dma(eng, out, in_, rd=None, wr=None):
        return P.add(eng, lambda e: e.dma_start(out=out, in_=in_),
                     reads=[rd] if rd is not None else [], writes=[wr] if wr is not None else [], dma=True)

    def wload(dst, src):
        return dma("pool", dst, src, wr=dst)

    def act(out, in_, func, bias=None, scale=None):
        kw = {}
        rds = [in_]
        if bias is not None:
            kw["bias"] = bias
            if isap(bias):
                rds.append(bias)
        if scale is not None:
            kw["scale"] = scale
            if isap(scale):
                rds.append(scale)
        return P.add("act", lambda e: e.activation(out, in_, func, **kw), reads=rds, writes=[out])

    def ts(eng, out, in0, s1, s2, op0, op1=None, accum=None):
        rds = [in0] + [s for s in (s1, s2) if isap(s)]
        wrs = [out] + ([accum] if accum is not None else [])
        if accum is not None:
            return P.add(eng, lambda e: e.tensor_scalar(out, in0, s1, s2, op0, op1, accum_out=accum),
                         reads=rds, writes=wrs)
        if op1 is None:
            return P.add(eng, lambda e: e.tensor_scalar(out, in0, s1, s2, op0), reads=rds, writes=wrs)
        return P.add(eng, lambda e: e.tensor_scalar(out, in0, s1, s2, op0, op1), reads=rds, writes=wrs)

    def stt(eng, out, in0, scalar, in1, op0, op1):
        rds = [in0, in1] + ([scalar] if isap(scalar) else [])
        return P.add(eng, lambda e: e.scalar_tensor_tensor(out, in0, scalar, in1, op0, op1),
                     reads=rds, writes=[out])

    def tt(eng, out, in0, in1, op):
        return P.add(eng, lambda e: e.tensor_tensor(out, in0, in1, op), reads=[in0, in1], writes=[out])

    def cp(eng, out, in_):
        if eng == "act":
            return act(out, in_, AF.Copy)
        return P.add(eng, lambda e: e.tensor_copy(out, in_), reads=[in_], writes=[out])

    def recip(out, in_):
        return P.add("dve", lambda e: e.reciprocal(out, in_), reads=[in_], writes=[out])

    def memset(eng, out, val):
        return P.add(eng, lambda e: e.memset(out, val), reads=[], writes=[out])

    def ccol(name, l, c=0, p=128):
        o = CL.off[(name, l)] + c
        return cst[0:p, o:o + 1]

    def cmat(name, p, w):
        return cm[0:p, CM[name]:CM[name] + w]

    dumps = {}

    def dump(name, ap):
        if name in dbg and name not in dumps:
            shp = list(ap.shape)
            d = nc.dram_tensor("dbg_" + name, shp, ap.dtype, kind="ExternalOutput").ap()
            dumps[name] = dma("sp", d, ap, rd=ap)

    dma("sp", cst[:], cst_d, wr=cst[:])
    dma("sp", cm[:], cm_d, wr=cm[:])
    dma("sp", cf[:], cf_d, wr=cf[:])

    def rstd_from_mean(r, ps_ap):
        act(r, ps_ap, AF.Sqrt, bias=EPS)
        recip(r, r)

    def rmsnorm_block(l, gname, src_tsl, dst, dst_tsl):
        ps = bank()
        for c in range(8):
            s = sq[c % 2]
            act(s[:], h[:, c, src_tsl], AF.Square)
            mm(ps[:], cmat("MEAN1024", 128, 128), s[:], c == 0, c == 7)
        r = rstd[ps_ctr[0] % 2]
        rstd_from_mean(r[:], ps[:])
        for c in range(8):
            stt("dve", dst[:, c, dst_tsl], h[:, c, src_tsl], ccol(gname, l, c), r[:], ALU.mult, ALU.mult)


    def red(eng, out, in_, op):
        return P.add(eng, lambda e: e.tensor_reduce(out, in_, AX.X, op), reads=[in_], writes=[out])

    def scan(out, d0, d1, init, op0, op1):
        return P.add("dve", lambda e: e.tensor_tensor_scan(out, d0, d1, init, op0, op1),
                     reads=[d0, d1], writes=[out])

    acc_banks = psum[4:8]

    def mixer_layer(b, l):
        nfb = small[0:4, 0:1]
        ts("dve", nfb, ccol("f_bias", l, 0, 4), -1.0, None, ALU.mult)
        negM = small[:, 1:2]
        gq = small[:, 2:3]
        gk = small[:, 3:4]
        lst = contextlib.ExitStack()
        tg = sbt(lst, "tg", [128, 64], F32)
        for nm, dstc in (("g_qn_row", gq), ("g_kn_row", gk)):
            o = CL.off[(nm, l)]
            row = cst[:, o:o + 64]
            ts("dve", tg[:], row, -1.0, None, ALU.mult)
            tt("dve", tg[:], tg[:], row, ALU.max)
            red("dve", dstc, tg[:], ALU.max)
        stt("dve", negM, gq, -8.0, gk, ALU.mult, ALU.mult)
        lst.close()
        memset("dve", Cst[:], 0.0)
        memset("pool", Cbf[:], 0.0)
        memset("pool", halo[:], 0.0)

        for j in range(NTB):
            tsl = slice(j * TB, (j + 1) * TB)
            blk = contextlib.ExitStack()
            xnb = sbt(blk, "xnb", [128, 8, TB], BF16)
            rmsnorm_block(l, "g_mix", tsl, xnb, slice(0, TB))
            if "attn" in stages:
                attn_block(b, l, j, tsl, xnb)
                P.barrier()
            if "mlstm" in stages:
                mlstm_block(b, l, j, tsl, xnb)
            blk.close()
            P.barrier()

    def head_fm(hx, ps_in, gcol, dst, cs32):
        xs, xb, t1, rs, sqh = hx
        cos32, sin32 = cs32
        cp("act", xs[:], ps_in)
        if gcol is not None:
            act(sqh[:], ps_in, AF.Square)
            psm = bank()
            mm(psm[0:64, :], cmat("MEAN64", 64, 64), sqh[:])
            rstd_from_mean(rs[:], psm[0:64, :])
            stt("dve", xs[:], xs[:], gcol, rs[:], ALU.mult, ALU.mult)
        cp("pool", xb[:], xs[:])
        psr = bank()
        mm(psr[0:64, :], cmat("RT", 64, 64), xb[:])
        tt("dve", t1[:], xs[:], cos32, ALU.mult)
        tt("dve", rs[:], psr[0:64, :], sin32, ALU.mult)
        tt("pool", dst, t1[:], rs[:], ALU.add)

    def attn_block(b, l, j, tsl, xnb):
        ph = contextlib.ExitStack()
        IQb = sbt(ph, "QIb", [64, 8, TB], BF16)
        QAb = IQb
        acc = sbt(ph, "acc", [128, S], F32)
        tmp = [sbt(ph, "tmp%d" % i, [128, TB], F32) for i in range(2)]
        maskb = sbt(ph, "maskb", [128, S], BF16)
        maskT = sbt(ph, "maskT", [128, 16, TB], BF16)
        pT = [sbt(ph, "pT%d" % i, [128, TB], BF16) for i in range(2)]
        hx = (sbt(ph, "xs", [64, TB], F32), sbt(ph, "xb", [64, TB], BF16), sbt(ph, "t1", [64, TB], F32),
              sbt(ph, "rs", [64, TB], F32), sbt(ph, "sqh", [64, TB], BF16))
        cqf = acc[:, 0:2 * TB].rearrange("p (c n) -> p c n", c=2)
        cqn = sbt(ph, "cqn", [128, 2, TB], BF16)
        wabs = sbt(ph, "wabs", [128, 4, 8], F32)
        wsgn = sbt(ph, "wsgn", [128, 4, 8], F32)
        bs = sbt(ph, "bs", [128, 8 + N_BISECT], F32)
        rc = tmp[0][0:64, :]
        cs32 = (tmp[0][0:64, :], tmp[1][0:64, :])

        def load_cs32():
            cp("act", cs32[0], cosF[:, tsl])
            cp("act", cs32[1], sinF[:, tsl])

        load_cs32()

        b1 = wbuf()
        wcq = b1[:, 0:2048].rearrange("p (c n) -> p c n", c=8)
        wqu = b1[:, 2048:3072].rearrange("p (c n) -> p c n", c=2)
        wiq = b1[:, 3072:4096].rearrange("p (c n) -> p c n", c=2)
        wload(wcq, w_in_d[l, :, 0:256].rearrange("(c p) n -> p c n", p=128))
        wload(wqu, w_qup_d[l].rearrange("(c p) n -> p c n", p=128))
        wload(wiq, w_iqup_d[l].rearrange("(c p) n -> p c n", p=128))
        b2 = wbuf()
        wgk = b2[:, 0:8 * 328].rearrange("p (c n) -> p c n", c=8)
        wload(wgk, w_in_d[l, :, 256:584].rearrange("(c p) n -> p c n", p=128))

        psm = bank()
        for cc in range(2):
            ps = bank()
            for kc in range(8):
                mm(ps[:], wcq[:, kc, cc * 128:(cc + 1) * 128], xnb[:, kc, :], kc == 0, kc == 7)
            cp("act", cqf[:, cc, :], ps[:])
            s = sq[cc % 2]
            act(s[:], ps[:], AF.Square)
            mm(psm[:], cmat("MEAN256", 128, 128), s[:], cc == 0, cc == 1)
        r = rstd[0]
        rstd_from_mean(r[:], psm[:])
        for cc in range(2):
            stt("dve", cqn[:, cc, :], cqf[:, cc, :], ccol("g_cq", l, cc), r[:], ALU.mult, ALU.mult)

        for g in range(2):
            ps = bank()
            for kc in range(8):
                mm(ps[0:64, :], wgk[:, kc, g * 64:(g + 1) * 64], xnb[:, kc, :], kc == 0, kc == 7)
            head_fm(hx, ps[0:64, :], ccol("g_kn", l, 0, 64), KA[:, g, tsl], cs32)
        ps = bank()
        for kc in range(8):
            mm(ps[0:64, :], wgk[:, kc, 256:320], xnb[:, kc, :], kc == 0, kc == 7)
        head_fm(hx, ps[0:64, :], ccol("g_ik", l, 0, 64), IK[:, tsl], cs32)
        for t in range(4):
            ps = bank()
            for kc in range(8):
                mm(ps[:, 0:128], xnb[:, kc, t * 128:(t + 1) * 128], wgk[:, kc, 128:256], kc == 0, kc == 7)
            cp("act", V[:, 4 * j + t, :], ps[:, 0:128])
            ps = bank()
            for kc in range(8):
                mm(ps[:, 0:8], xnb[:, kc, t * 128:(t + 1) * 128], wgk[:, kc, 320:328], kc == 0, kc == 7)
            act(wabs[:, t, :], ps[:, 0:8], AF.Abs, scale=IDX_SCALE)
            act(wsgn[:, t, :], ps[:, 0:8], AF.Sign)
        for hd in range(8):
            ps = bank()
            for kc in range(2):
                mm(ps[0:64, :], wiq[:, kc, hd * 64:(hd + 1) * 64], cqn[:, kc, :], kc == 0, kc == 1)
            head_fm(hx, ps[0:64, :], None, IQb[:, hd, :], cs32)
        if b == 0 and l == 0 and j == 0:
            dump("KA", KA[:, :, 0:TB])
            dump("IK", IK[:, 0:TB])
            dump("IQb", IQb[:])
            dump("V", V[:, 0:4, :])
            dump("wabs", wabs[:])
            dump("wsgn", wsgn[:])

        lo, mid, cnt, tcol, w0, hi, thr = [bs[:, i:i + 1] for i in range(7)]
        Wb = bs[:, 8:8 + N_BISECT]
        for t in range(4):
            qi = 4 * j + t
            L = 128 * (qi + 1)
            qsl = slice(t * 128, (t + 1) * 128)
            for c in range(j + 1):
                ncols = min(512, L - 512 * c)
                cs = slice(512 * c, 512 * c + ncols)
                for hd in range(8):
                    ps = bank()
                    mm(ps[:, 0:ncols], IQb[:, hd, qsl], IK[:, cs])
                    tm = tmp[hd % 2]
                    act(tm[:, 0:ncols], ps[:, 0:ncols], AF.Relu, scale=wabs[:, t, hd:hd + 1])
                    eng = "dve" if hd % 2 == 0 else "pool"
                    if hd == 0:
                        ts(eng, acc[:, cs], tm[:, 0:ncols], wsgn[:, t, hd:hd + 1], None, ALU.mult)
                    elif eng == "dve":
                        stt(eng, acc[:, cs], tm[:, 0:ncols], wsgn[:, t, hd:hd + 1], acc[:, cs], ALU.mult, ALU.add)
                    else:
                        ts("pool", tm[:, 0:ncols], tm[:, 0:ncols], wsgn[:, t, hd:hd + 1], None, ALU.mult)
                        tt("pool", acc[:, cs], acc[:, cs], tm[:, 0:ncols], ALU.add)
            if qi >= 2:
                red("dve", lo, acc[:, 0:L], ALU.min)
                red("dve", hi, acc[:, 0:L], ALU.max)
            tt("dve", acc[:, qi * 128:(qi + 1) * 128], acc[:, qi * 128:(qi + 1) * 128],
               cf[:, CF["CAUS"]:CF["CAUS"] + 128], ALU.add)
            if b == 0 and l == 0 and j == 0 and t == 3:
                dump("acc", acc[:, 0:512])
            if qi < 2:
                memset("dve", lo, -1.0e29)
            else:
                tt("dve", w0, hi, lo, ALU.subtract)
                ts("dve", w0, w0, 1.0001, 1e-12, ALU.mult, ALU.add)
                ts("dve", Wb, cf[:, CF["POW2"]:CF["POW2"] + N_BISECT], w0, None, ALU.mult)
                for k in range(N_BISECT):
                    tt("dve", mid, lo, Wb[:, k:k + 1], ALU.add)
                    ts("dve", maskb[:, 0:L], acc[:, 0:L], mid, None, ALU.is_ge, ALU.add, accum=cnt)
                    stt("dve", tcol, cnt, 255.5, Wb[:, k:k + 1], ALU.is_ge, ALU.mult)
                    tt("dve", lo, lo, tcol, ALU.add)
            ts("dve", maskb[:, 0:L], acc[:, 0:L], lo, None, ALU.is_ge)
            if b == 0 and l == 0 and j == 0 and t == 3:
                dump("maskb", maskb[:, 0:512])
                dump("bs", bs[:])
            for k4 in range(0, qi + 1, 4):
                n = min(4, qi + 1 - k4)
                ps = bank()
                for kk in range(n):
                    mm(ps[:, kk * 128:(kk + 1) * 128], maskb[:, (k4 + kk) * 128:(k4 + kk + 1) * 128],
                       cmat("IDENT", 128, 128))
                cp("act", maskT[:, k4:k4 + n, qsl], ps[:, 0:n * 128].rearrange("p (a q) -> p a q", a=n))
            for kt in range(qi + 1, 4 * (j + 1)):
                memset("pool", maskT[:, kt, qsl], 0.0)

        load_cs32()
        for hd in range(8):
            ps = bank()
            for kc in range(2):
                mm(ps[0:64, :], wqu[:, kc, hd * 64:(hd + 1) * 64], cqn[:, kc, :], kc == 0, kc == 1)
            head_fm(hx, ps[0:64, :], ccol("g_qn", l, 0, 64), QAb[:, hd, :], cs32)
        if b == 0 and l == 0 and j == 0:
            dump("QAb", QAb[:])
        nkt = 4 * (j + 1)
        for hd in range(8):
            g = hd // 4
            psn = acc_banks[(hd % 2) * 2]
            psd = acc_banks[(hd % 2) * 2 + 1]
            for kt in range(nkt):
                ps = bank()
                mm(ps[:], KA[:, g, kt * 128:(kt + 1) * 128], QAb[:, hd, :])
                pt = pT[kt % 2]
                act(pt[:], ps[:], AF.Exp, bias=small[:, 1:2], scale=0.125)
                tt("dve" if kt % 2 == 0 else "pool", pt[:], pt[:], maskT[:, kt, :], ALU.mult)
                mm(psn[0:64, :], V[:, kt, g * 64:(g + 1) * 64], pt[:], kt == 0, kt == nkt - 1)
                mm(psd[0:64, :], cmat("ONES", 128, 64), pt[:], kt == 0, kt == nkt - 1)
            recip(rc, psd[0:64, :])
            tt("dve", mixA[:, hd, :], psn[0:64, :], rc, ALU.mult)
        if b == 0 and l == 0 and j == 0:
            dump("mixA", mixA[:])
        wo = []
        for half in range(2):
            bb = wbuf()
            v = bb[0:64, :].rearrange("p (c n) -> p c n", c=4)
            wload(v, w_out_d[l, half * 256:(half + 1) * 256, :].rearrange("(c p) n -> p c n", p=64))
            wo.append(v)
        for dc in range(8):
            ps = bank()
            for hd in range(8):
                mm(ps[:], wo[hd // 4][:, hd % 4, dc * 128:(dc + 1) * 128], mixA[:, hd, :], hd == 0, hd == 7)
            tt("dve", h[:, dc, tsl], h[:, dc, tsl], ps[:], ALU.add)
        ph.close()

    def mlstm_block(b, l, j, tsl, xnb):
        ph = contextlib.ExitStack()
        lic = sbt(ph, "lic", [4, TB], F32)
        l1 = sbt(ph, "l1", [4, TB], F32)
        nbt = sbt(ph, "nbt", [4, TB], F32)
        aa = lic
        ag = l1
        eaeg = sbt(ph, "eaeg", [64, 8, 8], F32)
        EB = [sbt(ph, "EB%d" % i, [128, TB], BF16) for i in range(4)]
        EBL = sbt(ph, "EBL", [128, 4, 8], F32)
        xc = [sbt(ph, "xc%d" % i, [128, 4 + TB], F32) for i in range(2)]
        cacc = [sbt(ph, "cacc%d" % i, [128, TB], F32) for i in range(2)]
        qf = sbt(ph, "qf", [128, 4, TB], BF16)
        kf = sbt(ph, "kf", [128, 4, TB], BF16)
        kg = sbt(ph, "kg", [64, 8, 4, 128], BF16)
        vt = sbt(ph, "vt", [64, 8, 512], BF16)
        og = sbt(ph, "og", [128, 4, TB], BF16)
        Ssb = [sbt(ph, "Ssb%d" % i, [64, 64], BF16) for i in range(4)]
        hm = cacc[1]
        dn = cacc[0]

        wload(wS[:], w_in_d[l, :, 2632:2640].rearrange("(c p) n -> p c n", p=128))
        ps_i = bank()
        for kc in range(8):
            mm(ps_i[0:4, :], wS[:, kc, 0:4], xnb[:, kc, :], kc == 0, kc == 7)
        ps_f = bank()
        for kc in range(8):
            mm(ps_f[0:4, :], wS[:, kc, 4:8], xnb[:, kc, :], kc == 0, kc == 7)
        ts("dve", lic[:], ps_i[0:4, :], ccol("i_bias", l, 0, 4), None, ALU.add)
        act(l1[:], ps_f[0:4, :], AF.Exp, bias=small[0:4, 0:1], scale=-1.0)
        act(l1[:], l1[:], AF.Ln, bias=1.0)
        scan(nbt[:], cf[0:4, CF["RESET"]:CF["RESET"] + TB], l1[:], 0.0, ALU.mult, ALU.add)
        tt("dve", aa[:], lic[:], nbt[:], ALU.add)
        nb3 = nbt[:, :].rearrange("p (c s) -> p c s", c=8)
        for c in range(8):
            ts("dve", ag[:, c * 64:(c + 1) * 64], aa[:, c * 64:(c + 1) * 64], nbt[:, c * 64 + 63:c * 64 + 64], None,
               ALU.subtract)
        if KSTOP <= 1:
            ph.close()
            return
        hl = {}
        for nm, src in (("aa", aa), ("ag", ag), ("nb", nbt)):
            hi = sbt(ph, nm + "hi", [4, TB], BF16)
            lo_ = sbt(ph, nm + "lo", [4, TB], BF16)
            hf = xc[0][0:4, 0:TB]
            cp("dve", hi[:], src[:])
            cp("dve", hf, hi[:])
            tt("dve", hf, src[:], hf, ALU.subtract)
            cp("dve", lo_[:], hf)
            hl[nm] = (hi, lo_)
        i4 = cm[0:4, CM["I4"]:CM["I4"] + 4]
        psg = bank()
        for c in range(8):
            for q_, nm in ((0, "aa"), (4, "ag")):
                hi, lo_ = hl[nm]
                mm(psg[0:64, c * 8 + q_:c * 8 + q_ + 4], hi[:, c * 64:(c + 1) * 64], i4, True, False)
                mm(psg[0:64, c * 8 + q_:c * 8 + q_ + 4], lo_[:, c * 64:(c + 1) * 64], i4, False, True)
        act(eaeg[:, :, :].rearrange("p c n -> p (c n)"), psg[0:64, 0:64], AF.Exp)
        for hd in range(4):
            ps = bank()
            sel = cm[0:4, CM["SEL"] + hd * 128:CM["SEL"] + (hd + 1) * 128]
            mm(ps[:], sel, hl["nb"][0][:], True, False)
            mm(ps[:], sel, hl["nb"][1][:], False, True)
            act(EB[hd][:], ps[:], AF.Exp, scale=-1.0)
            act(EBL[:, hd, :], ps[:, 63:512:64], AF.Exp, scale=-1.0)
        if b == 0 and l == 0 and j == 0:
            dump("lic", lic[:])
            dump("nbt", nbt[:])
            dump("eaeg", eaeg[:])
            dump("EB0", EB[0][:])

        if KSTOP <= 2:
            ph.close()
            return
        for which in range(2):
            wbb = wbuf()
            w = wbb[:, :].rearrange("p (c n) -> p c n", c=8)
            c0 = 584 + which * 512
            wload(w, w_in_d[l, :, c0:c0 + 512].rearrange("(c p) n -> p c n", p=128))
            for cc in range(4):
                ch = which * 4 + cc
                ps = bank()
                for kc in range(8):
                    mm(ps[:], w[:, kc, cc * 128:(cc + 1) * 128], xnb[:, kc, :], kc == 0, kc == 7)
                x_ = xc[cc % 2]
                ca = cacc[cc % 2]
                cp("pool", x_[:, 0:4], halo[:, ch, :])
                cp("act", x_[:, 4:4 + TB], ps[:])
                if KSUB <= 1:
                    continue
                cw = CL.off[("conv_w", l)]
                act(ca[:], x_[:, 1:1 + TB], AF.Identity, bias=ccol("conv_b", l, ch), scale=cst[:, cw + ch:cw + ch + 1])
                for jt in range(1, 4):
                    stt("dve", ca[:], x_[:, 1 + jt:1 + jt + TB],
                        cst[:, cw + jt * 8 + ch:cw + jt * 8 + ch + 1], ca[:], ALU.mult, ALU.add)
                if KSUB <= 2:
                    continue
                cp("pool", halo[:, ch, :], x_[:, TB:TB + 4])
                if KSUB <= 3:
                    continue
                sg = x_[:, 4:4 + TB]
                act(sg, ca[:], AF.Sigmoid)
                if which == 0:
                    tt("dve", ca[:], ca[:], sg, ALU.mult)
                    ts("dve", qf[:, cc, :], ca[:], float(128 ** -0.5), None, ALU.mult)
                    tt("pool", qf[:, cc, :], qf[:, cc, :], EB[cc][:], ALU.mult)
                else:
                    tt("dve", kf[:, cc, :], ca[:], sg, ALU.mult)
        if KSTOP <= 3:
            ph.close()
            return
        wbb = wbuf()
        wv = wbb[:, :].rearrange("p (c n) -> p c n", c=8)
        wload(wv, w_in_d[l, :, 1608:2120].rearrange("(c p) n -> p c n", p=128))
        for c in range(8):
            ps = bank()
            for kc in range(8):
                mm(ps[0:64, :], xnb[:, kc, c * 64:(c + 1) * 64], wv[:, kc, :], kc == 0, kc == 7)
            cp("act" if c % 2 == 0 else "dve", vt[:, c, :], ps[0:64, :])
        if KSUB2 <= 1:
            ph.close()
            return
        for c in range(8):
            ps = bank()
            for hd in range(4):
                mm(ps[0:64, hd * 128:(hd + 1) * 128], kf[:, hd, c * 64:(c + 1) * 64], cmat("IDENT", 128, 128))
            for hd in range(4):
                ts("dve", kg[:, c, hd, :], ps[0:64, hd * 128:(hd + 1) * 128], eaeg[:, c, 4 + hd:5 + hd], None, ALU.mult)
        if KSUB2 <= 2:
            ph.close()
            return
        wbb = wbuf()
        w = wbb[:, :].rearrange("p (c n) -> p c n", c=8)
        wload(w, w_in_d[l, :, 2120:2632].rearrange("(c p) n -> p c n", p=128))
        for cc in range(4):
            ps = bank()
            for kc in range(8):
                mm(ps[:], w[:, kc, cc * 128:(cc + 1) * 128], xnb[:, kc, :], kc == 0, kc == 7)
            act(og[:, cc, :], ps[:], AF.Sigmoid)
        if b == 0 and l == 0 and j == 0:
            dump("qf", qf[:])
            dump("kf", kf[:])
            dump("vt", vt[:])
            dump("kg", kg[:])
            dump("og", og[:])

        if KSTOP <= 4:
            ph.close()
            return
        for pair in range(2):
            for c in range(8):
                csl = slice(c * 64, (c + 1) * 64)
                for hh in range(2):
                    hd = pair * 2 + hh
                    psN = acc_banks[hh * 2]
                    psDn = acc_banks[hh * 2 + 1]
                    psS = bank()
                    mm(psS[0:64, 0:64], kf[:, hd, csl], qf[:, hd, csl])
                    Sb = Ssb[(c * 2 + hh) % 4]
                    stt("dve", Sb[:], psS[0:64, 0:64], eaeg[:, c, hd:hd + 1], cf[0:64, CF["TRIU"]:CF["TRIU"] + 64], ALU.mult, ALU.mult)
                    mm(psN[:, csl], Cbf[:, hd, 0:128], qf[:, hd, csl], True, False)
                    mm(psN[:, csl], vt[:, c, hd * 128:(hd + 1) * 128], Sb[:], False, True)
                    mm(psDn[:, csl], Cbf[:, hd, 128:256], qf[:, hd, csl], True, False)
                    mm(psDn[:, csl], cmat("ONES", 64, 128), Sb[:], False, True)
                    psD = bank()
                    mm(psD[:, 0:128], kg[:, c, hd, :], vt[:, c, hd * 128:(hd + 1) * 128])
                    mm(psD[:, 128:256], kg[:, c, hd, :], cmat("ONES", 64, 128))
                    stt("dve", Cst[:, hd, :], Cst[:, hd, :], EBL[:, hd, c:c + 1], psD[:, 0:256],
                        ALU.mult, ALU.add)
                    cp("act", Cbf[:, hd, :], Cst[:, hd, :])
            for hh in range(2):
                hd = pair * 2 + hh
                psN = acc_banks[hh * 2]
                psDn = acc_banks[hh * 2 + 1]
                act(dn[:], psDn[:], AF.Abs)
                ts("dve", dn[:], dn[:], 1.0, None, ALU.max)
                recip(dn[:], dn[:])
                tt("dve", hm[:], psN[:], dn[:], ALU.mult)
                if b == 0 and l == 0 and j == 0 and hd == 0:
                    dump("hm0", hm[:])
                s = sq[hh]
                act(s[:], hm[:], AF.Square)
                psm = bank()
                mm(psm[:], cmat("MEAN128", 128, 128), s[:])
                r = rstd[hh]
                rstd_from_mean(r[:], psm[:])
                g_o = CL.off[("g_mh", l)] + hd
                hmb = sq[hh]
                stt("dve", hmb[:], hm[:], cst[:, g_o:g_o + 1], r[:], ALU.mult, ALU.mult)
                tt("pool", mixM[:, hd, :], hmb[:], og[:, hd, :], ALU.mult)
        if b == 0 and l == 0 and j == 0:
            dump("mixM", mixM[:])
        wbb = wbuf()
        wo = wbb[:, :].rearrange("p (c n) -> p c n", c=4)
        wload(wo, w_out_d[l, 512:1024, :].rearrange("(c p) n -> p c n", p=128))
        for dc in range(8):
            ps = bank()
            for kc in range(4):
                mm(ps[:], wo[:, kc, dc * 128:(dc + 1) * 128], mixM[:, kc, :], kc == 0, kc == 3)
            tt("dve", h[:, dc, tsl], h[:, dc, tsl], ps[:], ALU.add)
        ph.close()

    final_ops = []

    for b in range(nb):
        for c in range(8):
            dma("sp", h[:, c, :], x_d[b, c * 128:(c + 1) * 128, :], wr=h[:, c, :])
        if "mix" in stages:
            ph = contextlib.ExitStack()
            posi = sbt(ph, "posi", [64, S], I32)
            ang = sbt(ph, "ang", [64, S], F32)
            kf = sbt(ph, "kf", [64, S], F32)
            ki = sbt(ph, "ki", [64, S], I32)
            msk = sbt(ph, "msk", [64, S], F32)
            dma("sp", posi[:], pos_d[b], wr=posi[:])
            cp("dve", ang[:], posi[:])
            ts("dve", ang[:], ang[:], cf[0:64, CF["INV"]:CF["INV"] + 1], None, ALU.mult)
            for (dstT, shift) in ((sinF, 0.0), (cosF, np.pi / 2)):
                ts("dve", kf[:], ang[:], shift, 1.0 / TWO_PI, ALU.add, ALU.mult)
                cp("dve", ki[:], kf[:])
                cp("dve", kf[:], ki[:])
                ts("dve", msk[:], ang[:], shift, None, ALU.add)
                stt("dve", kf[:], kf[:], -TWO_PI, msk[:], ALU.mult, ALU.add)
                ts("dve", msk[:], kf[:], float(np.pi), None, ALU.is_gt)
                stt("dve", kf[:], msk[:], -TWO_PI, kf[:], ALU.mult, ALU.add)
                ts("dve", msk[:], kf[:], float(-np.pi), None, ALU.is_lt)
                stt("dve", kf[:], msk[:], TWO_PI, kf[:], ALU.mult, ALU.add)
                ts("dve", kf[:], kf[:], float(np.pi), float(-np.pi), ALU.min, ALU.max)
                act(dstT[:], kf[:], AF.Sin)
            dump("cosF", cosF[:])
            dump("sinF", sinF[:])
            ph.close()
            P.barrier()

        for l in range(nl):
            if "mix" in stages:
                mixer_layer(b, l)
            ph = contextlib.ExitStack()
            xn = sbt(ph, "xn", [128, 8, S], BF16)
            ubuf = sbt(ph, "ubuf", [128, 4, S], BF16)
            relu_t = [sbt(ph, "relu%d" % i, [128, TB], F32) for i in range(2)]
            if "ffn" in stages:
                for tb in range(NTB):
                    tsl = slice(tb * TB, (tb + 1) * TB)
                    rmsnorm_block(l, "g_mlp", tsl, xn, tsl)
                for j in range(8):
                    w1b = wbuf()
                    w2b = wbuf()
                    w1 = w1b[:, :].rearrange("p (c n) -> p c n", c=8)
                    w2 = w2b[:, :].rearrange("p (c n) -> p c n", c=4)
                    wload(w1, w_ff1_d[l, :, j * 512:(j + 1) * 512].rearrange("(c p) n -> p c n", p=128))
                    wload(w2, w_ff2_d[l, j * 512:(j + 1) * 512, :].rearrange("(c p) n -> p c n", p=128))
                    for tb in range(NTB):
                        tsl = slice(tb * TB, (tb + 1) * TB)
                        for fc in range(4):
                            ps = bank()
                            for kc in range(8):
                                mm(ps[:], w1[:, kc, fc * 128:(fc + 1) * 128], xn[:, kc, tsl], kc == 0, kc == 7)
                            rl = relu_t[fc % 2]
                            act(rl[:], ps[:], AF.Relu)
                            tt("dve" if fc % 2 == 0 else "pool", ubuf[:, fc, tsl], rl[:], rl[:], ALU.mult)
                    for tb in range(NTB):
                        tsl = slice(tb * TB, (tb + 1) * TB)
                        for dc in range(8):
                            ps = bank()
                            for fc in range(4):
                                mm(ps[:], w2[:, fc, dc * 128:(dc + 1) * 128], ubuf[:, fc, tsl], fc == 0, fc == 3)
                            tt("dve", h[:, dc, tsl], h[:, dc, tsl], ps[:], ALU.add)
            if "ple" in stages:
                for tb in range(NTB):
                    tsl = slice(tb * TB, (tb + 1) * TB)
                    rmsnorm_block(l, "g_ple", tsl, xn, tsl)
                pb = ubuf
                for kc in range(2):
                    wload(pb[:, kc, :], p_d[l, b, kc * 128:(kc + 1) * 128, :])
                for half in range(2):
                    wgb = wbuf()
                    wpb = wbuf()
                    wg = wgb[:, :].rearrange("p (c n) -> p c n", c=8)
                    wp = wpb[:, 0:1024].rearrange("p (c n) -> p c n", c=2)
                    wload(wg, w_pg_d[l, :, half * 512:(half + 1) * 512].rearrange("(c p) n -> p c n", p=128))
                    wload(wp, w_pl_d[l, :, half * 512:(half + 1) * 512].rearrange("(c p) n -> p c n", p=128))
                    for tb in range(NTB):
                        tsl = slice(tb * TB, (tb + 1) * TB)
                        for dcl in range(4):
                            dc = half * 4 + dcl
                            ps = bank()
                            for kc in range(8):
                                mm(ps[:], wg[:, kc, dcl * 128:(dcl + 1) * 128], xn[:, kc, tsl], kc == 0, kc == 7)
                            g = relu_t[dcl % 2]
                            act(g[:], ps[:], AF.Sigmoid, bias=ccol("b_ple", l, dc))
                            ps2 = bank()
                            for kc in range(2):
                                mm(ps2[:], wp[:, kc, dcl * 128:(dcl + 1) * 128], pb[:, kc, tsl], kc == 0, kc == 1)
                            tt("dve", g[:], g[:], ps2[:], ALU.mult)
                            tt("pool", h[:, dc, tsl], h[:, dc, tsl], g[:], ALU.add)
            ph.close()
            P.barrier()
        for c in range(8):
            final_ops.append(dma("sp", out_d[b, c * 128:(c + 1) * 128, :], h[:, c, :], rd=h[:, c, :]))

    final_ops += list(dumps.values())
    P.emit(final_wait_ops=final_ops)
    es.close()
    return nc


def make_in_maps(inp, nb, ncores):
    cst = build_cst(inp)
    cm = build_cm()
    cf = build_cf()
    x = inp["x"]
    p = inp["p"]
    pos = inp["positions"].astype(np.int32)
    in_maps = []
    for c in range(ncores):
        bs = slice(c * nb, (c + 1) * nb)
        m = {
            "x": np.ascontiguousarray(x[bs].transpose(0, 2, 1)),
            "p": np.ascontiguousarray(p[:, bs].transpose(0, 1, 3, 2)),
            "pos": np.ascontiguousarray(np.broadcast_to(pos[bs][:, None, :], (nb, 64, S))),
            "cst": cst, "cm": cm, "cf": cf,
        }
        for k in ("w_in", "w_q_up", "w_iq_up", "w_out", "w_ff1", "w_ff2", "w_ple_gate", "w_ple"):
            m[k] = np.ascontiguousarray(inp[k], dtype=np.float32)
        in_maps.append(m)
    return in_maps


def kernel(**inputs):
    inp = {k: np.asarray(v) for k, v in inputs.items()}
    nb = 2
    nc = build_nc(nb=nb, stages=RUN_STAGES)
    in_maps = make_in_maps(inp, nb, NCORES)
    res = run_bass_kernel_spmd(nc, in_maps, core_ids=list(range(NCORES)))
    out = np.concatenate([r["out"] for r in res.results], axis=0)
    return np.ascontiguousarray(out.transpose(0, 2, 1)).astype(np.float32)
```

```python
import contextlib
import os
import numpy as np
import concourse.bass as bass
import concourse.mybir as mybir
from concourse.bass_utils import run_bass_kernel_spmd

F32 = mybir.dt.float32
BF16 = mybir.dt.bfloat16
I32 = mybir.dt.int32
ALU = mybir.AluOpType
AF = mybir.ActivationFunctionType
AX = mybir.AxisListType

D = 1024
S = 2048
NL = 2
NCORES = 8
TB = 512
NTB = S // TB
EPS = 1e-6
IN_W = 2640
DFF = 4096

ENGS = ("pe", "act", "dve", "pool", "sp")
N_DMA_SEMS = 16


def _region(ap):
    if type(ap.tensor).__name__.startswith("PSum"):
        return (ap.tensor.name, 0, 128, 0, 1 << 30)
    pat = ap.ap
    row = pat[0][0]
    npart = pat[0][1]
    off = ap.offset
    if row <= 0:
        p0, f0 = 0, off
    else:
        p0 = off // row
        f0 = off - p0 * row
    ext = 1
    for st, cnt in pat[1:]:
        ext += (cnt - 1) * abs(st)
    sz = mybir.dt.size(ap.dtype)
    return (ap.tensor.name, p0, p0 + npart, f0 * sz, (f0 + ext) * sz)


def _overlap(a, b):
    return a[1] < b[2] and b[1] < a[2] and a[3] < b[4] and b[3] < a[4]


class Op:
    __slots__ = ("eng", "fn", "reads", "writes", "dma", "deps", "sig", "idx", "dmak")


class Prog:
    def __init__(self, nc):
        self.nc = nc
        self.ops = []
        self.hist = {}
        self.last_op = {}
        self.bar_id = 0
        self.bar_deps = set()
        self.eng_bar = {}
        self.dma_pending = []
        self.pending = {}

    def barrier(self):
        deps = set(self.last_op.values()) | set(self.dma_pending)
        self.dma_pending = []
        for e in ENGS:
            self.pending.setdefault(e, set()).update(deps)

    def add(self, eng, fn, reads=(), writes=(), dma=False):
        op = Op()
        op.eng, op.fn, op.dma = eng, fn, dma
        op.reads = [_region(a) for a in reads if a is not None]
        op.writes = [_region(a) for a in writes if a is not None]
        op.deps = set()
        op.sig = None
        op.dmak = None
        op.idx = len(self.ops)
        self.ops.append(op)
        ops = self.ops
        for r in op.reads:
            for (reg, oi, isw) in self.hist.setdefault(r[0], []):
                if isw and _overlap(reg, r):
                    op.deps.add(oi)
        for w in op.writes:
            keep = []
            for rec in self.hist.setdefault(w[0], []):
                reg, oi, isw = rec
                if _overlap(reg, w):
                    op.deps.add(oi)
                    if w[1] <= reg[1] and reg[2] <= w[2] and w[3] <= reg[3] and reg[4] <= w[4]:
                        continue
                keep.append(rec)
            self.hist[w[0]] = keep
        for r in op.reads:
            h = self.hist[r[0]]
            if not dma:
                h[:] = [rec for rec in h if not ((not rec[2]) and rec[0] == r
                                                 and ops[rec[1]].eng == eng and not ops[rec[1]].dma)]
            h.append((r, op.idx, False))
        for w in op.writes:
            self.hist[w[0]].append((w, op.idx, True))
        if self.pending.get(eng):
            op.deps |= self.pending[eng]
            self.pending[eng] = set()
        if not dma:
            self.last_op[eng] = op.idx
        else:
            self.dma_pending.append(op.idx)
        op.deps.discard(op.idx)
        return op

    def emit(self, final_wait_ops=()):
        nc = self.nc
        ops = self.ops
        for op in ops:
            if op.eng == "pe" and not op.dma:
                op.deps = {d for d in op.deps if not (ops[d].eng == "pe" and not ops[d].dma)}
        needed = set()
        for op in ops:
            needed |= op.deps
        for o in final_wait_ops:
            needed.add(o.idx)
        cnt = {e: 0 for e in ENGS}
        dmak = {"sp": 0, "pool": 0, "act": 0}
        dma_ops = {"sp": [], "pool": [], "act": []}
        for op in ops:
            if op.dma:
                k = dmak[op.eng]
                op.dmak = k
                op.sig = ("dma", (op.eng, k % N_DMA_SEMS), 16 * (k // N_DMA_SEMS + 1))
                dmak[op.eng] += 1
                dma_ops[op.eng].append(op)
            elif op.idx in needed:
                cnt[op.eng] += 1
                op.sig = (op.eng, cnt[op.eng])
        with contextlib.ExitStack() as st:
            sems = {e: st.enter_context(nc.semaphore("s_" + e)) for e in ENGS}
            dsems = {(q, i): st.enter_context(nc.semaphore("d_%s_%d" % (q, i)))
                     for q in ("sp", "pool") for i in range(N_DMA_SEMS)}
            block = st.enter_context(nc.Block())
            per_eng = {e: [op for op in ops if op.eng == e] for e in ENGS}

            def run(engname, eng):
                known = {}
                for op in per_eng[engname]:
                    waits = {}
                    deps = set(op.deps)
                    if op.dma and op.dmak >= N_DMA_SEMS:
                        deps.add(dma_ops[op.eng][op.dmak - N_DMA_SEMS].idx)
                    for d in deps:
                        s = ops[d].sig
                        if s[0] == "dma":
                            key, v = ("dma", s[1]), s[2]
                        else:
                            key, v = ("c", s[0]), s[1]
                        if known.get(key, 0) >= v:
                            continue
                        if waits.get(key, 0) < v:
                            waits[key] = v
                    for key, v in waits.items():
                        sem = dsems[key[1]] if key[0] == "dma" else sems[key[1]]
                        eng.wait_ge(sem, v)
                        known[key] = v
                    ins = op.fn(eng)
                    if op.sig is not None:
                        if op.sig[0] == "dma":
                            ins.then_inc(dsems[op.sig[1]], 16)
                        else:
                            ins.then_inc(sems[op.sig[0]], 1)
                if engname == "sp":
                    for o in final_wait_ops:
                        s = o.sig
                        if s[0] == "dma":
                            eng.wait_ge(dsems[s[1]], s[2])
                        else:
                            eng.wait_ge(sems[s[0]], s[1])

            @block.tensor
            def _(e):
                run("pe", e)

            @block.scalar
            def _(e):
                run("act", e)

            @block.vector
            def _(e):
                run("dve", e)

            @block.gpsimd
            def _(e):
                run("pool", e)

            @block.sync
            def _(e):
                run("sp", e)


IDX_SCALE = (8 ** -0.5) * (64 ** -0.5)
KSTOP = int(os.environ.get('K_STOP', '99'))
RUN_STAGES = ("mix", "attn", "mlstm", "ffn", "ple")
KSUB = int(os.environ.get('K_SUB', '99'))
KSUB2 = int(os.environ.get('K_SUB2', '99'))
NEG_BIG = -1.0e30
N_BISECT = 24
TWO_PI = 2.0 * np.pi


def _fm_cols(v, p=128):
    v = np.asarray(v, np.float32)
    return np.ascontiguousarray(v.reshape(-1, p).T)


class CstLayout:
    def __init__(self):
        self.off = {}
        self.n = 0

    def add(self, name, ncols):
        self.off[name] = self.n
        self.n += ncols


def make_cst_layout():
    L = CstLayout()
    for l in range(NL):
        for nm in ("g_mix", "g_mlp", "g_ple", "b_ple", "conv_b"):
            L.add((nm, l), 8)
        L.add(("g_cq", l), 2)
        for nm in ("g_qn", "g_kn", "g_ik", "i_bias", "f_bias"):
            L.add((nm, l), 1)
        L.add(("conv_w", l), 32)
        L.add(("g_mh", l), 4)
        L.add(("g_qn_row", l), 64)
        L.add(("g_kn_row", l), 64)
    return L


CL = make_cst_layout()


def build_cst(inp):
    c = np.zeros((128, CL.n), np.float32)
    for l in range(NL):
        for nm, key in (("g_mix", "g_mix"), ("g_mlp", "g_mlp"), ("g_ple", "g_ple"),
                        ("b_ple", "b_ple_gate"), ("conv_b", "conv_b")):
            o = CL.off[(nm, l)]
            c[:, o:o + 8] = _fm_cols(inp[key][l])
        o = CL.off[("g_cq", l)]
        c[:, o:o + 2] = _fm_cols(inp["g_cq"][l])
        for nm in ("g_qn", "g_kn", "g_ik"):
            o = CL.off[(nm, l)]
            c[0:64, o] = inp[nm][l]
        for nm in ("i_bias", "f_bias"):
            o = CL.off[(nm, l)]
            c[0:4, o] = inp[nm][l]
        o = CL.off[("conv_w", l)]
        for j in range(4):
            c[:, o + j * 8:o + j * 8 + 8] = _fm_cols(inp["conv_w"][l, j])
        o = CL.off[("g_mh", l)]
        c[:, o:o + 4] = np.ascontiguousarray(inp["g_mh"][l].T)
        o = CL.off[("g_qn_row", l)]
        c[:, o:o + 64] = np.broadcast_to(inp["g_qn"][l][None, :], (128, 64))
        o = CL.off[("g_kn_row", l)]
        c[:, o:o + 64] = np.broadcast_to(inp["g_kn"][l][None, :], (128, 64))
    return c


CM = {}
_o = 0
for _nm, _w in (("MEAN1024", 128), ("MEAN256", 128), ("MEAN128", 128), ("MEAN64", 64), ("ONES", 128),
                ("IDENT", 128), ("RT", 64), ("TRIU", 64), ("I4", 4), ("SEL", 512)):
    CM[_nm] = _o
    _o += _w
CM_N = _o

CF = {}
_o = 0
for _nm, _w in (("I4", 4), ("SEL", 512), ("RESET", 512), ("INV", 1), ("CAUS", 128), ("POW2", N_BISECT), ("TRIU", 64)):
    CF[_nm] = _o
    _o += _w
CF_N = _o


def build_cm():
    import ml_dtypes
    m = np.zeros((128, CM_N), np.float32)
    m[:, CM["MEAN1024"]:CM["MEAN1024"] + 128] = 1.0 / 1024.0
    m[:, CM["MEAN256"]:CM["MEAN256"] + 128] = 1.0 / 256.0
    m[:, CM["MEAN128"]:CM["MEAN128"] + 128] = 1.0 / 128.0
    m[0:64, CM["MEAN64"]:CM["MEAN64"] + 64] = 1.0 / 64.0
    m[:, CM["ONES"]:CM["ONES"] + 128] = 1.0
    m[:, CM["IDENT"]:CM["IDENT"] + 128] = np.eye(128)
    rt = np.zeros((64, 64), np.float32)
    for i in range(8):
        rt[i + 8, i] = -1.0
        rt[i, i + 8] = 1.0
    m[0:64, CM["RT"]:CM["RT"] + 64] = rt
    m[0:64, CM["TRIU"]:CM["TRIU"] + 64] = np.triu(np.ones((64, 64)))
    m[0:4, CM["I4"]:CM["I4"] + 4] = np.eye(4)
    for hd in range(4):
        m[hd, CM["SEL"] + hd * 128:CM["SEL"] + (hd + 1) * 128] = 1.0
    return m.astype(ml_dtypes.bfloat16)


def build_cf():
    f = np.zeros((128, CF_N), np.float32)
    f[0:4, CF["I4"]:CF["I4"] + 4] = np.eye(4)
    for hd in range(4):
        f[hd, CF["SEL"] + hd * 128:CF["SEL"] + (hd + 1) * 128] = 1.0
    r = np.ones(512, np.float32)
    r[0::64] = 0.0
    f[0:4, CF["RESET"]:CF["RESET"] + 512] = r[None, :]
    inv = (500000.0 ** (-(np.arange(8, dtype=np.float32) * 2.0) / 16.0)).astype(np.float32)
    f[0:8, CF["INV"]] = inv
    f[8:16, CF["INV"]] = inv
    caus = np.where(np.arange(128)[None, :] <= np.arange(128)[:, None], 0.0, NEG_BIG)
    f[:, CF["CAUS"]:CF["CAUS"] + 128] = caus
    f[:, CF["POW2"]:CF["POW2"] + N_BISECT] = (0.5 ** np.arange(1, N_BISECT + 1))[None, :]
    f[0:64, CF["TRIU"]:CF["TRIU"] + 64] = np.triu(np.ones((64, 64)))
    return f


def build_nc(nb=2, nl=NL, dbg=(), stages=("mix", "attn", "mlstm", "ffn", "ple")):
    nc = bass.Bass("TRN2", target_bir_lowering=False)

    def din(name, shape, dt=F32):
        return nc.dram_tensor(name, shape, dt, kind="ExternalInput").ap()

    x_d = din("x", [nb, D, S])
    p_d = din("p", [NL, nb, 256, S])
    pos_d = din("pos", [nb, 64, S], I32)
    cst_d = din("cst", [128, CL.n])
    cm_d = din("cm", [128, CM_N], BF16)
    cf_d = din("cf", [128, CF_N])
    w_in_d = din("w_in", [NL, D, IN_W])
    w_qup_d = din("w_q_up", [NL, 256, 512])
    w_iqup_d = din("w_iq_up", [NL, 256, 512])
    w_out_d = din("w_out", [NL, D, D])
    w_ff1_d = din("w_ff1", [NL, D, DFF])
    w_ff2_d = din("w_ff2", [NL, DFF, D])
    w_pg_d = din("w_ple_gate", [NL, D, D])
    w_pl_d = din("w_ple", [NL, 256, D])
    out_d = nc.dram_tensor("out", [nb, D, S], F32, kind="ExternalOutput").ap()

    P = Prog(nc)
    es = contextlib.ExitStack()
    uid = [0]

    def sbt(stack, name, shape, dt):
        uid[0] += 1
        return stack.enter_context(nc.sbuf_tensor("%s_%d" % (name, uid[0]), shape, dt))

    def sb(name, shape, dt):
        return sbt(es, name, shape, dt)

    h = sb("h", [128, 8, S], F32)
    cst = sb("cst", [128, CL.n], F32)
    cm = sb("cm", [128, CM_N], BF16)
    cf = sb("cf", [128, CF_N], F32)
    wb = [sb("wb%d" % i, [128, 4096], BF16) for i in range(3)]
    wS = sb("wS", [128, 8, 8], BF16)
    sq = [sb("sq%d" % i, [128, TB], BF16) for i in range(2)]
    rstd = [sb("rstd%d" % i, [128, TB], F32) for i in range(2)]
    KA = sb("KA", [64, 2, S], BF16)
    IK = sb("IK", [64, S], BF16)
    V = sb("V", [128, 16, 128], BF16)
    cosF = sb("cosF", [64, S], BF16)
    sinF = sb("sinF", [64, S], BF16)
    Cst = sb("Cst", [128, 4, 256], F32)
    Cbf = sb("Cbf", [128, 4, 256], BF16)
    halo = sb("halo", [128, 8, 4], F32)
    mixA = sb("mixA", [64, 8, TB], BF16)
    mixM = sb("mixM", [128, 4, TB], BF16)
    small = sb("small", [128, 16], F32)
    psum = [es.enter_context(nc.psum_tensor("ps%d" % i, [128, 512], F32)) for i in range(8)]
    ps_ctr = [0]
    wb_ctr = [0]

    def bank():
        b = psum[ps_ctr[0] % 4]
        ps_ctr[0] += 1
        return b

    def wbuf():
        b = wb[wb_ctr[0] % 3]
        wb_ctr[0] += 1
        return b

    def isap(v):
        return v is not None and not isinstance(v, (int, float))

    def mm(out, lhsT, rhs, start=True, stop=True):
        return P.add("pe", lambda e: e.matmul(out, lhsT, rhs, start=start, stop=stop),
                     reads=[lhsT, rhs], writes=[out])

    def dma(eng, out, in_, rd=None, wr=None):
        return P.add(eng, lambda e: e.dma_start(out=out, in_=in_),
                     reads=[rd] if rd is not None else [], writes=[wr] if wr is not None else [], dma=True)

    def wload(dst, src):
        return dma("pool", dst, src, wr=dst)

    def act(out, in_, func, bias=None, scale=None):
        kw = {}
        rds = [in_]
        if bias is not None:
            kw["bias"] = bias
            if isap(bias):
                rds.append(bias)
        if scale is not None:
            kw["scale"] = scale
            if isap(scale):
                rds.append(scale)
        return P.add("act", lambda e: e.activation(out, in_, func, **kw), reads=rds, writes=[out])

    def ts(eng, out, in0, s1, s2, op0, op1=None, accum=None):
        rds = [in0] + [s for s in (s1, s2) if isap(s)]
        wrs = [out] + ([accum] if accum is not None else [])
        if accum is not None:
            return P.add(eng, lambda e: e.tensor_scalar(out, in0, s1, s2, op0, op1, accum_out=accum),
                         reads=rds, writes=wrs)
        if op1 is None:
            return P.add(eng, lambda e: e.tensor_scalar(out, in0, s1, s2, op0), reads=rds, writes=wrs)
        return P.add(eng, lambda e: e.tensor_scalar(out, in0, s1, s2, op0, op1), reads=rds, writes=wrs)

    def stt(eng, out, in0, scalar, in1, op0, op1):
        rds = [in0, in1] + ([scalar] if isap(scalar) else [])
        return P.add(eng, lambda e: e.scalar_tensor_tensor(out, in0, scalar, in1, op0, op1),
                     reads=rds, writes=[out])

    def tt(eng, out, in0, in1, op):
        return P.add(eng, lambda e: e.tensor_tensor(out, in0, in1, op), reads=[in0, in1], writes=[out])

    def cp(eng, out, in_):
        if eng == "act":
            return act(out, in_, AF.Copy)
        return P.add(eng, lambda e: e.tensor_copy(out, in_), reads=[in_], writes=[out])

    def recip(out, in_):
        return P.add("dve", lambda e: e.reciprocal(out, in_), reads=[in_], writes=[out])

    def memset(eng, out, val):
        return P.add(eng, lambda e: e.memset(out, val), reads=[], writes=[out])

    def ccol(name, l, c=0, p=128):
        o = CL.off[(name, l)] + c
        return cst[0:p, o:o + 1]

    def cmat(name, p, w):
        return cm[0:p, CM[name]:CM[name] + w]

    dumps = {}

    def dump(name, ap):
        if name in dbg and name not in dumps:
            shp = list(ap.shape)
            d = nc.dram_tensor("dbg_" + name, shp, ap.dtype, kind="ExternalOutput").ap()
            dumps[name] = dma("sp", d, ap, rd=ap)

    dma("sp", cst[:], cst_d, wr=cst[:])
    dma("sp", cm[:], cm_d, wr=cm[:])
    dma("sp", cf[:], cf_d, wr=cf[:])

    def rstd_from_mean(r, ps_ap):
        act(r, ps_ap, AF.Ln, bias=EPS)
        act(r, r, AF.Exp, scale=-0.5)

    def recip_act(out, in_):
        act(out, in_, AF.Ln)
        act(out, out, AF.Exp, scale=-1.0)

    def rmsnorm_block(l, gname, src_tsl, dst, dst_tsl):
        ps = bank()
        for c in range(8):
            s = sq[c % 2]
            act(s[:], h[:, c, src_tsl], AF.Square)
            mm(ps[:], cmat("MEAN1024", 128, 128), s[:], c == 0, c == 7)
        r = rstd[ps_ctr[0] % 2]
        rstd_from_mean(r[:], ps[:])
        for c in range(8):
            stt("dve", dst[:, c, dst_tsl], h[:, c, src_tsl], ccol(gname, l, c), r[:], ALU.mult, ALU.mult)


    def red(eng, out, in_, op):
        return P.add(eng, lambda e: e.tensor_reduce(out, in_, AX.X, op), reads=[in_], writes=[out])

    def scan(out, d0, d1, init, op0, op1):
        return P.add("dve", lambda e: e.tensor_tensor_scan(out, d0, d1, init, op0, op1),
                     reads=[d0, d1], writes=[out])

    acc_banks = psum[4:8]

    def mixer_layer(b, l):
        nfb = small[0:4, 0:1]
        ts("dve", nfb, ccol("f_bias", l, 0, 4), -1.0, None, ALU.mult)
        negM = small[:, 1:2]
        gq = small[:, 2:3]
        gk = small[:, 3:4]
        lst = contextlib.ExitStack()
        tg = sbt(lst, "tg", [128, 64], F32)
        for nm, dstc in (("g_qn_row", gq), ("g_kn_row", gk)):
            o = CL.off[(nm, l)]
            row = cst[:, o:o + 64]
            ts("dve", tg[:], row, -1.0, None, ALU.mult)
            tt("dve", tg[:], tg[:], row, ALU.max)
            red("dve", dstc, tg[:], ALU.max)
        stt("dve", negM, gq, -8.0, gk, ALU.mult, ALU.mult)
        lst.close()
        memset("dve", Cst[:], 0.0)
        memset("pool", Cbf[:], 0.0)
        memset("pool", halo[:], 0.0)

        for j in range(NTB):
            tsl = slice(j * TB, (j + 1) * TB)
            blk = contextlib.ExitStack()
            xnb = sbt(blk, "xnb", [128, 8, TB], BF16)
            rmsnorm_block(l, "g_mix", tsl, xnb, slice(0, TB))
            if "attn" in stages:
                attn_block(b, l, j, tsl, xnb)
                P.barrier()
            if "mlstm" in stages:
                mlstm_block(b, l, j, tsl, xnb)
            blk.close()
            P.barrier()

    def head_fm(hx, ps_in, gcol, dst, cs32):
        xs, xb, t1, rs, sqh = hx
        cos32, sin32 = cs32
        cp("act", xs[:], ps_in)
        if gcol is not None:
            act(sqh[:], ps_in, AF.Square)
            psm = bank()
            mm(psm[0:64, :], cmat("MEAN64", 64, 64), sqh[:])
            rstd_from_mean(rs[:], psm[0:64, :])
            stt("dve", xs[:], xs[:], gcol, rs[:], ALU.mult, ALU.mult)
        cp("act", xb[:], xs[:])
        psr = bank()
        mm(psr[0:64, :], cmat("RT", 64, 64), xb[:])
        tt("dve", t1[:], xs[:], cos32, ALU.mult)
        tt("dve", rs[:], psr[0:64, :], sin32, ALU.mult)
        tt("dve", dst, t1[:], rs[:], ALU.add)

    def attn_block(b, l, j, tsl, xnb):
        ph = contextlib.ExitStack()
        IQb = sbt(ph, "QIb", [64, 8, TB], BF16)
        QAb = IQb
        acc = sbt(ph, "acc", [128, S], F32)
        tmp = [sbt(ph, "tmp%d" % i, [128, TB], F32) for i in range(2)]
        maskb = sbt(ph, "maskb", [128, S], BF16)
        maskT = sbt(ph, "maskT", [128, 16, TB], BF16)
        pT = [sbt(ph, "pT%d" % i, [128, TB], BF16) for i in range(2)]
        hx = (sbt(ph, "xs", [64, TB], F32), sbt(ph, "xb", [64, TB], BF16), sbt(ph, "t1", [64, TB], F32),
              sbt(ph, "rs", [64, TB], F32), sbt(ph, "sqh", [64, TB], BF16))
        cqf = acc[:, 0:2 * TB].rearrange("p (c n) -> p c n", c=2)
        cqn = sbt(ph, "cqn", [128, 2, TB], BF16)
        wabs = sbt(ph, "wabs", [128, 4, 8], F32)
        wsgn = sbt(ph, "wsgn", [128, 4, 8], F32)
        bs = sbt(ph, "bs", [128, 8 + N_BISECT], F32)
        rc = tmp[0][0:64, :]
        cs32 = (tmp[0][0:64, :], tmp[1][0:64, :])

        def load_cs32():
            cp("act", cs32[0], cosF[:, tsl])
            cp("act", cs32[1], sinF[:, tsl])

        load_cs32()

        b1 = wbuf()
        wcq = b1[:, 0:2048].rearrange("p (c n) -> p c n", c=8)
        wqu = b1[:, 2048:3072].rearrange("p (c n) -> p c n", c=2)
        wiq = b1[:, 3072:4096].rearrange("p (c n) -> p c n", c=2)
        wload(wcq, w_in_d[l, :, 0:256].rearrange("(c p) n -> p c n", p=128))
        wload(wqu, w_qup_d[l].rearrange("(c p) n -> p c n", p=128))
        wload(wiq, w_iqup_d[l].rearrange("(c p) n -> p c n", p=128))
        b2 = wbuf()
        wgk = b2[:, 0:8 * 328].rearrange("p (c n) -> p c n", c=8)
        wload(wgk, w_in_d[l, :, 256:584].rearrange("(c p) n -> p c n", p=128))

        psm = bank()
        for cc in range(2):
            ps = bank()
            for kc in range(8):
                mm(ps[:], wcq[:, kc, cc * 128:(cc + 1) * 128], xnb[:, kc, :], kc == 0, kc == 7)
            cp("act", cqf[:, cc, :], ps[:])
            s = sq[cc % 2]
            act(s[:], ps[:], AF.Square)
            mm(psm[:], cmat("MEAN256", 128, 128), s[:], cc == 0, cc == 1)
        r = rstd[0]
        rstd_from_mean(r[:], psm[:])
        for cc in range(2):
            stt("dve", cqn[:, cc, :], cqf[:, cc, :], ccol("g_cq", l, cc), r[:], ALU.mult, ALU.mult)

        for g in range(2):
            ps = bank()
            for kc in range(8):
                mm(ps[0:64, :], wgk[:, kc, g * 64:(g + 1) * 64], xnb[:, kc, :], kc == 0, kc == 7)
            head_fm(hx, ps[0:64, :], ccol("g_kn", l, 0, 64), KA[:, g, tsl], cs32)
        ps = bank()
        for kc in range(8):
            mm(ps[0:64, :], wgk[:, kc, 256:320], xnb[:, kc, :], kc == 0, kc == 7)
        head_fm(hx, ps[0:64, :], ccol("g_ik", l, 0, 64), IK[:, tsl], cs32)
        for t in range(4):
            ps = bank()
            for kc in range(8):
                mm(ps[:, 0:128], xnb[:, kc, t * 128:(t + 1) * 128], wgk[:, kc, 128:256], kc == 0, kc == 7)
            cp("act", V[:, 4 * j + t, :], ps[:, 0:128])
            ps = bank()
            for kc in range(8):
                mm(ps[:, 0:8], xnb[:, kc, t * 128:(t + 1) * 128], wgk[:, kc, 320:328], kc == 0, kc == 7)
            act(wabs[:, t, :], ps[:, 0:8], AF.Abs, scale=IDX_SCALE)
            act(wsgn[:, t, :], ps[:, 0:8], AF.Sign)
        for hd in range(8):
            ps = bank()
            for kc in range(2):
                mm(ps[0:64, :], wiq[:, kc, hd * 64:(hd + 1) * 64], cqn[:, kc, :], kc == 0, kc == 1)
            head_fm(hx, ps[0:64, :], None, IQb[:, hd, :], cs32)
        if b == 0 and l == 0 and j == 0:
            dump("KA", KA[:, :, 0:TB])
            dump("IK", IK[:, 0:TB])
            dump("IQb", IQb[:])
            dump("V", V[:, 0:4, :])
            dump("wabs", wabs[:])
            dump("wsgn", wsgn[:])

        lo, mid, cnt, tcol, w0, hi, thr = [bs[:, i:i + 1] for i in range(7)]
        Wb = bs[:, 8:8 + N_BISECT]
        for t in range(4):
            qi = 4 * j + t
            L = 128 * (qi + 1)
            qsl = slice(t * 128, (t + 1) * 128)
            for c in range(j + 1):
                ncols = min(512, L - 512 * c)
                cs = slice(512 * c, 512 * c + ncols)
                for hd in range(8):
                    ps = bank()
                    mm(ps[:, 0:ncols], IQb[:, hd, qsl], IK[:, cs])
                    tm = tmp[hd % 2]
                    act(tm[:, 0:ncols], ps[:, 0:ncols], AF.Relu, scale=wabs[:, t, hd:hd + 1])
                    eng = "dve"
                    if hd == 0:
                        ts(eng, acc[:, cs], tm[:, 0:ncols], wsgn[:, t, hd:hd + 1], None, ALU.mult)
                    elif eng == "dve":
                        stt(eng, acc[:, cs], tm[:, 0:ncols], wsgn[:, t, hd:hd + 1], acc[:, cs], ALU.mult, ALU.add)
                    else:
                        ts("pool", tm[:, 0:ncols], tm[:, 0:ncols], wsgn[:, t, hd:hd + 1], None, ALU.mult)
                        tt("pool", acc[:, cs], acc[:, cs], tm[:, 0:ncols], ALU.add)
            if qi >= 2:
                red("dve", lo, acc[:, 0:L], ALU.min)
                red("dve", hi, acc[:, 0:L], ALU.max)
            tt("dve", acc[:, qi * 128:(qi + 1) * 128], acc[:, qi * 128:(qi + 1) * 128],
               cf[:, CF["CAUS"]:CF["CAUS"] + 128], ALU.add)
            if b == 0 and l == 0 and j == 0 and t == 3:
                dump("acc", acc[:, 0:512])
            if qi < 2:
                memset("dve", lo, -1.0e29)
            else:
                tt("dve", w0, hi, lo, ALU.subtract)
                ts("dve", w0, w0, 1.0001, 1e-12, ALU.mult, ALU.add)
                ts("dve", Wb, cf[:, CF["POW2"]:CF["POW2"] + N_BISECT], w0, None, ALU.mult)
                for k in range(N_BISECT):
                    tt("dve", mid, lo, Wb[:, k:k + 1], ALU.add)
                    ts("dve", maskb[:, 0:L], acc[:, 0:L], mid, None, ALU.is_ge, ALU.add, accum=cnt)
                    stt("dve", tcol, cnt, 255.5, Wb[:, k:k + 1], ALU.is_ge, ALU.mult)
                    tt("dve", lo, lo, tcol, ALU.add)
            ts("dve", maskb[:, 0:L], acc[:, 0:L], lo, None, ALU.is_ge)
            if b == 0 and l == 0 and j == 0 and t == 3:
                dump("maskb", maskb[:, 0:512])
                dump("bs", bs[:])
            for k4 in range(0, qi + 1, 4):
                n = min(4, qi + 1 - k4)
                ps = bank()
                for kk in range(n):
                    mm(ps[:, kk * 128:(kk + 1) * 128], maskb[:, (k4 + kk) * 128:(k4 + kk + 1) * 128],
                       cmat("IDENT", 128, 128))
                cp("act", maskT[:, k4:k4 + n, qsl], ps[:, 0:n * 128].rearrange("p (a q) -> p a q", a=n))
            for kt in range(qi + 1, 4 * (j + 1)):
                memset("pool", maskT[:, kt, qsl], 0.0)

        load_cs32()
        for hd in range(8):
            ps = bank()
            for kc in range(2):
                mm(ps[0:64, :], wqu[:, kc, hd * 64:(hd + 1) * 64], cqn[:, kc, :], kc == 0, kc == 1)
            head_fm(hx, ps[0:64, :], ccol("g_qn", l, 0, 64), QAb[:, hd, :], cs32)
        if b == 0 and l == 0 and j == 0:
            dump("QAb", QAb[:])
        nkt = 4 * (j + 1)
        for hd in range(8):
            g = hd // 4
            psn = acc_banks[(hd % 2) * 2]
            psd = acc_banks[(hd % 2) * 2 + 1]
            for kt in range(nkt):
                ps = bank()
                mm(ps[:], KA[:, g, kt * 128:(kt + 1) * 128], QAb[:, hd, :])
                pt = pT[kt % 2]
                act(pt[:], ps[:], AF.Exp, bias=small[:, 1:2], scale=0.125)
                tt("dve", pt[:], pt[:], maskT[:, kt, :], ALU.mult)
                mm(psn[0:64, :], V[:, kt, g * 64:(g + 1) * 64], pt[:], kt == 0, kt == nkt - 1)
                mm(psd[0:64, :], cmat("ONES", 128, 64), pt[:], kt == 0, kt == nkt - 1)
            recip_act(rc, psd[0:64, :])
            tt("dve", mixA[:, hd, :], psn[0:64, :], rc, ALU.mult)
        if b == 0 and l == 0 and j == 0:
            dump("mixA", mixA[:])
        wo = []
        for half in range(2):
            bb = wbuf()
            v = bb[0:64, :].rearrange("p (c n) -> p c n", c=4)
            wload(v, w_out_d[l, half * 256:(half + 1) * 256, :].rearrange("(c p) n -> p c n", p=64))
            wo.append(v)
        for dc in range(8):
            ps = bank()
            for hd in range(8):
                mm(ps[:], wo[hd // 4][:, hd % 4, dc * 128:(dc + 1) * 128], mixA[:, hd, :], hd == 0, hd == 7)
            tt("dve", h[:, dc, tsl], h[:, dc, tsl], ps[:], ALU.add)
        ph.close()

    def mlstm_block(b, l, j, tsl, xnb):
        ph = contextlib.ExitStack()
        lic = sbt(ph, "lic", [4, TB], F32)
        l1 = sbt(ph, "l1", [4, TB], F32)
        nbt = sbt(ph, "nbt", [4, TB], F32)
        aa = lic
        ag = l1
        eaeg = sbt(ph, "eaeg", [64, 8, 8], F32)
        EB = [sbt(ph, "EB%d" % i, [128, TB], BF16) for i in range(4)]
        EBL = sbt(ph, "EBL", [128, 4, 8], F32)
        xc = [sbt(ph, "xc%d" % i, [128, 4 + TB], F32) for i in range(2)]
        cacc = [sbt(ph, "cacc%d" % i, [128, TB], F32) for i in range(2)]
        qf = sbt(ph, "qf", [128, 4, TB], BF16)
        kf = sbt(ph, "kf", [128, 4, TB], BF16)
        kg = sbt(ph, "kg", [64, 8, 4, 128], BF16)
        vt = sbt(ph, "vt", [64, 8, 512], BF16)
        og = sbt(ph, "og", [128, 4, TB], BF16)
        Ssb = [sbt(ph, "Ssb%d" % i, [64, 64], BF16) for i in range(4)]
        hm = cacc[1]
        dn = cacc[0]

        wload(wS[:], w_in_d[l, :, 2632:2640].rearrange("(c p) n -> p c n", p=128))
        ps_i = bank()
        for kc in range(8):
            mm(ps_i[0:4, :], wS[:, kc, 0:4], xnb[:, kc, :], kc == 0, kc == 7)
        ps_f = bank()
        for kc in range(8):
            mm(ps_f[0:4, :], wS[:, kc, 4:8], xnb[:, kc, :], kc == 0, kc == 7)
        ts("dve", lic[:], ps_i[0:4, :], ccol("i_bias", l, 0, 4), None, ALU.add)
        act(l1[:], ps_f[0:4, :], AF.Exp, bias=small[0:4, 0:1], scale=-1.0)
        act(l1[:], l1[:], AF.Ln, bias=1.0)
        scan(nbt[:], cf[0:4, CF["RESET"]:CF["RESET"] + TB], l1[:], 0.0, ALU.mult, ALU.add)
        tt("dve", aa[:], lic[:], nbt[:], ALU.add)
        nb3 = nbt[:, :].rearrange("p (c s) -> p c s", c=8)
        for c in range(8):
            ts("dve", ag[:, c * 64:(c + 1) * 64], aa[:, c * 64:(c + 1) * 64], nbt[:, c * 64 + 63:c * 64 + 64], None,
               ALU.subtract)
        if KSTOP <= 1:
            ph.close()
            return
        hl = {}
        for nm, src in (("aa", aa), ("ag", ag), ("nb", nbt)):
            hi = sbt(ph, nm + "hi", [4, TB], BF16)
            lo_ = sbt(ph, nm + "lo", [4, TB], BF16)
            hf = xc[0][0:4, 0:TB]
            cp("dve", hi[:], src[:])
            cp("dve", hf, hi[:])
            tt("dve", hf, src[:], hf, ALU.subtract)
            cp("dve", lo_[:], hf)
            hl[nm] = (hi, lo_)
        i4 = cm[0:4, CM["I4"]:CM["I4"] + 4]
        psg = bank()
        for c in range(8):
            for q_, nm in ((0, "aa"), (4, "ag")):
                hi, lo_ = hl[nm]
                mm(psg[0:64, c * 8 + q_:c * 8 + q_ + 4], hi[:, c * 64:(c + 1) * 64], i4, True, False)
                mm(psg[0:64, c * 8 + q_:c * 8 + q_ + 4], lo_[:, c * 64:(c + 1) * 64], i4, False, True)
        act(eaeg[:, :, :].rearrange("p c n -> p (c n)"), psg[0:64, 0:64], AF.Exp)
        for hd in range(4):
            ps = bank()
            sel = cm[0:4, CM["SEL"] + hd * 128:CM["SEL"] + (hd + 1) * 128]
            mm(ps[:], sel, hl["nb"][0][:], True, False)
            mm(ps[:], sel, hl["nb"][1][:], False, True)
            act(EB[hd][:], ps[:], AF.Exp, scale=-1.0)
            act(EBL[:, hd, :], ps[:, 63:512:64], AF.Exp, scale=-1.0)
        if b == 0 and l == 0 and j == 0:
            dump("lic", lic[:])
            dump("nbt", nbt[:])
            dump("eaeg", eaeg[:])
            dump("EB0", EB[0][:])

        if KSTOP <= 2:
            ph.close()
            return
        for which in range(2):
            wbb = wbuf()
            w = wbb[:, :].rearrange("p (c n) -> p c n", c=8)
            c0 = 584 + which * 512
            wload(w, w_in_d[l, :, c0:c0 + 512].rearrange("(c p) n -> p c n", p=128))
            for cc in range(4):
                ch = which * 4 + cc
                ps = bank()
                for kc in range(8):
                    mm(ps[:], w[:, kc, cc * 128:(cc + 1) * 128], xnb[:, kc, :], kc == 0, kc == 7)
                x_ = xc[cc % 2]
                ca = cacc[cc % 2]
                cp("pool", x_[:, 0:4], halo[:, ch, :])
                cp("act", x_[:, 4:4 + TB], ps[:])
                if KSUB <= 1:
                    continue
                cw = CL.off[("conv_w", l)]
                act(ca[:], x_[:, 1:1 + TB], AF.Identity, bias=ccol("conv_b", l, ch), scale=cst[:, cw + ch:cw + ch + 1])
                for jt in range(1, 4):
                    stt("dve", ca[:], x_[:, 1 + jt:1 + jt + TB],
                        cst[:, cw + jt * 8 + ch:cw + jt * 8 + ch + 1], ca[:], ALU.mult, ALU.add)
                if KSUB <= 2:
                    continue
                cp("pool", halo[:, ch, :], x_[:, TB:TB + 4])
                if KSUB <= 3:
                    continue
                sg = x_[:, 4:4 + TB]
                act(sg, ca[:], AF.Sigmoid)
                if which == 0:
                    tt("dve", ca[:], ca[:], sg, ALU.mult)
                    ts("dve", qf[:, cc, :], ca[:], float(128 ** -0.5), None, ALU.mult)
                    tt("dve", qf[:, cc, :], qf[:, cc, :], EB[cc][:], ALU.mult)
                else:
                    tt("dve", kf[:, cc, :], ca[:], sg, ALU.mult)
        if KSTOP <= 3:
            ph.close()
            return
        wbb = wbuf()
        wv = wbb[:, :].rearrange("p (c n) -> p c n", c=8)
        wload(wv, w_in_d[l, :, 1608:2120].rearrange("(c p) n -> p c n", p=128))
        for c in range(8):
            ps = bank()
            for kc in range(8):
                mm(ps[0:64, :], xnb[:, kc, c * 64:(c + 1) * 64], wv[:, kc, :], kc == 0, kc == 7)
            cp("act" if c % 2 == 0 else "dve", vt[:, c, :], ps[0:64, :])
        if KSUB2 <= 1:
            ph.close()
            return
        for c in range(8):
            ps = bank()
            for hd in range(4):
                mm(ps[0:64, hd * 128:(hd + 1) * 128], kf[:, hd, c * 64:(c + 1) * 64], cmat("IDENT", 128, 128))
            for hd in range(4):
                ts("dve", kg[:, c, hd, :], ps[0:64, hd * 128:(hd + 1) * 128], eaeg[:, c, 4 + hd:5 + hd], None, ALU.mult)
        if KSUB2 <= 2:
            ph.close()
            return
        wbb = wbuf()
        w = wbb[:, :].rearrange("p (c n) -> p c n", c=8)
        wload(w, w_in_d[l, :, 2120:2632].rearrange("(c p) n -> p c n", p=128))
        for cc in range(4):
            ps = bank()
            for kc in range(8):
                mm(ps[:], w[:, kc, cc * 128:(cc + 1) * 128], xnb[:, kc, :], kc == 0, kc == 7)
            act(og[:, cc, :], ps[:], AF.Sigmoid)
        if b == 0 and l == 0 and j == 0:
            dump("qf", qf[:])
            dump("kf", kf[:])
            dump("vt", vt[:])
            dump("kg", kg[:])
            dump("og", og[:])

        if KSTOP <= 4:
            ph.close()
            return
        for pair in range(2):
            for c in range(8):
                csl = slice(c * 64, (c + 1) * 64)
                for hh in range(2):
                    hd = pair * 2 + hh
                    psN = acc_banks[hh * 2]
                    psDn = acc_banks[hh * 2 + 1]
                    psS = bank()
                    mm(psS[0:64, 0:64], kf[:, hd, csl], qf[:, hd, csl])
                    Sb = Ssb[(c * 2 + hh) % 4]
                    stt("dve", Sb[:], psS[0:64, 0:64], eaeg[:, c, hd:hd + 1], cf[0:64, CF["TRIU"]:CF["TRIU"] + 64], ALU.mult, ALU.mult)
                    mm(psN[:, csl], Cbf[:, hd, 0:128], qf[:, hd, csl], True, False)
                    mm(psN[:, csl], vt[:, c, hd * 128:(hd + 1) * 128], Sb[:], False, True)
                    mm(psDn[:, csl], Cbf[:, hd, 128:256], qf[:, hd, csl], True, False)
                    mm(psDn[:, csl], cmat("ONES", 64, 128), Sb[:], False, True)
                    psD = bank()
                    mm(psD[:, 0:128], kg[:, c, hd, :], vt[:, c, hd * 128:(hd + 1) * 128])
                    mm(psD[:, 128:256], kg[:, c, hd, :], cmat("ONES", 64, 128))
                    stt("dve", Cst[:, hd, :], Cst[:, hd, :], EBL[:, hd, c:c + 1], psD[:, 0:256],
                        ALU.mult, ALU.add)
                    cp("act", Cbf[:, hd, :], Cst[:, hd, :])
            for hh in range(2):
                hd = pair * 2 + hh
                psN = acc_banks[hh * 2]
                psDn = acc_banks[hh * 2 + 1]
                act(dn[:], psDn[:], AF.Abs)
                ts("dve", dn[:], dn[:], 1.0, None, ALU.max)
                recip_act(dn[:], dn[:])
                tt("dve", hm[:], psN[:], dn[:], ALU.mult)
                if b == 0 and l == 0 and j == 0 and hd == 0:
                    dump("hm0", hm[:])
                s = sq[hh]
                act(s[:], hm[:], AF.Square)
                psm = bank()
                mm(psm[:], cmat("MEAN128", 128, 128), s[:])
                r = rstd[hh]
                rstd_from_mean(r[:], psm[:])
                g_o = CL.off[("g_mh", l)] + hd
                hmb = sq[hh]
                stt("dve", hmb[:], hm[:], cst[:, g_o:g_o + 1], r[:], ALU.mult, ALU.mult)
                tt("dve", mixM[:, hd, :], hmb[:], og[:, hd, :], ALU.mult)
        if b == 0 and l == 0 and j == 0:
            dump("mixM", mixM[:])
        wbb = wbuf()
        wo = wbb[:, :].rearrange("p (c n) -> p c n", c=4)
        wload(wo, w_out_d[l, 512:1024, :].rearrange("(c p) n -> p c n", p=128))
        for dc in range(8):
            ps = bank()
            for kc in range(4):
                mm(ps[:], wo[:, kc, dc * 128:(dc + 1) * 128], mixM[:, kc, :], kc == 0, kc == 3)
            tt("dve", h[:, dc, tsl], h[:, dc, tsl], ps[:], ALU.add)
        ph.close()

    final_ops = []

    for b in range(nb):
        for c in range(8):
            dma("sp", h[:, c, :], x_d[b, c * 128:(c + 1) * 128, :], wr=h[:, c, :])
        if "mix" in stages:
            ph = contextlib.ExitStack()
            posi = sbt(ph, "posi", [64, S], I32)
            ang = sbt(ph, "ang", [64, S], F32)
            kf = sbt(ph, "kf", [64, S], F32)
            ki = sbt(ph, "ki", [64, S], I32)
            msk = sbt(ph, "msk", [64, S], F32)
            dma("sp", posi[:], pos_d[b], wr=posi[:])
            cp("dve", ang[:], posi[:])
            ts("dve", ang[:], ang[:], cf[0:64, CF["INV"]:CF["INV"] + 1], None, ALU.mult)
            for (dstT, shift) in ((sinF, 0.0), (cosF, np.pi / 2)):
                ts("dve", kf[:], ang[:], shift, 1.0 / TWO_PI, ALU.add, ALU.mult)
                cp("dve", ki[:], kf[:])
                cp("dve", kf[:], ki[:])
                ts("dve", msk[:], ang[:], shift, None, ALU.add)
                stt("dve", kf[:], kf[:], -TWO_PI, msk[:], ALU.mult, ALU.add)
                ts("dve", msk[:], kf[:], float(np.pi), None, ALU.is_gt)
                stt("dve", kf[:], msk[:], -TWO_PI, kf[:], ALU.mult, ALU.add)
                ts("dve", msk[:], kf[:], float(-np.pi), None, ALU.is_lt)
                stt("dve", kf[:], msk[:], TWO_PI, kf[:], ALU.mult, ALU.add)
                ts("dve", kf[:], kf[:], float(np.pi), float(-np.pi), ALU.min, ALU.max)
                act(dstT[:], kf[:], AF.Sin)
            dump("cosF", cosF[:])
            dump("sinF", sinF[:])
            ph.close()
            P.barrier()

        for l in range(nl):
            if "mix" in stages:
                mixer_layer(b, l)
            ph = contextlib.ExitStack()
            xn = sbt(ph, "xn", [128, 8, S], BF16)
            ubuf = sbt(ph, "ubuf", [128, 4, S], BF16)
            relu_t = [sbt(ph, "relu%d" % i, [128, TB], F32) for i in range(2)]
            if "ffn" in stages:
                for tb in range(NTB):
                    tsl = slice(tb * TB, (tb + 1) * TB)
                    rmsnorm_block(l, "g_mlp", tsl, xn, tsl)
                for j in range(8):
                    w1b = wbuf()
                    w2b = wbuf()
                    w1 = w1b[:, :].rearrange("p (c n) -> p c n", c=8)
                    w2 = w2b[:, :].rearrange("p (c n) -> p c n", c=4)
                    wload(w1, w_ff1_d[l, :, j * 512:(j + 1) * 512].rearrange("(c p) n -> p c n", p=128))
                    wload(w2, w_ff2_d[l, j * 512:(j + 1) * 512, :].rearrange("(c p) n -> p c n", p=128))
                    for tb in range(NTB):
                        tsl = slice(tb * TB, (tb + 1) * TB)
                        for fc in range(4):
                            ps = bank()
                            for kc in range(8):
                                mm(ps[:], w1[:, kc, fc * 128:(fc + 1) * 128], xn[:, kc, tsl], kc == 0, kc == 7)
                            rl = relu_t[fc % 2]
                            act(rl[:], ps[:], AF.Relu)
                            tt("dve", ubuf[:, fc, tsl], rl[:], rl[:], ALU.mult)
                    for tb in range(NTB):
                        tsl = slice(tb * TB, (tb + 1) * TB)
                        for dc in range(8):
                            ps = bank()
                            for fc in range(4):
                                mm(ps[:], w2[:, fc, dc * 128:(dc + 1) * 128], ubuf[:, fc, tsl], fc == 0, fc == 3)
                            tt("dve", h[:, dc, tsl], h[:, dc, tsl], ps[:], ALU.add)
            if "ple" in stages:
                for tb in range(NTB):
                    tsl = slice(tb * TB, (tb + 1) * TB)
                    rmsnorm_block(l, "g_ple", tsl, xn, tsl)
                pb = ubuf
                for kc in range(2):
                    wload(pb[:, kc, :], p_d[l, b, kc * 128:(kc + 1) * 128, :])
                for half in range(2):
                    wgb = wbuf()
                    wpb = wbuf()
                    wg = wgb[:, :].rearrange("p (c n) -> p c n", c=8)
                    wp = wpb[:, 0:1024].rearrange("p (c n) -> p c n", c=2)
                    wload(wg, w_pg_d[l, :, half * 512:(half + 1) * 512].rearrange("(c p) n -> p c n", p=128))
                    wload(wp, w_pl_d[l, :, half * 512:(half + 1) * 512].rearrange("(c p) n -> p c n", p=128))
                    for tb in range(NTB):
                        tsl = slice(tb * TB, (tb + 1) * TB)
                        for dcl in range(4):
                            dc = half * 4 + dcl
                            ps = bank()
                            for kc in range(8):
                                mm(ps[:], wg[:, kc, dcl * 128:(dcl + 1) * 128], xn[:, kc, tsl], kc == 0, kc == 7)
                            g = relu_t[dcl % 2]
                            act(g[:], ps[:], AF.Sigmoid, bias=ccol("b_ple", l, dc))
                            ps2 = bank()
                            for kc in range(2):
                                mm(ps2[:], wp[:, kc, dcl * 128:(dcl + 1) * 128], pb[:, kc, tsl], kc == 0, kc == 1)
                            tt("dve", g[:], g[:], ps2[:], ALU.mult)
                            tt("dve", h[:, dc, tsl], h[:, dc, tsl], g[:], ALU.add)
            ph.close()
            P.barrier()
        for c in range(8):
            final_ops.append(dma("sp", out_d[b, c * 128:(c + 1) * 128, :], h[:, c, :], rd=h[:, c, :]))

    final_ops += list(dumps.values())
    P.emit(final_wait_ops=final_ops)
    es.close()
    return nc


def make_in_maps(inp, nb, ncores):
    cst = build_cst(inp)
    cm = build_cm()
    cf = build_cf()
    x = inp["x"]
    p = inp["p"]
    pos = inp["positions"].astype(np.int32)
    in_maps = []
    for c in range(ncores):
        bs = slice(c * nb, (c + 1) * nb)
        m = {
            "x": np.ascontiguousarray(x[bs].transpose(0, 2, 1)),
            "p": np.ascontiguousarray(p[:, bs].transpose(0, 1, 3, 2)),
            "pos": np.ascontiguousarray(np.broadcast_to(pos[bs][:, None, :], (nb, 64, S))),
            "cst": cst, "cm": cm, "cf": cf,
        }
        for k in ("w_in", "w_q_up", "w_iq_up", "w_out", "w_ff1", "w_ff2", "w_ple_gate", "w_ple"):
            m[k] = np.ascontiguousarray(inp[k], dtype=np.float32)
        in_maps.append(m)
    return in_maps


def kernel(**inputs):
    inp = {k: np.asarray(v) for k, v in inputs.items()}
    nb = 2
    nc = build_nc(nb=nb, stages=RUN_STAGES)
    in_maps = make_in_maps(inp, nb, NCORES)
    res = run_bass_kernel_spmd(nc, in_maps, core_ids=list(range(NCORES)))
    out = np.concatenate([r["out"] for r in res.results], axis=0)
    return np.ascontiguousarray(out.transpose(0, 2, 1)).astype(np.float32)
```

```python
import contextlib
import os
import numpy as np
import concourse.bass as bass
import concourse.mybir as mybir
from concourse.bass_utils import run_bass_kernel_spmd

F32 = mybir.dt.float32
BF16 = mybir.dt.bfloat16
I32 = mybir.dt.int32
ALU = mybir.AluOpType
AF = mybir.ActivationFunctionType
AX = mybir.AxisListType

D = 1024
S = 2048
NL = 2
NCORES = 8
TB = 512
NTB = S // TB
EPS = 1e-6
IN_W = 2640
DFF = 4096

ENGS = ("pe", "act", "dve", "pool", "sp")
N_DMA_SEMS = 16


def _region(ap):
    if type(ap.tensor).__name__.startswith("PSum"):
        return (ap.tensor.name, 0, 128, 0, 1 << 30)
    pat = ap.ap
    row = pat[0][0]
    npart = pat[0][1]
    off = ap.offset
    if row <= 0:
        p0, f0 = 0, off
    else:
        p0 = off // row
        f0 = off - p0 * row
    ext = 1
    for st, cnt in pat[1:]:
        ext += (cnt - 1) * abs(st)
    sz = mybir.dt.size(ap.dtype)
    return (ap.tensor.name, p0, p0 + npart, f0 * sz, (f0 + ext) * sz)


def _overlap(a, b):
    return a[1] < b[2] and b[1] < a[2] and a[3] < b[4] and b[3] < a[4]


class Op:
    __slots__ = ("eng", "fn", "reads", "writes", "dma", "deps", "sig", "idx", "dmak")


class Prog:
    def __init__(self, nc):
        self.nc = nc
        self.ops = []
        self.hist = {}
        self.last_op = {}
        self.bar_id = 0
        self.bar_deps = set()
        self.eng_bar = {}
        self.dma_pending = []
        self.pending = {}

    def barrier(self):
        deps = set(self.last_op.values()) | set(self.dma_pending)
        self.dma_pending = []
        for e in ENGS:
            self.pending.setdefault(e, set()).update(deps)

    def add(self, eng, fn, reads=(), writes=(), dma=False):
        op = Op()
        op.eng, op.fn, op.dma = eng, fn, dma
        op.reads = [_region(a) for a in reads if a is not None]
        op.writes = [_region(a) for a in writes if a is not None]
        op.deps = set()
        op.sig = None
        op.dmak = None
        op.idx = len(self.ops)
        self.ops.append(op)
        ops = self.ops
        for r in op.reads:
            for (reg, oi, isw) in self.hist.setdefault(r[0], []):
                if isw and _overlap(reg, r):
                    op.deps.add(oi)
        for w in op.writes:
            keep = []
            for rec in self.hist.setdefault(w[0], []):
                reg, oi, isw = rec
                if _overlap(reg, w):
                    op.deps.add(oi)
                    if w[1] <= reg[1] and reg[2] <= w[2] and w[3] <= reg[3] and reg[4] <= w[4]:
                        continue
                keep.append(rec)
            self.hist[w[0]] = keep
        for r in op.reads:
            h = self.hist[r[0]]
            if not dma:
                h[:] = [rec for rec in h if not ((not rec[2]) and rec[0] == r
                                                 and ops[rec[1]].eng == eng and not ops[rec[1]].dma)]
            h.append((r, op.idx, False))
        for w in op.writes:
            self.hist[w[0]].append((w, op.idx, True))
        if self.pending.get(eng):
            op.deps |= self.pending[eng]
            self.pending[eng] = set()
        if not dma:
            self.last_op[eng] = op.idx
        else:
            self.dma_pending.append(op.idx)
        op.deps.discard(op.idx)
        return op

    def emit(self, final_wait_ops=()):
        nc = self.nc
        ops = self.ops
        for op in ops:
            if op.eng == "pe" and not op.dma:
                op.deps = {d for d in op.deps if not (ops[d].eng == "pe" and not ops[d].dma)}
        needed = set()
        for op in ops:
            needed |= op.deps
        for o in final_wait_ops:
            needed.add(o.idx)
        cnt = {e: 0 for e in ENGS}
        dmak = {"sp": 0, "pool": 0, "act": 0}
        dma_ops = {"sp": [], "pool": [], "act": []}
        for op in ops:
            if op.dma:
                k = dmak[op.eng]
                op.dmak = k
                op.sig = ("dma", (op.eng, k % N_DMA_SEMS), 16 * (k // N_DMA_SEMS + 1))
                dmak[op.eng] += 1
                dma_ops[op.eng].append(op)
            elif op.idx in needed:
                cnt[op.eng] += 1
                op.sig = (op.eng, cnt[op.eng])
        with contextlib.ExitStack() as st:
            sems = {e: st.enter_context(nc.semaphore("s_" + e)) for e in ENGS}
            dsems = {(q, i): st.enter_context(nc.semaphore("d_%s_%d" % (q, i)))
                     for q in ("sp", "pool") for i in range(N_DMA_SEMS)}
            block = st.enter_context(nc.Block())
            per_eng = {e: [op for op in ops if op.eng == e] for e in ENGS}

            def run(engname, eng):
                known = {}
                for op in per_eng[engname]:
                    waits = {}
                    deps = set(op.deps)
                    if op.dma and op.dmak >= N_DMA_SEMS:
                        deps.add(dma_ops[op.eng][op.dmak - N_DMA_SEMS].idx)
                    for d in deps:
                        s = ops[d].sig
                        if s[0] == "dma":
                            key, v = ("dma", s[1]), s[2]
                        else:
                            key, v = ("c", s[0]), s[1]
                        if known.get(key, 0) >= v:
                            continue
                        if waits.get(key, 0) < v:
                            waits[key] = v
                    for key, v in waits.items():
                        sem = dsems[key[1]] if key[0] == "dma" else sems[key[1]]
                        eng.wait_ge(sem, v)
                        known[key] = v
                    ins = op.fn(eng)
                    if op.sig is not None:
                        if op.sig[0] == "dma":
                            ins.then_inc(dsems[op.sig[1]], 16)
                        else:
                            ins.then_inc(sems[op.sig[0]], 1)
                if engname == "sp":
                    for o in final_wait_ops:
                        s = o.sig
                        if s[0] == "dma":
                            eng.wait_ge(dsems[s[1]], s[2])
                        else:
                            eng.wait_ge(sems[s[0]], s[1])

            @block.tensor
            def _(e):
                run("pe", e)

            @block.scalar
            def _(e):
                run("act", e)

            @block.vector
            def _(e):
                run("dve", e)

            @block.gpsimd
            def _(e):
                run("pool", e)

            @block.sync
            def _(e):
                run("sp", e)


IDX_SCALE = (8 ** -0.5) * (64 ** -0.5)
KSTOP = int(os.environ.get('K_STOP', '99'))
RUN_STAGES = ("mix", "attn", "mlstm", "ffn", "ple")
KSUB = int(os.environ.get('K_SUB', '99'))
KSUB2 = int(os.environ.get('K_SUB2', '99'))
NEG_BIG = -1.0e30
N_BISECT = 24
TWO_PI = 2.0 * np.pi


def _fm_cols(v, p=128):
    v = np.asarray(v, np.float32)
    return np.ascontiguousarray(v.reshape(-1, p).T)


class CstLayout:
    def __init__(self):
        self.off = {}
        self.n = 0

    def add(self, name, ncols):
        self.off[name] = self.n
        self.n += ncols


def make_cst_layout():
    L = CstLayout()
    for l in range(NL):
        for nm in ("g_mix", "g_mlp", "g_ple", "b_ple", "conv_b"):
            L.add((nm, l), 8)
        L.add(("g_cq", l), 2)
        for nm in ("g_qn", "g_kn", "g_ik", "i_bias", "f_bias"):
            L.add((nm, l), 1)
        L.add(("conv_w", l), 32)
        L.add(("g_mh", l), 4)
        L.add(("g_qn_row", l), 64)
        L.add(("g_kn_row", l), 64)
    return L


CL = make_cst_layout()


def build_cst(inp):
    c = np.zeros((128, CL.n), np.float32)
    for l in range(NL):
        for nm, key in (("g_mix", "g_mix"), ("g_mlp", "g_mlp"), ("g_ple", "g_ple"),
                        ("b_ple", "b_ple_gate"), ("conv_b", "conv_b")):
            o = CL.off[(nm, l)]
            c[:, o:o + 8] = _fm_cols(inp[key][l])
        o = CL.off[("g_cq", l)]
        c[:, o:o + 2] = _fm_cols(inp["g_cq"][l])
        for nm in ("g_qn", "g_kn", "g_ik"):
            o = CL.off[(nm, l)]
            c[0:64, o] = inp[nm][l]
        for nm in ("i_bias", "f_bias"):
            o = CL.off[(nm, l)]
            c[0:4, o] = inp[nm][l]
        o = CL.off[("conv_w", l)]
        for j in range(4):
            c[:, o + j * 8:o + j * 8 + 8] = _fm_cols(inp["conv_w"][l, j])
        o = CL.off[("g_mh", l)]
        c[:, o:o + 4] = np.ascontiguousarray(inp["g_mh"][l].T)
        o = CL.off[("g_qn_row", l)]
        c[:, o:o + 64] = np.broadcast_to(inp["g_qn"][l][None, :], (128, 64))
        o = CL.off[("g_kn_row", l)]
        c[:, o:o + 64] = np.broadcast_to(inp["g_kn"][l][None, :], (128, 64))
    return c


CM = {}
_o = 0
for _nm, _w in (("MEAN1024", 128), ("MEAN256", 128), ("MEAN128", 128), ("MEAN64", 64), ("ONES", 128),
                ("IDENT", 128), ("RT", 64), ("TRIU", 64), ("I4", 4), ("SEL", 512)):
    CM[_nm] = _o
    _o += _w
CM_N = _o

CF = {}
_o = 0
for _nm, _w in (("I4", 4), ("SEL", 512), ("RESET", 512), ("INV", 1), ("CAUS", 128), ("POW2", N_BISECT), ("TRIU", 64)):
    CF[_nm] = _o
    _o += _w
CF_N = _o


def build_cm():
    import ml_dtypes
    m = np.zeros((128, CM_N), np.float32)
    m[:, CM["MEAN1024"]:CM["MEAN1024"] + 128] = 1.0 / 1024.0
    m[:, CM["MEAN256"]:CM["MEAN256"] + 128] = 1.0 / 256.0
    m[:, CM["MEAN128"]:CM["MEAN128"] + 128] = 1.0 / 128.0
    m[0:64, CM["MEAN64"]:CM["MEAN64"] + 64] = 1.0 / 64.0
    m[:, CM["ONES"]:CM["ONES"] + 128] = 1.0
    m[:, CM["IDENT"]:CM["IDENT"] + 128] = np.eye(128)
    rt = np.zeros((64, 64), np.float32)
    for i in range(8):
        rt[i + 8, i] = -1.0
        rt[i, i + 8] = 1.0
    m[0:64, CM["RT"]:CM["RT"] + 64] = rt
    m[0:64, CM["TRIU"]:CM["TRIU"] + 64] = np.triu(np.ones((64, 64)))
    m[0:4, CM["I4"]:CM["I4"] + 4] = np.eye(4)
    for hd in range(4):
        m[hd, CM["SEL"] + hd * 128:CM["SEL"] + (hd + 1) * 128] = 1.0
    return m.astype(ml_dtypes.bfloat16)


def build_cf():
    f = np.zeros((128, CF_N), np.float32)
    f[0:4, CF["I4"]:CF["I4"] + 4] = np.eye(4)
    for hd in range(4):
        f[hd, CF["SEL"] + hd * 128:CF["SEL"] + (hd + 1) * 128] = 1.0
    r = np.ones(512, np.float32)
    r[0::64] = 0.0
    f[0:4, CF["RESET"]:CF["RESET"] + 512] = r[None, :]
    inv = (500000.0 ** (-(np.arange(8, dtype=np.float32) * 2.0) / 16.0)).astype(np.float32)
    f[0:8, CF["INV"]] = inv
    f[8:16, CF["INV"]] = inv
    caus = np.where(np.arange(128)[None, :] <= np.arange(128)[:, None], 0.0, NEG_BIG)
    f[:, CF["CAUS"]:CF["CAUS"] + 128] = caus
    f[:, CF["POW2"]:CF["POW2"] + N_BISECT] = (0.5 ** np.arange(1, N_BISECT + 1))[None, :]
    f[0:64, CF["TRIU"]:CF["TRIU"] + 64] = np.triu(np.ones((64, 64)))
    return f


def build_nc(nb=2, nl=NL, dbg=(), stages=("mix", "attn", "mlstm", "ffn", "ple")):
    nc = bass.Bass("TRN2", target_bir_lowering=False)

    def din(name, shape, dt=F32):
        return nc.dram_tensor(name, shape, dt, kind="ExternalInput").ap()

    x_d = din("x", [nb, D, S])
    p_d = din("p", [NL, nb, 256, S])
    pos_d = din("pos", [nb, 64, S], I32)
    cst_d = din("cst", [128, CL.n])
    cm_d = din("cm", [128, CM_N], BF16)
    cf_d = din("cf", [128, CF_N])
    w_in_d = din("w_in", [NL, D, IN_W])
    w_qup_d = din("w_q_up", [NL, 256, 512])
    w_iqup_d = din("w_iq_up", [NL, 256, 512])
    w_out_d = din("w_out", [NL, D, D])
    w_ff1_d = din("w_ff1", [NL, D, DFF])
    w_ff2_d = din("w_ff2", [NL, DFF, D])
    w_pg_d = din("w_ple_gate", [NL, D, D])
    w_pl_d = din("w_ple", [NL, 256, D])
    out_d = nc.dram_tensor("out", [nb, D, S], F32, kind="ExternalOutput").ap()

    P = Prog(nc)
    es = contextlib.ExitStack()
    uid = [0]

    def sbt(stack, name, shape, dt):
        uid[0] += 1
        return stack.enter_context(nc.sbuf_tensor("%s_%d" % (name, uid[0]), shape, dt))

    def sb(name, shape, dt):
        return sbt(es, name, shape, dt)

    h = sb("h", [128, 8, S], F32)
    cst = sb("cst", [128, CL.n], F32)
    cm = sb("cm", [128, CM_N], BF16)
    cf = sb("cf", [128, CF_N], F32)
    wb = [sb("wb%d" % i, [128, 4096], BF16) for i in range(3)]
    wS = sb("wS", [128, 8, 8], BF16)
    sq = [sb("sq%d" % i, [128, TB], BF16) for i in range(2)]
    rstd = [sb("rstd%d" % i, [128, TB], F32) for i in range(2)]
    KA = sb("KA", [64, 2, S], BF16)
    IK = sb("IK", [64, S], BF16)
    V = sb("V", [128, 16, 128], BF16)
    cosF = sb("cosF", [64, S], BF16)
    sinF = sb("sinF", [64, S], BF16)
    Cst = sb("Cst", [128, 4, 256], F32)
    Cbf = sb("Cbf", [128, 4, 256], BF16)
    halo = sb("halo", [128, 8, 4], F32)
    mixA = sb("mixA", [64, 8, TB], BF16)
    mixM = sb("mixM", [128, 4, TB], BF16)
    small = sb("small", [128, 16], F32)
    psum = [es.enter_context(nc.psum_tensor("ps%d" % i, [128, 512], F32)) for i in range(8)]
    ps_ctr = [0]
    wb_ctr = [0]

    nbank = [8]

    def bank():
        b = psum[ps_ctr[0] % nbank[0]]
        ps_ctr[0] += 1
        return b

    def wbuf():
        b = wb[wb_ctr[0] % 3]
        wb_ctr[0] += 1
        return b

    def isap(v):
        return v is not None and not isinstance(v, (int, float))

    def mm(out, lhsT, rhs, start=True, stop=True):
        return P.add("pe", lambda e: e.matmul(out, lhsT, rhs, start=start, stop=stop),
                     reads=[lhsT, rhs], writes=[out])

    def dma(eng, out, in_, rd=None, wr=None):
        return P.add(eng, lambda e: e.dma_start(out=out, in_=in_),
                     reads=[rd] if rd is not None else [], writes=[wr] if wr is not None else [], dma=True)

    def wload(dst, src):
        return dma("pool", dst, src, wr=dst)

    def act(out, in_, func, bias=None, scale=None):
        kw = {}
        rds = [in_]
        if bias is not None:
            kw["bias"] = bias
            if isap(bias):
                rds.append(bias)
        if scale is not None:
            kw["scale"] = scale
            if isap(scale):
                rds.append(scale)
        return P.add("act", lambda e: e.activation(out, in_, func, **kw), reads=rds, writes=[out])

    def ts(eng, out, in0, s1, s2, op0, op1=None, accum=None):
        rds = [in0] + [s for s in (s1, s2) if isap(s)]
        wrs = [out] + ([accum] if accum is not None else [])
        if accum is not None:
            return P.add(eng, lambda e: e.tensor_scalar(out, in0, s1, s2, op0, op1, accum_out=accum),
                         reads=rds, writes=wrs)
        if op1 is None:
            return P.add(eng, lambda e: e.tensor_scalar(out, in0, s1, s2, op0), reads=rds, writes=wrs)
        return P.add(eng, lambda e: e.tensor_scalar(out, in0, s1, s2, op0, op1), reads=rds, writes=wrs)

    def stt(eng, out, in0, scalar, in1, op0, op1):
        rds = [in0, in1] + ([scalar] if isap(scalar) else [])
        return P.add(eng, lambda e: e.scalar_tensor_tensor(out, in0, scalar, in1, op0, op1),
                     reads=rds, writes=[out])

    def tt(eng, out, in0, in1, op):
        return P.add(eng, lambda e: e.tensor_tensor(out, in0, in1, op), reads=[in0, in1], writes=[out])

    def cp(eng, out, in_):
        if eng == "act":
            return act(out, in_, AF.Copy)
        return P.add(eng, lambda e: e.tensor_copy(out, in_), reads=[in_], writes=[out])

    def recip(out, in_):
        return P.add("dve", lambda e: e.reciprocal(out, in_), reads=[in_], writes=[out])

    def memset(eng, out, val):
        return P.add(eng, lambda e: e.memset(out, val), reads=[], writes=[out])

    def ccol(name, l, c=0, p=128):
        o = CL.off[(name, l)] + c
        return cst[0:p, o:o + 1]

    def cmat(name, p, w):
        return cm[0:p, CM[name]:CM[name] + w]

    dumps = {}

    def dump(name, ap):
        if name in dbg and name not in dumps:
            shp = list(ap.shape)
            d = nc.dram_tensor("dbg_" + name, shp, ap.dtype, kind="ExternalOutput").ap()
            dumps[name] = dma("sp", d, ap, rd=ap)

    dma("sp", cst[:], cst_d, wr=cst[:])
    dma("sp", cm[:], cm_d, wr=cm[:])
    dma("sp", cf[:], cf_d, wr=cf[:])

    def rstd_from_mean(r, ps_ap):
        act(r, ps_ap, AF.Ln, bias=EPS)
        act(r, r, AF.Exp, scale=-0.5)

    def recip_act(out, in_):
        act(out, in_, AF.Ln)
        act(out, out, AF.Exp, scale=-1.0)

    def rmsnorm_block(l, gname, src_tsl, dst, dst_tsl):
        ps = bank()
        for c in range(8):
            s = sq[c % 2]
            act(s[:], h[:, c, src_tsl], AF.Square)
            mm(ps[:], cmat("MEAN1024", 128, 128), s[:], c == 0, c == 7)
        r = rstd[ps_ctr[0] % 2]
        rstd_from_mean(r[:], ps[:])
        for c in range(8):
            stt("dve", dst[:, c, dst_tsl], h[:, c, src_tsl], ccol(gname, l, c), r[:], ALU.mult, ALU.mult)


    def red(eng, out, in_, op):
        return P.add(eng, lambda e: e.tensor_reduce(out, in_, AX.X, op), reads=[in_], writes=[out])

    def scan(out, d0, d1, init, op0, op1):
        return P.add("dve", lambda e: e.tensor_tensor_scan(out, d0, d1, init, op0, op1),
                     reads=[d0, d1], writes=[out])

    acc_banks = psum[4:8]

    def mixer_layer(b, l):
        nfb = small[0:4, 0:1]
        ts("dve", nfb, ccol("f_bias", l, 0, 4), -1.0, None, ALU.mult)
        negM = small[:, 1:2]
        gq = small[:, 2:3]
        gk = small[:, 3:4]
        lst = contextlib.ExitStack()
        tg = sbt(lst, "tg", [128, 64], F32)
        for nm, dstc in (("g_qn_row", gq), ("g_kn_row", gk)):
            o = CL.off[(nm, l)]
            row = cst[:, o:o + 64]
            ts("dve", tg[:], row, -1.0, None, ALU.mult)
            tt("dve", tg[:], tg[:], row, ALU.max)
            red("dve", dstc, tg[:], ALU.max)
        stt("dve", negM, gq, -8.0, gk, ALU.mult, ALU.mult)
        lst.close()
        memset("dve", Cst[:], 0.0)
        memset("pool", Cbf[:], 0.0)
        memset("pool", halo[:], 0.0)

        for j in range(NTB):
            tsl = slice(j * TB, (j + 1) * TB)
            blk = contextlib.ExitStack()
            xnb = sbt(blk, "xnb", [128, 8, TB], BF16)
            rmsnorm_block(l, "g_mix", tsl, xnb, slice(0, TB))
            if "attn" in stages:
                attn_block(b, l, j, tsl, xnb)
                P.barrier()
            if "mlstm" in stages:
                mlstm_block(b, l, j, tsl, xnb)
            blk.close()
            P.barrier()

    def head_fm(hx, ps_in, gcol, dst, cs32):
        xs, xb, t1, rs, sqh = hx
        cos32, sin32 = cs32
        cp("act", xs, ps_in)
        if gcol is not None:
            act(sqh, ps_in, AF.Square)
            psm = bank()
            mm(psm[0:64, :], cmat("MEAN64", 64, 64), sqh)
            rstd_from_mean(rs, psm[0:64, :])
            stt("dve", xs, xs, gcol, rs, ALU.mult, ALU.mult)
        cp("act", xb, xs)
        psr = bank()
        mm(psr[0:64, :], cmat("RT", 64, 64), xb)
        tt("dve", t1, xs, cos32, ALU.mult)
        tt("dve", rs, psr[0:64, :], sin32, ALU.mult)
        tt("dve", dst, t1, rs, ALU.add)

    def attn_block(b, l, j, tsl, xnb):
        ph = contextlib.ExitStack()
        IQb = sbt(ph, "QIb", [64, 8, TB], BF16)
        QAb = IQb
        acc = sbt(ph, "acc", [128, S], F32)
        tmp = [sbt(ph, "tmp%d" % i, [128, TB], F32) for i in range(2)]
        maskb = sbt(ph, "maskb", [128, S], BF16)
        maskT = sbt(ph, "maskT", [128, 16, TB], BF16)
        pT = [sbt(ph, "pT%d" % i, [128, TB], BF16) for i in range(2)]
        hx = (sbt(ph, "xs", [64, TB], F32), sbt(ph, "xb", [64, TB], BF16), sbt(ph, "t1", [64, TB], F32),
              sbt(ph, "rs", [64, TB], F32), sbt(ph, "sqh", [64, TB], BF16))
        cqf = acc[:, 0:2 * TB].rearrange("p (c n) -> p c n", c=2)
        hx2 = (acc[0:64, 0:TB], maskb[0:64, 0:TB], acc[0:64, TB:2 * TB], acc[0:64, 2 * TB:3 * TB],
               maskb[0:64, TB:2 * TB])
        hx1 = tuple(t_[:] for t_ in hx)
        hxs = [hx1, hx2]
        hx_ctr = [0]

        def next_hx():
            hx_ctr[0] += 1
            return hxs[hx_ctr[0] % 2]
        cqn = sbt(ph, "cqn", [128, 2, TB], BF16)
        wabs = sbt(ph, "wabs", [128, 4, 8], F32)
        wsgn = sbt(ph, "wsgn", [128, 4, 8], F32)
        bs = sbt(ph, "bs", [128, 8 + N_BISECT], F32)
        rc = tmp[0][0:64, :]
        cs32 = (tmp[0][0:64, :], tmp[1][0:64, :])

        def load_cs32():
            cp("act", cs32[0], cosF[:, tsl])
            cp("act", cs32[1], sinF[:, tsl])

        load_cs32()

        b1 = wbuf()
        wcq = b1[:, 0:2048].rearrange("p (c n) -> p c n", c=8)
        wqu = b1[:, 2048:3072].rearrange("p (c n) -> p c n", c=2)
        wiq = b1[:, 3072:4096].rearrange("p (c n) -> p c n", c=2)
        wload(wcq, w_in_d[l, :, 0:256].rearrange("(c p) n -> p c n", p=128))
        wload(wqu, w_qup_d[l].rearrange("(c p) n -> p c n", p=128))
        wload(wiq, w_iqup_d[l].rearrange("(c p) n -> p c n", p=128))
        b2 = wbuf()
        wgk = b2[:, 0:8 * 328].rearrange("p (c n) -> p c n", c=8)
        wload(wgk, w_in_d[l, :, 256:584].rearrange("(c p) n -> p c n", p=128))

        psm = bank()
        for cc in range(2):
            ps = bank()
            for kc in range(8):
                mm(ps[:], wcq[:, kc, cc * 128:(cc + 1) * 128], xnb[:, kc, :], kc == 0, kc == 7)
            cp("act", cqf[:, cc, :], ps[:])
            s = sq[cc % 2]
            act(s[:], ps[:], AF.Square)
            mm(psm[:], cmat("MEAN256", 128, 128), s[:], cc == 0, cc == 1)
        r = rstd[0]
        rstd_from_mean(r[:], psm[:])
        for cc in range(2):
            stt("dve", cqn[:, cc, :], cqf[:, cc, :], ccol("g_cq", l, cc), r[:], ALU.mult, ALU.mult)

        for g in range(2):
            ps = bank()
            for kc in range(8):
                mm(ps[0:64, :], wgk[:, kc, g * 64:(g + 1) * 64], xnb[:, kc, :], kc == 0, kc == 7)
            head_fm(next_hx(), ps[0:64, :], ccol("g_kn", l, 0, 64), KA[:, g, tsl], cs32)
        ps = bank()
        for kc in range(8):
            mm(ps[0:64, :], wgk[:, kc, 256:320], xnb[:, kc, :], kc == 0, kc == 7)
        head_fm(next_hx(), ps[0:64, :], ccol("g_ik", l, 0, 64), IK[:, tsl], cs32)
        for t in range(4):
            ps = bank()
            for kc in range(8):
                mm(ps[:, 0:128], xnb[:, kc, t * 128:(t + 1) * 128], wgk[:, kc, 128:256], kc == 0, kc == 7)
            cp("act", V[:, 4 * j + t, :], ps[:, 0:128])
            ps = bank()
            for kc in range(8):
                mm(ps[:, 0:8], xnb[:, kc, t * 128:(t + 1) * 128], wgk[:, kc, 320:328], kc == 0, kc == 7)
            act(wabs[:, t, :], ps[:, 0:8], AF.Abs, scale=IDX_SCALE)
            act(wsgn[:, t, :], ps[:, 0:8], AF.Sign)
        for hd in range(8):
            ps = bank()
            for kc in range(2):
                mm(ps[0:64, :], wiq[:, kc, hd * 64:(hd + 1) * 64], cqn[:, kc, :], kc == 0, kc == 1)
            head_fm(next_hx(), ps[0:64, :], None, IQb[:, hd, :], cs32)
        if b == 0 and l == 0 and j == 0:
            dump("KA", KA[:, :, 0:TB])
            dump("IK", IK[:, 0:TB])
            dump("IQb", IQb[:])
            dump("V", V[:, 0:4, :])
            dump("wabs", wabs[:])
            dump("wsgn", wsgn[:])

        lo, mid, cnt, tcol, w0, hi, thr = [bs[:, i:i + 1] for i in range(7)]
        Wb = bs[:, 8:8 + N_BISECT]
        for t in range(4):
            qi = 4 * j + t
            L = 128 * (qi + 1)
            qsl = slice(t * 128, (t + 1) * 128)
            for c in range(j + 1):
                ncols = min(512, L - 512 * c)
                cs = slice(512 * c, 512 * c + ncols)
                for hd in range(8):
                    ps = bank()
                    mm(ps[:, 0:ncols], IQb[:, hd, qsl], IK[:, cs])
                    tm = tmp[hd % 2]
                    act(tm[:, 0:ncols], ps[:, 0:ncols], AF.Relu, scale=wabs[:, t, hd:hd + 1])
                    eng = "dve"
                    if hd == 0:
                        ts(eng, acc[:, cs], tm[:, 0:ncols], wsgn[:, t, hd:hd + 1], None, ALU.mult)
                    elif eng == "dve":
                        stt(eng, acc[:, cs], tm[:, 0:ncols], wsgn[:, t, hd:hd + 1], acc[:, cs], ALU.mult, ALU.add)
                    else:
                        ts("pool", tm[:, 0:ncols], tm[:, 0:ncols], wsgn[:, t, hd:hd + 1], None, ALU.mult)
                        tt("pool", acc[:, cs], acc[:, cs], tm[:, 0:ncols], ALU.add)
            if qi >= 2:
                red("dve", lo, acc[:, 0:L], ALU.min)
                red("dve", hi, acc[:, 0:L], ALU.max)
            tt("dve", acc[:, qi * 128:(qi + 1) * 128], acc[:, qi * 128:(qi + 1) * 128],
               cf[:, CF["CAUS"]:CF["CAUS"] + 128], ALU.add)
            if b == 0 and l == 0 and j == 0 and t == 3:
                dump("acc", acc[:, 0:512])
            if qi < 2:
                memset("dve", lo, -1.0e29)
            else:
                tt("dve", w0, hi, lo, ALU.subtract)
                ts("dve", w0, w0, 1.0001, 1e-12, ALU.mult, ALU.add)
                ts("dve", Wb, cf[:, CF["POW2"]:CF["POW2"] + N_BISECT], w0, None, ALU.mult)
                tt("dve", mid, lo, Wb[:, 0:1], ALU.add)
                for k in range(N_BISECT):
                    ts("dve", maskb[:, 0:L], acc[:, 0:L], mid, None, ALU.is_ge, ALU.add, accum=cnt)
                    stt("dve", tcol, cnt, 255.5, Wb[:, k:k + 1], ALU.is_ge, ALU.mult)
                    kn = min(k + 1, N_BISECT - 1)
                    stt("dve", mid, tcol, Wb[:, kn:kn + 1], mid, ALU.subtract, ALU.add)
                cp("dve", lo, mid)
            ts("dve", maskb[:, 0:L], acc[:, 0:L], lo, None, ALU.is_ge)
            if b == 0 and l == 0 and j == 0 and t == 3:
                dump("maskb", maskb[:, 0:512])
                dump("bs", bs[:])
            for k4 in range(0, qi + 1, 4):
                n = min(4, qi + 1 - k4)
                ps = bank()
                for kk in range(n):
                    mm(ps[:, kk * 128:(kk + 1) * 128], maskb[:, (k4 + kk) * 128:(k4 + kk + 1) * 128],
                       cmat("IDENT", 128, 128))
                cp("act", maskT[:, k4:k4 + n, qsl], ps[:, 0:n * 128].rearrange("p (a q) -> p a q", a=n))
            for kt in range(qi + 1, 4 * (j + 1)):
                memset("pool", maskT[:, kt, qsl], 0.0)

        load_cs32()
        for hd in range(8):
            ps = bank()
            for kc in range(2):
                mm(ps[0:64, :], wqu[:, kc, hd * 64:(hd + 1) * 64], cqn[:, kc, :], kc == 0, kc == 1)
            head_fm(next_hx(), ps[0:64, :], ccol("g_qn", l, 0, 64), QAb[:, hd, :], cs32)
        if b == 0 and l == 0 and j == 0:
            dump("QAb", QAb[:])
        nbank[0] = 4
        nkt = 4 * (j + 1)
        for hd in range(8):
            g = hd // 4
            psn = acc_banks[(hd % 2) * 2]
            psd = acc_banks[(hd % 2) * 2 + 1]
            for kt in range(nkt):
                ps = bank()
                mm(ps[:], KA[:, g, kt * 128:(kt + 1) * 128], QAb[:, hd, :])
                pt = pT[kt % 2]
                act(pt[:], ps[:], AF.Exp, bias=small[:, 1:2], scale=0.125)
                tt("dve", pt[:], pt[:], maskT[:, kt, :], ALU.mult)
                mm(psn[0:64, :], V[:, kt, g * 64:(g + 1) * 64], pt[:], kt == 0, kt == nkt - 1)
                mm(psd[0:64, :], cmat("ONES", 128, 64), pt[:], kt == 0, kt == nkt - 1)
            recip_act(rc, psd[0:64, :])
            tt("dve", mixA[:, hd, :], psn[0:64, :], rc, ALU.mult)
        if b == 0 and l == 0 and j == 0:
            dump("mixA", mixA[:])
        nbank[0] = 8
        wo = []
        for half in range(2):
            bb = wbuf()
            v = bb[0:64, :].rearrange("p (c n) -> p c n", c=4)
            wload(v, w_out_d[l, half * 256:(half + 1) * 256, :].rearrange("(c p) n -> p c n", p=64))
            wo.append(v)
        for dc in range(8):
            ps = bank()
            for hd in range(8):
                mm(ps[:], wo[hd // 4][:, hd % 4, dc * 128:(dc + 1) * 128], mixA[:, hd, :], hd == 0, hd == 7)
            tt("dve", h[:, dc, tsl], h[:, dc, tsl], ps[:], ALU.add)
        ph.close()

    def mlstm_block(b, l, j, tsl, xnb):
        ph = contextlib.ExitStack()
        lic = sbt(ph, "lic", [4, TB], F32)
        l1 = sbt(ph, "l1", [4, TB], F32)
        nbt = sbt(ph, "nbt", [4, TB], F32)
        aa = lic
        ag = l1
        eaeg = sbt(ph, "eaeg", [64, 8, 8], F32)
        EB = [sbt(ph, "EB%d" % i, [128, TB], BF16) for i in range(4)]
        EBL = sbt(ph, "EBL", [128, 4, 8], F32)
        xc = [sbt(ph, "xc%d" % i, [128, 4 + TB], F32) for i in range(2)]
        cacc = [sbt(ph, "cacc%d" % i, [128, TB], F32) for i in range(2)]
        qf = sbt(ph, "qf", [128, 4, TB], BF16)
        kf = sbt(ph, "kf", [128, 4, TB], BF16)
        kg = sbt(ph, "kg", [64, 8, 4, 128], BF16)
        vt = sbt(ph, "vt", [64, 8, 512], BF16)
        og = sbt(ph, "og", [128, 4, TB], BF16)
        Ssb = [sbt(ph, "Ssb%d" % i, [64, 64], BF16) for i in range(4)]
        hm = cacc[1]
        dn = cacc[0]

        wload(wS[:], w_in_d[l, :, 2632:2640].rearrange("(c p) n -> p c n", p=128))
        ps_i = bank()
        for kc in range(8):
            mm(ps_i[0:4, :], wS[:, kc, 0:4], xnb[:, kc, :], kc == 0, kc == 7)
        ps_f = bank()
        for kc in range(8):
            mm(ps_f[0:4, :], wS[:, kc, 4:8], xnb[:, kc, :], kc == 0, kc == 7)
        ts("dve", lic[:], ps_i[0:4, :], ccol("i_bias", l, 0, 4), None, ALU.add)
        act(l1[:], ps_f[0:4, :], AF.Exp, bias=small[0:4, 0:1], scale=-1.0)
        act(l1[:], l1[:], AF.Ln, bias=1.0)
        scan(nbt[:], cf[0:4, CF["RESET"]:CF["RESET"] + TB], l1[:], 0.0, ALU.mult, ALU.add)
        tt("dve", aa[:], lic[:], nbt[:], ALU.add)
        nb3 = nbt[:, :].rearrange("p (c s) -> p c s", c=8)
        for c in range(8):
            ts("dve", ag[:, c * 64:(c + 1) * 64], aa[:, c * 64:(c + 1) * 64], nbt[:, c * 64 + 63:c * 64 + 64], None,
               ALU.subtract)
        if KSTOP <= 1:
            ph.close()
            return
        hl = {}
        for nm, src in (("aa", aa), ("ag", ag), ("nb", nbt)):
            hi = sbt(ph, nm + "hi", [4, TB], BF16)
            lo_ = sbt(ph, nm + "lo", [4, TB], BF16)
            hf = xc[0][0:4, 0:TB]
            cp("dve", hi[:], src[:])
            cp("dve", hf, hi[:])
            tt("dve", hf, src[:], hf, ALU.subtract)
            cp("dve", lo_[:], hf)
            hl[nm] = (hi, lo_)
        i4 = cm[0:4, CM["I4"]:CM["I4"] + 4]
        psg = bank()
        for c in range(8):
            for q_, nm in ((0, "aa"), (4, "ag")):
                hi, lo_ = hl[nm]
                mm(psg[0:64, c * 8 + q_:c * 8 + q_ + 4], hi[:, c * 64:(c + 1) * 64], i4, True, False)
                mm(psg[0:64, c * 8 + q_:c * 8 + q_ + 4], lo_[:, c * 64:(c + 1) * 64], i4, False, True)
        act(eaeg[:, :, :].rearrange("p c n -> p (c n)"), psg[0:64, 0:64], AF.Exp)
        for hd in range(4):
            ps = bank()
            sel = cm[0:4, CM["SEL"] + hd * 128:CM["SEL"] + (hd + 1) * 128]
            mm(ps[:], sel, hl["nb"][0][:], True, False)
            mm(ps[:], sel, hl["nb"][1][:], False, True)
            act(EB[hd][:], ps[:], AF.Exp, scale=-1.0)
            act(EBL[:, hd, :], ps[:, 63:512:64], AF.Exp, scale=-1.0)
        if b == 0 and l == 0 and j == 0:
            dump("lic", lic[:])
            dump("nbt", nbt[:])
            dump("eaeg", eaeg[:])
            dump("EB0", EB[0][:])

        if KSTOP <= 2:
            ph.close()
            return
        for which in range(2):
            wbb = wbuf()
            w = wbb[:, :].rearrange("p (c n) -> p c n", c=8)
            c0 = 584 + which * 512
            wload(w, w_in_d[l, :, c0:c0 + 512].rearrange("(c p) n -> p c n", p=128))
            for cc in range(4):
                ch = which * 4 + cc
                ps = bank()
                for kc in range(8):
                    mm(ps[:], w[:, kc, cc * 128:(cc + 1) * 128], xnb[:, kc, :], kc == 0, kc == 7)
                x_ = xc[cc % 2]
                ca = cacc[cc % 2]
                cp("pool", x_[:, 0:4], halo[:, ch, :])
                cp("act", x_[:, 4:4 + TB], ps[:])
                if KSUB <= 1:
                    continue
                cw = CL.off[("conv_w", l)]
                act(ca[:], x_[:, 1:1 + TB], AF.Identity, bias=ccol("conv_b", l, ch), scale=cst[:, cw + ch:cw + ch + 1])
                for jt in range(1, 4):
                    stt("dve", ca[:], x_[:, 1 + jt:1 + jt + TB],
                        cst[:, cw + jt * 8 + ch:cw + jt * 8 + ch + 1], ca[:], ALU.mult, ALU.add)
                if KSUB <= 2:
                    continue
                cp("pool", halo[:, ch, :], x_[:, TB:TB + 4])
                if KSUB <= 3:
                    continue
                sg = x_[:, 4:4 + TB]
                act(sg, ca[:], AF.Sigmoid)
                if which == 0:
                    tt("dve", ca[:], ca[:], sg, ALU.mult)
                    ts("dve", qf[:, cc, :], ca[:], float(128 ** -0.5), None, ALU.mult)
                    tt("dve", qf[:, cc, :], qf[:, cc, :], EB[cc][:], ALU.mult)
                else:
                    tt("dve", kf[:, cc, :], ca[:], sg, ALU.mult)
        if KSTOP <= 3:
            ph.close()
            return
        wbb = wbuf()
        wv = wbb[:, :].rearrange("p (c n) -> p c n", c=8)
        wload(wv, w_in_d[l, :, 1608:2120].rearrange("(c p) n -> p c n", p=128))
        for c in range(8):
            ps = bank()
            for kc in range(8):
                mm(ps[0:64, :], xnb[:, kc, c * 64:(c + 1) * 64], wv[:, kc, :], kc == 0, kc == 7)
            cp("act" if c % 2 == 0 else "dve", vt[:, c, :], ps[0:64, :])
        if KSUB2 <= 1:
            ph.close()
            return
        for c in range(8):
            ps = bank()
            for hd in range(4):
                mm(ps[0:64, hd * 128:(hd + 1) * 128], kf[:, hd, c * 64:(c + 1) * 64], cmat("IDENT", 128, 128))
            for hd in range(4):
                ts("dve", kg[:, c, hd, :], ps[0:64, hd * 128:(hd + 1) * 128], eaeg[:, c, 4 + hd:5 + hd], None, ALU.mult)
        if KSUB2 <= 2:
            ph.close()
            return
        wbb = wbuf()
        w = wbb[:, :].rearrange("p (c n) -> p c n", c=8)
        wload(w, w_in_d[l, :, 2120:2632].rearrange("(c p) n -> p c n", p=128))
        for cc in range(4):
            ps = bank()
            for kc in range(8):
                mm(ps[:], w[:, kc, cc * 128:(cc + 1) * 128], xnb[:, kc, :], kc == 0, kc == 7)
            act(og[:, cc, :], ps[:], AF.Sigmoid)
        if b == 0 and l == 0 and j == 0:
            dump("qf", qf[:])
            dump("kf", kf[:])
            dump("vt", vt[:])
            dump("kg", kg[:])
            dump("og", og[:])

        if KSTOP <= 4:
            ph.close()
            return
        nbank[0] = 4
        for pair in range(2):
            for c in range(8):
                csl = slice(c * 64, (c + 1) * 64)
                for hh in range(2):
                    hd = pair * 2 + hh
                    psN = acc_banks[hh * 2]
                    psDn = acc_banks[hh * 2 + 1]
                    psS = bank()
                    mm(psS[0:64, 0:64], kf[:, hd, csl], qf[:, hd, csl])
                    Sb = Ssb[(c * 2 + hh) % 4]
                    stt("dve", Sb[:], psS[0:64, 0:64], eaeg[:, c, hd:hd + 1], cf[0:64, CF["TRIU"]:CF["TRIU"] + 64], ALU.mult, ALU.mult)
                    mm(psN[:, csl], Cbf[:, hd, 0:128], qf[:, hd, csl], True, False)
                    mm(psN[:, csl], vt[:, c, hd * 128:(hd + 1) * 128], Sb[:], False, True)
                    mm(psDn[:, csl], Cbf[:, hd, 128:256], qf[:, hd, csl], True, False)
                    mm(psDn[:, csl], cmat("ONES", 64, 128), Sb[:], False, True)
                    psD = bank()
                    mm(psD[:, 0:128], kg[:, c, hd, :], vt[:, c, hd * 128:(hd + 1) * 128])
                    mm(psD[:, 128:256], kg[:, c, hd, :], cmat("ONES", 64, 128))
                    stt("dve", Cst[:, hd, :], Cst[:, hd, :], EBL[:, hd, c:c + 1], psD[:, 0:256],
                        ALU.mult, ALU.add)
                    cp("act", Cbf[:, hd, :], Cst[:, hd, :])
            for hh in range(2):
                hd = pair * 2 + hh
                psN = acc_banks[hh * 2]
                psDn = acc_banks[hh * 2 + 1]
                act(dn[:], psDn[:], AF.Abs)
                ts("dve", dn[:], dn[:], 1.0, None, ALU.max)
                recip_act(dn[:], dn[:])
                tt("dve", hm[:], psN[:], dn[:], ALU.mult)
                if b == 0 and l == 0 and j == 0 and hd == 0:
                    dump("hm0", hm[:])
                s = sq[hh]
                act(s[:], hm[:], AF.Square)
                psm = bank()
                mm(psm[:], cmat("MEAN128", 128, 128), s[:])
                r = rstd[hh]
                rstd_from_mean(r[:], psm[:])
                g_o = CL.off[("g_mh", l)] + hd
                hmb = sq[hh]
                stt("dve", hmb[:], hm[:], cst[:, g_o:g_o + 1], r[:], ALU.mult, ALU.mult)
                tt("dve", mixM[:, hd, :], hmb[:], og[:, hd, :], ALU.mult)
        if b == 0 and l == 0 and j == 0:
            dump("mixM", mixM[:])
        nbank[0] = 8
        wbb = wbuf()
        wo = wbb[:, :].rearrange("p (c n) -> p c n", c=4)
        wload(wo, w_out_d[l, 512:1024, :].rearrange("(c p) n -> p c n", p=128))
        for dc in range(8):
            ps = bank()
            for kc in range(4):
                mm(ps[:], wo[:, kc, dc * 128:(dc + 1) * 128], mixM[:, kc, :], kc == 0, kc == 3)
            tt("dve", h[:, dc, tsl], h[:, dc, tsl], ps[:], ALU.add)
        ph.close()

    final_ops = []

    for b in range(nb):
        for c in range(8):
            dma("sp", h[:, c, :], x_d[b, c * 128:(c + 1) * 128, :], wr=h[:, c, :])
        if "mix" in stages:
            ph = contextlib.ExitStack()
            posi = sbt(ph, "posi", [64, S], I32)
            ang = sbt(ph, "ang", [64, S], F32)
            kf = sbt(ph, "kf", [64, S], F32)
            ki = sbt(ph, "ki", [64, S], I32)
            msk = sbt(ph, "msk", [64, S], F32)
            dma("sp", posi[:], pos_d[b], wr=posi[:])
            cp("dve", ang[:], posi[:])
            ts("dve", ang[:], ang[:], cf[0:64, CF["INV"]:CF["INV"] + 1], None, ALU.mult)
            for (dstT, shift) in ((sinF, 0.0), (cosF, np.pi / 2)):
                ts("dve", kf[:], ang[:], shift, 1.0 / TWO_PI, ALU.add, ALU.mult)
                cp("dve", ki[:], kf[:])
                cp("dve", kf[:], ki[:])
                ts("dve", msk[:], ang[:], shift, None, ALU.add)
                stt("dve", kf[:], kf[:], -TWO_PI, msk[:], ALU.mult, ALU.add)
                ts("dve", msk[:], kf[:], float(np.pi), None, ALU.is_gt)
                stt("dve", kf[:], msk[:], -TWO_PI, kf[:], ALU.mult, ALU.add)
                ts("dve", msk[:], kf[:], float(-np.pi), None, ALU.is_lt)
                stt("dve", kf[:], msk[:], TWO_PI, kf[:], ALU.mult, ALU.add)
                ts("dve", kf[:], kf[:], float(np.pi), float(-np.pi), ALU.min, ALU.max)
                act(dstT[:], kf[:], AF.Sin)
            dump("cosF", cosF[:])
            dump("sinF", sinF[:])
            ph.close()
            P.barrier()

        for l in range(nl):
            if "mix" in stages:
                mixer_layer(b, l)
            ph = contextlib.ExitStack()
            xn = sbt(ph, "xn", [128, 8, S], BF16)
            ubuf = sbt(ph, "ubuf", [128, 4, S], BF16)
            relu_t = [sbt(ph, "relu%d" % i, [128, TB], F32) for i in range(2)]
            if "ffn" in stages:
                for tb in range(NTB):
                    tsl = slice(tb * TB, (tb + 1) * TB)
                    rmsnorm_block(l, "g_mlp", tsl, xn, tsl)
                for j in range(8):
                    w1b = wbuf()
                    w2b = wbuf()
                    w1 = w1b[:, :].rearrange("p (c n) -> p c n", c=8)
                    w2 = w2b[:, :].rearrange("p (c n) -> p c n", c=4)
                    wload(w1, w_ff1_d[l, :, j * 512:(j + 1) * 512].rearrange("(c p) n -> p c n", p=128))
                    wload(w2, w_ff2_d[l, j * 512:(j + 1) * 512, :].rearrange("(c p) n -> p c n", p=128))
                    for tb in range(NTB):
                        tsl = slice(tb * TB, (tb + 1) * TB)
                        for fc in range(4):
                            ps = bank()
                            for kc in range(8):
                                mm(ps[:], w1[:, kc, fc * 128:(fc + 1) * 128], xn[:, kc, tsl], kc == 0, kc == 7)
                            rl = relu_t[fc % 2]
                            act(rl[:], ps[:], AF.Relu)
                            tt("dve", ubuf[:, fc, tsl], rl[:], rl[:], ALU.mult)
                    for tb in range(NTB):
                        tsl = slice(tb * TB, (tb + 1) * TB)
                        for dc in range(8):
                            ps = bank()
                            for fc in range(4):
                                mm(ps[:], w2[:, fc, dc * 128:(dc + 1) * 128], ubuf[:, fc, tsl], fc == 0, fc == 3)
                            tt("dve", h[:, dc, tsl], h[:, dc, tsl], ps[:], ALU.add)
            if "ple" in stages:
                for tb in range(NTB):
                    tsl = slice(tb * TB, (tb + 1) * TB)
                    rmsnorm_block(l, "g_ple", tsl, xn, tsl)
                pb = ubuf
                for kc in range(2):
                    wload(pb[:, kc, :], p_d[l, b, kc * 128:(kc + 1) * 128, :])
                for half in range(2):
                    wgb = wbuf()
                    wpb = wbuf()
                    wg = wgb[:, :].rearrange("p (c n) -> p c n", c=8)
                    wp = wpb[:, 0:1024].rearrange("p (c n) -> p c n", c=2)
                    wload(wg, w_pg_d[l, :, half * 512:(half + 1) * 512].rearrange("(c p) n -> p c n", p=128))
                    wload(wp, w_pl_d[l, :, half * 512:(half + 1) * 512].rearrange("(c p) n -> p c n", p=128))
                    for tb in range(NTB):
                        tsl = slice(tb * TB, (tb + 1) * TB)
                        for dcl in range(4):
                            dc = half * 4 + dcl
                            ps = bank()
                            for kc in range(8):
                                mm(ps[:], wg[:, kc, dcl * 128:(dcl + 1) * 128], xn[:, kc, tsl], kc == 0, kc == 7)
                            g = relu_t[dcl % 2]
                            act(g[:], ps[:], AF.Sigmoid, bias=ccol("b_ple", l, dc))
                            ps2 = bank()
                            for kc in range(2):
                                mm(ps2[:], wp[:, kc, dcl * 128:(dcl + 1) * 128], pb[:, kc, tsl], kc == 0, kc == 1)
                            tt("dve", g[:], g[:], ps2[:], ALU.mult)
                            tt("dve", h[:, dc, tsl], h[:, dc, tsl], g[:], ALU.add)
            ph.close()
            P.barrier()
        for c in range(8):
            final_ops.append(dma("sp", out_d[b, c * 128:(c + 1) * 128, :], h[:, c, :], rd=h[:, c, :]))

    final_ops += list(dumps.values())
    P.emit(final_wait_ops=final_ops)
    es.close()
    return nc


def make_in_maps(inp, nb, ncores):
    cst = build_cst(inp)
    cm = build_cm()
    cf = build_cf()
    x = inp["x"]
    p = inp["p"]
    pos = inp["positions"].astype(np.int32)
    in_maps = []
    for c in range(ncores):
        bs = slice(c * nb, (c + 1) * nb)
        m = {
            "x": np.ascontiguousarray(x[bs].transpose(0, 2, 1)),
            "p": np.ascontiguousarray(p[:, bs].transpose(0, 1, 3, 2)),
            "pos": np.ascontiguousarray(np.broadcast_to(pos[bs][:, None, :], (nb, 64, S))),
            "cst": cst, "cm": cm, "cf": cf,
        }
        for k in ("w_in", "w_q_up", "w_iq_up", "w_out", "w_ff1", "w_ff2", "w_ple_gate", "w_ple"):
            m[k] = np.ascontiguousarray(inp[k], dtype=np.float32)
        in_maps.append(m)
    return in_maps


def kernel(**inputs):
    inp = {k: np.asarray(v) for k, v in inputs.items()}
    nb = 2
    nc = build_nc(nb=nb, stages=RUN_STAGES)
    in_maps = make_in_maps(inp, nb, NCORES)
    res = run_bass_kernel_spmd(nc, in_maps, core_ids=list(range(NCORES)))
    out = np.concatenate([r["out"] for r in res.results], axis=0)
    return np.ascontiguousarray(out.transpose(0, 2, 1)).astype(np.float32)
```

```python
import contextlib
import os
import numpy as np
import concourse.bass as bass
import concourse.mybir as mybir
from concourse.bass_utils import run_bass_kernel_spmd

F32 = mybir.dt.float32
BF16 = mybir.dt.bfloat16
I32 = mybir.dt.int32
ALU = mybir.AluOpType
AF = mybir.ActivationFunctionType
AX = mybir.AxisListType

D = 1024
S = 2048
NL = 2
NCORES = 8
TB = 512
NTB = S // TB
EPS = 1e-6
IN_W = 2640
DFF = 4096

ENGS = ("pe", "act", "dve", "pool", "sp")
N_DMA_SEMS = 16


def _region(ap):
    if type(ap.tensor).__name__.startswith("PSum"):
        return (ap.tensor.name, 0, 128, 0, 1 << 30)
    pat = ap.ap
    row = pat[0][0]
    npart = pat[0][1]
    off = ap.offset
    if row <= 0:
        p0, f0 = 0, off
    else:
        p0 = off // row
        f0 = off - p0 * row
    ext = 1
    for st, cnt in pat[1:]:
        ext += (cnt - 1) * abs(st)
    sz = mybir.dt.size(ap.dtype)
    return (ap.tensor.name, p0, p0 + npart, f0 * sz, (f0 + ext) * sz)


def _overlap(a, b):
    return a[1] < b[2] and b[1] < a[2] and a[3] < b[4] and b[3] < a[4]


class Op:
    __slots__ = ("eng", "fn", "reads", "writes", "dma", "deps", "sig", "idx", "dmak")


class Prog:
    def __init__(self, nc):
        self.nc = nc
        self.ops = []
        self.hist = {}
        self.last_op = {}
        self.bar_id = 0
        self.bar_deps = set()
        self.eng_bar = {}
        self.dma_pending = []
        self.pending = {}

    def barrier(self):
        deps = set(self.last_op.values()) | set(self.dma_pending)
        self.dma_pending = []
        for e in ENGS:
            self.pending.setdefault(e, set()).update(deps)

    def add(self, eng, fn, reads=(), writes=(), dma=False):
        op = Op()
        op.eng, op.fn, op.dma = eng, fn, dma
        op.reads = [_region(a) for a in reads if a is not None]
        op.writes = [_region(a) for a in writes if a is not None]
        op.deps = set()
        op.sig = None
        op.dmak = None
        op.idx = len(self.ops)
        self.ops.append(op)
        ops = self.ops
        for r in op.reads:
            for (reg, oi, isw) in self.hist.setdefault(r[0], []):
                if isw and _overlap(reg, r):
                    op.deps.add(oi)
        for w in op.writes:
            keep = []
            for rec in self.hist.setdefault(w[0], []):
                reg, oi, isw = rec
                if _overlap(reg, w):
                    op.deps.add(oi)
                    if w[1] <= reg[1] and reg[2] <= w[2] and w[3] <= reg[3] and reg[4] <= w[4]:
                        continue
                keep.append(rec)
            self.hist[w[0]] = keep
        for r in op.reads:
            h = self.hist[r[0]]
            if not dma:
                h[:] = [rec for rec in h if not ((not rec[2]) and rec[0] == r
                                                 and ops[rec[1]].eng == eng and not ops[rec[1]].dma)]
            h.append((r, op.idx, False))
        for w in op.writes:
            self.hist[w[0]].append((w, op.idx, True))
        if self.pending.get(eng):
            op.deps |= self.pending[eng]
            self.pending[eng] = set()
        if not dma:
            self.last_op[eng] = op.idx
        else:
            self.dma_pending.append(op.idx)
        op.deps.discard(op.idx)
        return op

    def emit(self, final_wait_ops=()):
        nc = self.nc
        ops = self.ops
        for op in ops:
            if op.eng == "pe" and not op.dma:
                op.deps = {d for d in op.deps if not (ops[d].eng == "pe" and not ops[d].dma)}
        needed = set()
        for op in ops:
            needed |= op.deps
        for o in final_wait_ops:
            needed.add(o.idx)
        cnt = {e: 0 for e in ENGS}
        dmak = {"sp": 0, "pool": 0, "act": 0}
        dma_ops = {"sp": [], "pool": [], "act": []}
        for op in ops:
            if op.dma:
                k = dmak[op.eng]
                op.dmak = k
                op.sig = ("dma", (op.eng, k % N_DMA_SEMS), 16 * (k // N_DMA_SEMS + 1))
                dmak[op.eng] += 1
                dma_ops[op.eng].append(op)
            elif op.idx in needed:
                cnt[op.eng] += 1
                op.sig = (op.eng, cnt[op.eng])
        with contextlib.ExitStack() as st:
            sems = {e: st.enter_context(nc.semaphore("s_" + e)) for e in ENGS}
            dsems = {(q, i): st.enter_context(nc.semaphore("d_%s_%d" % (q, i)))
                     for q in ("sp", "pool") for i in range(N_DMA_SEMS)}
            block = st.enter_context(nc.Block())
            per_eng = {e: [op for op in ops if op.eng == e] for e in ENGS}

            def run(engname, eng):
                known = {}
                for op in per_eng[engname]:
                    waits = {}
                    deps = set(op.deps)
                    if op.dma and op.dmak >= N_DMA_SEMS:
                        deps.add(dma_ops[op.eng][op.dmak - N_DMA_SEMS].idx)
                    for d in deps:
                        s = ops[d].sig
                        if s[0] == "dma":
                            key, v = ("dma", s[1]), s[2]
                        else:
                            key, v = ("c", s[0]), s[1]
                        if known.get(key, 0) >= v:
                            continue
                        if waits.get(key, 0) < v:
                            waits[key] = v
                    for key, v in waits.items():
                        sem = dsems[key[1]] if key[0] == "dma" else sems[key[1]]
                        eng.wait_ge(sem, v)
                        known[key] = v
                    ins = op.fn(eng)
                    if op.sig is not None:
                        if op.sig[0] == "dma":
                            ins.then_inc(dsems[op.sig[1]], 16)
                        else:
                            ins.then_inc(sems[op.sig[0]], 1)
                if engname == "sp":
                    for o in final_wait_ops:
                        s = o.sig
                        if s[0] == "dma":
                            eng.wait_ge(dsems[s[1]], s[2])
                        else:
                            eng.wait_ge(sems[s[0]], s[1])

            @block.tensor
            def _(e):
                run("pe", e)

            @block.scalar
            def _(e):
                run("act", e)

            @block.vector
            def _(e):
                run("dve", e)

            @block.gpsimd
            def _(e):
                run("pool", e)

            @block.sync
            def _(e):
                run("sp", e)


IDX_SCALE = (8 ** -0.5) * (64 ** -0.5)
KSTOP = int(os.environ.get('K_STOP', '99'))
RUN_STAGES = ("mix", "attn", "mlstm", "ffn", "ple")
KSUB = int(os.environ.get('K_SUB', '99'))
KSUB2 = int(os.environ.get('K_SUB2', '99'))
NEG_BIG = -1.0e30
N_BISECT = 24
TWO_PI = 2.0 * np.pi


def _fm_cols(v, p=128):
    v = np.asarray(v, np.float32)
    return np.ascontiguousarray(v.reshape(-1, p).T)


class CstLayout:
    def __init__(self):
        self.off = {}
        self.n = 0

    def add(self, name, ncols):
        self.off[name] = self.n
        self.n += ncols


def make_cst_layout():
    L = CstLayout()
    for l in range(NL):
        for nm in ("g_mix", "g_mlp", "g_ple", "b_ple", "conv_b"):
            L.add((nm, l), 8)
        L.add(("g_cq", l), 2)
        for nm in ("g_qn", "g_kn", "g_ik", "i_bias", "f_bias"):
            L.add((nm, l), 1)
        L.add(("conv_w", l), 32)
        L.add(("g_mh", l), 4)
        L.add(("g_qn_row", l), 64)
        L.add(("g_kn_row", l), 64)
    return L


CL = make_cst_layout()


def build_cst(inp):
    c = np.zeros((128, CL.n), np.float32)
    for l in range(NL):
        for nm, key in (("g_mix", "g_mix"), ("g_mlp", "g_mlp"), ("g_ple", "g_ple"),
                        ("b_ple", "b_ple_gate"), ("conv_b", "conv_b")):
            o = CL.off[(nm, l)]
            c[:, o:o + 8] = _fm_cols(inp[key][l])
        o = CL.off[("g_cq", l)]
        c[:, o:o + 2] = _fm_cols(inp["g_cq"][l])
        for nm in ("g_qn", "g_kn", "g_ik"):
            o = CL.off[(nm, l)]
            c[0:64, o] = inp[nm][l]
        for nm in ("i_bias", "f_bias"):
            o = CL.off[(nm, l)]
            c[0:4, o] = inp[nm][l]
        o = CL.off[("conv_w", l)]
        for j in range(4):
            c[:, o + j * 8:o + j * 8 + 8] = _fm_cols(inp["conv_w"][l, j])
        o = CL.off[("g_mh", l)]
        c[:, o:o + 4] = np.ascontiguousarray(inp["g_mh"][l].T)
        o = CL.off[("g_qn_row", l)]
        c[:, o:o + 64] = np.broadcast_to(inp["g_qn"][l][None, :], (128, 64))
        o = CL.off[("g_kn_row", l)]
        c[:, o:o + 64] = np.broadcast_to(inp["g_kn"][l][None, :], (128, 64))
    return c


CM = {}
_o = 0
for _nm, _w in (("MEAN1024", 128), ("MEAN256", 128), ("MEAN128", 128), ("MEAN64", 64), ("ONES", 128),
                ("IDENT", 128), ("RT", 64), ("TRIU", 64), ("I4", 4), ("SEL", 512)):
    CM[_nm] = _o
    _o += _w
CM_N = _o

CF = {}
_o = 0
for _nm, _w in (("I4", 4), ("SEL", 512), ("RESET", 512), ("INV", 1), ("CAUS", 128), ("POW2", N_BISECT), ("TRIU", 64)):
    CF[_nm] = _o
    _o += _w
CF_N = _o


def build_cm():
    import ml_dtypes
    m = np.zeros((128, CM_N), np.float32)
    m[:, CM["MEAN1024"]:CM["MEAN1024"] + 128] = 1.0 / 1024.0
    m[:, CM["MEAN256"]:CM["MEAN256"] + 128] = 1.0 / 256.0
    m[:, CM["MEAN128"]:CM["MEAN128"] + 128] = 1.0 / 128.0
    m[0:64, CM["MEAN64"]:CM["MEAN64"] + 64] = 1.0 / 64.0
    m[:, CM["ONES"]:CM["ONES"] + 128] = 1.0
    m[:, CM["IDENT"]:CM["IDENT"] + 128] = np.eye(128)
    rt = np.zeros((64, 64), np.float32)
    for i in range(8):
        rt[i + 8, i] = -1.0
        rt[i, i + 8] = 1.0
    m[0:64, CM["RT"]:CM["RT"] + 64] = rt
    m[0:64, CM["TRIU"]:CM["TRIU"] + 64] = np.triu(np.ones((64, 64)))
    m[0:4, CM["I4"]:CM["I4"] + 4] = np.eye(4)
    for hd in range(4):
        m[hd, CM["SEL"] + hd * 128:CM["SEL"] + (hd + 1) * 128] = 1.0
    return m.astype(ml_dtypes.bfloat16)


def build_cf():
    f = np.zeros((128, CF_N), np.float32)
    f[0:4, CF["I4"]:CF["I4"] + 4] = np.eye(4)
    for hd in range(4):
        f[hd, CF["SEL"] + hd * 128:CF["SEL"] + (hd + 1) * 128] = 1.0
    r = np.ones(512, np.float32)
    r[0::64] = 0.0
    f[0:4, CF["RESET"]:CF["RESET"] + 512] = r[None, :]
    inv = (500000.0 ** (-(np.arange(8, dtype=np.float32) * 2.0) / 16.0)).astype(np.float32)
    f[0:8, CF["INV"]] = inv
    f[8:16, CF["INV"]] = inv
    caus = np.where(np.arange(128)[None, :] <= np.arange(128)[:, None], 0.0, NEG_BIG)
    f[:, CF["CAUS"]:CF["CAUS"] + 128] = caus
    f[:, CF["POW2"]:CF["POW2"] + N_BISECT] = (0.5 ** np.arange(1, N_BISECT + 1))[None, :]
    f[0:64, CF["TRIU"]:CF["TRIU"] + 64] = np.triu(np.ones((64, 64)))
    return f


def build_nc(nb=2, nl=NL, dbg=(), stages=("mix", "attn", "mlstm", "ffn", "ple")):
    nc = bass.Bass("TRN2", target_bir_lowering=False)

    def din(name, shape, dt=F32):
        return nc.dram_tensor(name, shape, dt, kind="ExternalInput").ap()

    x_d = din("x", [nb, D, S])
    p_d = din("p", [NL, nb, 256, S])
    pos_d = din("pos", [nb, 64, S], I32)
    cst_d = din("cst", [128, CL.n])
    cm_d = din("cm", [128, CM_N], BF16)
    cf_d = din("cf", [128, CF_N])
    w_in_d = din("w_in", [NL, D, IN_W])
    w_qup_d = din("w_q_up", [NL, 256, 512])
    w_iqup_d = din("w_iq_up", [NL, 256, 512])
    w_out_d = din("w_out", [NL, D, D])
    w_ff1_d = din("w_ff1", [NL, D, DFF])
    w_ff2_d = din("w_ff2", [NL, DFF, D])
    w_pg_d = din("w_ple_gate", [NL, D, D])
    w_pl_d = din("w_ple", [NL, 256, D])
    out_d = nc.dram_tensor("out", [nb, D, S], F32, kind="ExternalOutput").ap()

    P = Prog(nc)
    es = contextlib.ExitStack()
    uid = [0]

    def sbt(stack, name, shape, dt):
        uid[0] += 1
        return stack.enter_context(nc.sbuf_tensor("%s_%d" % (name, uid[0]), shape, dt))

    def sb(name, shape, dt):
        return sbt(es, name, shape, dt)

    h = sb("h", [128, 8, S], F32)
    cst = sb("cst", [128, CL.n], F32)
    cm = sb("cm", [128, CM_N], BF16)
    cf = sb("cf", [128, CF_N], F32)
    wb = [sb("wb%d" % i, [128, 4096], BF16) for i in range(3)]
    wS = sb("wS", [128, 8, 8], BF16)
    sq = [sb("sq%d" % i, [128, TB], BF16) for i in range(2)]
    rstd = [sb("rstd%d" % i, [128, TB], F32) for i in range(2)]
    KA = sb("KA", [64, 2, S], BF16)
    IK = sb("IK", [64, S], BF16)
    V = sb("V", [128, 16, 128], BF16)
    cosF = sb("cosF", [64, S], BF16)
    sinF = sb("sinF", [64, S], BF16)
    Cst = sb("Cst", [128, 4, 256], F32)
    Cbf = sb("Cbf", [128, 4, 256], BF16)
    halo = sb("halo", [128, 8, 4], F32)
    mixA = sb("mixA", [64, 8, TB], BF16)
    mixM = sb("mixM", [128, 4, TB], BF16)
    small = sb("small", [128, 16], F32)
    psum = [es.enter_context(nc.psum_tensor("ps%d" % i, [128, 512], F32)) for i in range(8)]
    ps_ctr = [0]
    wb_ctr = [0]

    nbank = [8]

    def bank():
        b = psum[ps_ctr[0] % nbank[0]]
        ps_ctr[0] += 1
        return b

    def wbuf():
        b = wb[wb_ctr[0] % 3]
        wb_ctr[0] += 1
        return b

    def isap(v):
        return v is not None and not isinstance(v, (int, float))

    def mm(out, lhsT, rhs, start=True, stop=True):
        return P.add("pe", lambda e: e.matmul(out, lhsT, rhs, start=start, stop=stop),
                     reads=[lhsT, rhs], writes=[out])

    def dma(eng, out, in_, rd=None, wr=None):
        return P.add(eng, lambda e: e.dma_start(out=out, in_=in_),
                     reads=[rd] if rd is not None else [], writes=[wr] if wr is not None else [], dma=True)

    def wload(dst, src):
        return dma("pool", dst, src, wr=dst)

    def act(out, in_, func, bias=None, scale=None):
        kw = {}
        rds = [in_]
        if bias is not None:
            kw["bias"] = bias
            if isap(bias):
                rds.append(bias)
        if scale is not None:
            kw["scale"] = scale
            if isap(scale):
                rds.append(scale)
        return P.add("act", lambda e: e.activation(out, in_, func, **kw), reads=rds, writes=[out])

    def ts(eng, out, in0, s1, s2, op0, op1=None, accum=None):
        rds = [in0] + [s for s in (s1, s2) if isap(s)]
        wrs = [out] + ([accum] if accum is not None else [])
        if accum is not None:
            return P.add(eng, lambda e: e.tensor_scalar(out, in0, s1, s2, op0, op1, accum_out=accum),
                         reads=rds, writes=wrs)
        if op1 is None:
            return P.add(eng, lambda e: e.tensor_scalar(out, in0, s1, s2, op0), reads=rds, writes=wrs)
        return P.add(eng, lambda e: e.tensor_scalar(out, in0, s1, s2, op0, op1), reads=rds, writes=wrs)

    def stt(eng, out, in0, scalar, in1, op0, op1):
        rds = [in0, in1] + ([scalar] if isap(scalar) else [])
        return P.add(eng, lambda e: e.scalar_tensor_tensor(out, in0, scalar, in1, op0, op1),
                     reads=rds, writes=[out])

    def tt(eng, out, in0, in1, op):
        return P.add(eng, lambda e: e.tensor_tensor(out, in0, in1, op), reads=[in0, in1], writes=[out])

    def cp(eng, out, in_):
        if eng == "act":
            return act(out, in_, AF.Copy)
        return P.add(eng, lambda e: e.tensor_copy(out, in_), reads=[in_], writes=[out])

    def recip(out, in_):
        return P.add("dve", lambda e: e.reciprocal(out, in_), reads=[in_], writes=[out])

    def memset(eng, out, val):
        return P.add(eng, lambda e: e.memset(out, val), reads=[], writes=[out])

    def ccol(name, l, c=0, p=128):
        o = CL.off[(name, l)] + c
        return cst[0:p, o:o + 1]

    def cmat(name, p, w):
        return cm[0:p, CM[name]:CM[name] + w]

    dumps = {}

    def dump(name, ap):
        if name in dbg and name not in dumps:
            shp = list(ap.shape)
            d = nc.dram_tensor("dbg_" + name, shp, ap.dtype, kind="ExternalOutput").ap()
            dumps[name] = dma("sp", d, ap, rd=ap)

    dma("sp", cst[:], cst_d, wr=cst[:])
    dma("sp", cm[:], cm_d, wr=cm[:])
    dma("sp", cf[:], cf_d, wr=cf[:])

    def rstd_from_mean(r, ps_ap):
        act(r, ps_ap, AF.Ln, bias=EPS)
        act(r, r, AF.Exp, scale=-0.5)

    def recip_act(out, in_):
        act(out, in_, AF.Ln)
        act(out, out, AF.Exp, scale=-1.0)

    def rmsnorm_block(l, gname, src_tsl, dst, dst_tsl):
        ps = bank()
        for c in range(8):
            s = sq[c % 2]
            act(s[:], h[:, c, src_tsl], AF.Square)
            mm(ps[:], cmat("MEAN1024", 128, 128), s[:], c == 0, c == 7)
        r = rstd[ps_ctr[0] % 2]
        rstd_from_mean(r[:], ps[:])
        for c in range(8):
            stt("dve", dst[:, c, dst_tsl], h[:, c, src_tsl], ccol(gname, l, c), r[:], ALU.mult, ALU.mult)


    def red(eng, out, in_, op):
        return P.add(eng, lambda e: e.tensor_reduce(out, in_, AX.X, op), reads=[in_], writes=[out])

    def scan(out, d0, d1, init, op0, op1):
        return P.add("dve", lambda e: e.tensor_tensor_scan(out, d0, d1, init, op0, op1),
                     reads=[d0, d1], writes=[out])

    acc_banks = psum[4:8]

    def mixer_layer(b, l):
        nfb = small[0:4, 0:1]
        ts("dve", nfb, ccol("f_bias", l, 0, 4), -1.0, None, ALU.mult)
        negM = small[:, 1:2]
        gq = small[:, 2:3]
        gk = small[:, 3:4]
        lst = contextlib.ExitStack()
        tg = sbt(lst, "tg", [128, 64], F32)
        for nm, dstc in (("g_qn_row", gq), ("g_kn_row", gk)):
            o = CL.off[(nm, l)]
            row = cst[:, o:o + 64]
            ts("dve", tg[:], row, -1.0, None, ALU.mult)
            tt("dve", tg[:], tg[:], row, ALU.max)
            red("dve", dstc, tg[:], ALU.max)
        stt("dve", negM, gq, -8.0, gk, ALU.mult, ALU.mult)
        lst.close()
        memset("dve", Cst[:], 0.0)
        memset("pool", Cbf[:], 0.0)
        memset("pool", halo[:], 0.0)

        for j in range(NTB):
            tsl = slice(j * TB, (j + 1) * TB)
            blk = contextlib.ExitStack()
            xnb = sbt(blk, "xnb", [128, 8, TB], BF16)
            rmsnorm_block(l, "g_mix", tsl, xnb, slice(0, TB))
            if "attn" in stages:
                attn_block(b, l, j, tsl, xnb)
                P.barrier()
            if "mlstm" in stages:
                mlstm_block(b, l, j, tsl, xnb)
            blk.close()
            P.barrier()

    def head_fm(hx, ps_in, gcol, dst, cs32):
        xs, xb, t1, rs, sqh = hx
        cos32, sin32 = cs32
        cp("act", xs, ps_in)
        if gcol is not None:
            act(sqh, ps_in, AF.Square)
            psm = bank()
            mm(psm[0:64, :], cmat("MEAN64", 64, 64), sqh)
            rstd_from_mean(rs, psm[0:64, :])
            stt("dve", xs, xs, gcol, rs, ALU.mult, ALU.mult)
        cp("act", xb, xs)
        psr = bank()
        mm(psr[0:64, :], cmat("RT", 64, 64), xb)
        tt("dve", t1, xs, cos32, ALU.mult)
        tt("dve", rs, psr[0:64, :], sin32, ALU.mult)
        tt("dve", dst, t1, rs, ALU.add)

    def attn_block(b, l, j, tsl, xnb):
        ph = contextlib.ExitStack()
        IQb = sbt(ph, "QIb", [64, 8, TB], BF16)
        QAb = IQb
        acc = sbt(ph, "acc", [128, S], F32)
        tmp = [sbt(ph, "tmp%d" % i, [128, TB], F32) for i in range(2)]
        maskb = sbt(ph, "maskb", [128, S], BF16)
        maskT = sbt(ph, "maskT", [128, 16, TB], BF16)
        pT = [sbt(ph, "pT%d" % i, [128, TB], BF16) for i in range(2)]
        hx = (sbt(ph, "xs", [64, TB], F32), sbt(ph, "xb", [64, TB], BF16), sbt(ph, "t1", [64, TB], F32),
              sbt(ph, "rs", [64, TB], F32), sbt(ph, "sqh", [64, TB], BF16))
        cqf = acc[:, 0:2 * TB].rearrange("p (c n) -> p c n", c=2)
        hx2 = (acc[0:64, 0:TB], maskb[0:64, 0:TB], acc[0:64, TB:2 * TB], acc[0:64, 2 * TB:3 * TB],
               maskb[0:64, TB:2 * TB])
        hx1 = tuple(t_[:] for t_ in hx)
        hxs = [hx1, hx2]
        hx_ctr = [0]

        def next_hx():
            hx_ctr[0] += 1
            return hxs[hx_ctr[0] % 2]
        cqn = sbt(ph, "cqn", [128, 2, TB], BF16)
        wabs = sbt(ph, "wabs", [128, 4, 8], F32)
        wsgn = sbt(ph, "wsgn", [128, 4, 8], F32)
        bs = sbt(ph, "bs", [128, 8 + N_BISECT], F32)
        rc = tmp[0][0:64, :]
        cs32 = (tmp[0][0:64, :], tmp[1][0:64, :])

        def load_cs32():
            cp("act", cs32[0], cosF[:, tsl])
            cp("act", cs32[1], sinF[:, tsl])

        load_cs32()

        b1 = wbuf()
        wcq = b1[:, 0:2048].rearrange("p (c n) -> p c n", c=8)
        wqu = b1[:, 2048:3072].rearrange("p (c n) -> p c n", c=2)
        wiq = b1[:, 3072:4096].rearrange("p (c n) -> p c n", c=2)
        wload(wcq, w_in_d[l, :, 0:256].rearrange("(c p) n -> p c n", p=128))
        wload(wqu, w_qup_d[l].rearrange("(c p) n -> p c n", p=128))
        wload(wiq, w_iqup_d[l].rearrange("(c p) n -> p c n", p=128))
        b2 = wbuf()
        wgk = b2[:, 0:8 * 328].rearrange("p (c n) -> p c n", c=8)
        wload(wgk, w_in_d[l, :, 256:584].rearrange("(c p) n -> p c n", p=128))

        psm = bank()
        for cc in range(2):
            ps = bank()
            for kc in range(8):
                mm(ps[:], wcq[:, kc, cc * 128:(cc + 1) * 128], xnb[:, kc, :], kc == 0, kc == 7)
            cp("act", cqf[:, cc, :], ps[:])
            s = sq[cc % 2]
            act(s[:], ps[:], AF.Square)
            mm(psm[:], cmat("MEAN256", 128, 128), s[:], cc == 0, cc == 1)
        r = rstd[0]
        rstd_from_mean(r[:], psm[:])
        for cc in range(2):
            stt("dve", cqn[:, cc, :], cqf[:, cc, :], ccol("g_cq", l, cc), r[:], ALU.mult, ALU.mult)

        for g in range(2):
            ps = bank()
            for kc in range(8):
                mm(ps[0:64, :], wgk[:, kc, g * 64:(g + 1) * 64], xnb[:, kc, :], kc == 0, kc == 7)
            head_fm(next_hx(), ps[0:64, :], ccol("g_kn", l, 0, 64), KA[:, g, tsl], cs32)
        ps = bank()
        for kc in range(8):
            mm(ps[0:64, :], wgk[:, kc, 256:320], xnb[:, kc, :], kc == 0, kc == 7)
        head_fm(next_hx(), ps[0:64, :], ccol("g_ik", l, 0, 64), IK[:, tsl], cs32)
        for t in range(4):
            ps = bank()
            for kc in range(8):
                mm(ps[:, 0:128], xnb[:, kc, t * 128:(t + 1) * 128], wgk[:, kc, 128:256], kc == 0, kc == 7)
            cp("act", V[:, 4 * j + t, :], ps[:, 0:128])
            ps = bank()
            for kc in range(8):
                mm(ps[:, 0:8], xnb[:, kc, t * 128:(t + 1) * 128], wgk[:, kc, 320:328], kc == 0, kc == 7)
            act(wabs[:, t, :], ps[:, 0:8], AF.Abs, scale=IDX_SCALE)
            act(wsgn[:, t, :], ps[:, 0:8], AF.Sign)
        for hd in range(8):
            ps = bank()
            for kc in range(2):
                mm(ps[0:64, :], wiq[:, kc, hd * 64:(hd + 1) * 64], cqn[:, kc, :], kc == 0, kc == 1)
            head_fm(next_hx(), ps[0:64, :], None, IQb[:, hd, :], cs32)
        if b == 0 and l == 0 and j == 0:
            dump("KA", KA[:, :, 0:TB])
            dump("IK", IK[:, 0:TB])
            dump("IQb", IQb[:])
            dump("V", V[:, 0:4, :])
            dump("wabs", wabs[:])
            dump("wsgn", wsgn[:])

        lo, mid, cnt, tcol, w0, hi, thr = [bs[:, i:i + 1] for i in range(7)]
        s2col = bs[:, 7:8]
        acc_junk = maskb[:, 1024:2048]
        Wb = bs[:, 8:8 + N_BISECT]
        for t in range(4):
            qi = 4 * j + t
            L = 128 * (qi + 1)
            qsl = slice(t * 128, (t + 1) * 128)
            for c in range(j + 1):
                ncols = min(512, L - 512 * c)
                cs = slice(512 * c, 512 * c + ncols)
                for hd in range(8):
                    ps = bank()
                    mm(ps[:, 0:ncols], IQb[:, hd, qsl], IK[:, cs])
                    tm = tmp[hd % 2]
                    act(tm[:, 0:ncols], ps[:, 0:ncols], AF.Relu, scale=wabs[:, t, hd:hd + 1])
                    eng = "dve"
                    if hd == 0:
                        ts(eng, acc[:, cs], tm[:, 0:ncols], wsgn[:, t, hd:hd + 1], None, ALU.mult)
                    elif eng == "dve":
                        stt(eng, acc[:, cs], tm[:, 0:ncols], wsgn[:, t, hd:hd + 1], acc[:, cs], ALU.mult, ALU.add)
                    else:
                        ts("pool", tm[:, 0:ncols], tm[:, 0:ncols], wsgn[:, t, hd:hd + 1], None, ALU.mult)
                        tt("pool", acc[:, cs], acc[:, cs], tm[:, 0:ncols], ALU.add)
            if qi >= 2:
                red("dve", lo, acc[:, 0:L], ALU.min)
                red("dve", hi, acc[:, 0:L], ALU.max)
            tt("dve", acc[:, qi * 128:(qi + 1) * 128], acc[:, qi * 128:(qi + 1) * 128],
               cf[:, CF["CAUS"]:CF["CAUS"] + 128], ALU.add)
            if b == 0 and l == 0 and j == 0 and t == 3:
                dump("acc", acc[:, 0:512])
            if qi < 2:
                memset("dve", lo, -1.0e29)
            else:
                tt("dve", w0, hi, lo, ALU.subtract)
                ts("dve", w0, w0, 1.0001, 1e-12, ALU.mult, ALU.add)
                ts("dve", Wb, cf[:, CF["POW2"]:CF["POW2"] + N_BISECT], w0, None, ALU.mult)
                tt("dve", mid, lo, Wb[:, 0:1], ALU.add)
                split = L >= 768
                L1 = (L // 2) if split else L
                n2 = L - L1
                for k in range(N_BISECT):
                    ts("dve", maskb[:, 0:L1], acc[:, 0:L1], mid, None, ALU.is_ge, ALU.add, accum=cnt)
                    if split:
                        P.add("act", (lambda o_, i_, b_, a_: lambda e: e.activation(o_, i_, AF.Sign, bias=b_, scale=-1.0,
                                                                                      accum_out=a_))(
                            tmp[1][:, 0:n2] if n2 <= TB else acc_junk[:, 0:n2], acc[:, L1:L], mid, s2col),
                            reads=[acc[:, L1:L], mid], writes=[(tmp[1][:, 0:n2] if n2 <= TB else acc_junk[:, 0:n2]), s2col])
                        stt("dve", hi, cnt, 2.0, s2col, ALU.mult, ALU.subtract)
                        stt("dve", tcol, hi, float(511 - n2), Wb[:, k:k + 1], ALU.is_ge, ALU.mult)
                    else:
                        stt("dve", tcol, cnt, 255.5, Wb[:, k:k + 1], ALU.is_ge, ALU.mult)
                    kn = min(k + 1, N_BISECT - 1)
                    stt("dve", mid, tcol, Wb[:, kn:kn + 1], mid, ALU.subtract, ALU.add)
                cp("dve", lo, mid)
            ts("dve", maskb[:, 0:L], acc[:, 0:L], lo, None, ALU.is_ge)
            if b == 0 and l == 0 and j == 0 and t == 3:
                dump("maskb", maskb[:, 0:512])
                dump("bs", bs[:])
            for k4 in range(0, qi + 1, 4):
                n = min(4, qi + 1 - k4)
                ps = bank()
                for kk in range(n):
                    mm(ps[:, kk * 128:(kk + 1) * 128], maskb[:, (k4 + kk) * 128:(k4 + kk + 1) * 128],
                       cmat("IDENT", 128, 128))
                cp("act", maskT[:, k4:k4 + n, qsl], ps[:, 0:n * 128].rearrange("p (a q) -> p a q", a=n))
            for kt in range(qi + 1, 4 * (j + 1)):
                memset("pool", maskT[:, kt, qsl], 0.0)

        load_cs32()
        for hd in range(8):
            ps = bank()
            for kc in range(2):
                mm(ps[0:64, :], wqu[:, kc, hd * 64:(hd + 1) * 64], cqn[:, kc, :], kc == 0, kc == 1)
            head_fm(next_hx(), ps[0:64, :], ccol("g_qn", l, 0, 64), QAb[:, hd, :], cs32)
        if b == 0 and l == 0 and j == 0:
            dump("QAb", QAb[:])
        nbank[0] = 4
        nkt = 4 * (j + 1)
        for hd in range(8):
            g = hd // 4
            psn = acc_banks[(hd % 2) * 2]
            psd = acc_banks[(hd % 2) * 2 + 1]
            for kt in range(nkt):
                ps = bank()
                mm(ps[:], KA[:, g, kt * 128:(kt + 1) * 128], QAb[:, hd, :])
                pt = pT[kt % 2]
                act(pt[:], ps[:], AF.Exp, bias=small[:, 1:2], scale=0.125)
                tt("dve", pt[:], pt[:], maskT[:, kt, :], ALU.mult)
                mm(psn[0:64, :], V[:, kt, g * 64:(g + 1) * 64], pt[:], kt == 0, kt == nkt - 1)
                mm(psd[0:64, :], cmat("ONES", 128, 64), pt[:], kt == 0, kt == nkt - 1)
            recip_act(rc, psd[0:64, :])
            tt("dve", mixA[:, hd, :], psn[0:64, :], rc, ALU.mult)
        if b == 0 and l == 0 and j == 0:
            dump("mixA", mixA[:])
        nbank[0] = 8
        wo = []
        for half in range(2):
            bb = wbuf()
            v = bb[0:64, :].rearrange("p (c n) -> p c n", c=4)
            wload(v, w_out_d[l, half * 256:(half + 1) * 256, :].rearrange("(c p) n -> p c n", p=64))
            wo.append(v)
        for dc in range(8):
            ps = bank()
            for hd in range(8):
                mm(ps[:], wo[hd // 4][:, hd % 4, dc * 128:(dc + 1) * 128], mixA[:, hd, :], hd == 0, hd == 7)
            tt("dve", h[:, dc, tsl], h[:, dc, tsl], ps[:], ALU.add)
        ph.close()

    def mlstm_block(b, l, j, tsl, xnb):
        ph = contextlib.ExitStack()
        lic = sbt(ph, "lic", [4, TB], F32)
        l1 = sbt(ph, "l1", [4, TB], F32)
        nbt = sbt(ph, "nbt", [4, TB], F32)
        aa = lic
        ag = l1
        eaeg = sbt(ph, "eaeg", [64, 8, 8], F32)
        EB = [sbt(ph, "EB%d" % i, [128, TB], BF16) for i in range(4)]
        EBL = sbt(ph, "EBL", [128, 4, 8], F32)
        xc = [sbt(ph, "xc%d" % i, [128, 4 + TB], F32) for i in range(2)]
        cacc = [sbt(ph, "cacc%d" % i, [128, TB], F32) for i in range(2)]
        qf = sbt(ph, "qf", [128, 4, TB], BF16)
        kf = sbt(ph, "kf", [128, 4, TB], BF16)
        kg = sbt(ph, "kg", [64, 8, 4, 128], BF16)
        vt = sbt(ph, "vt", [64, 8, 512], BF16)
        og = sbt(ph, "og", [128, 4, TB], BF16)
        Ssb = [sbt(ph, "Ssb%d" % i, [64, 64], BF16) for i in range(4)]
        hm = cacc[1]
        dn = cacc[0]

        wload(wS[:], w_in_d[l, :, 2632:2640].rearrange("(c p) n -> p c n", p=128))
        ps_i = bank()
        for kc in range(8):
            mm(ps_i[0:4, :], wS[:, kc, 0:4], xnb[:, kc, :], kc == 0, kc == 7)
        ps_f = bank()
        for kc in range(8):
            mm(ps_f[0:4, :], wS[:, kc, 4:8], xnb[:, kc, :], kc == 0, kc == 7)
        ts("dve", lic[:], ps_i[0:4, :], ccol("i_bias", l, 0, 4), None, ALU.add)
        act(l1[:], ps_f[0:4, :], AF.Exp, bias=small[0:4, 0:1], scale=-1.0)
        act(l1[:], l1[:], AF.Ln, bias=1.0)
        scan(nbt[:], cf[0:4, CF["RESET"]:CF["RESET"] + TB], l1[:], 0.0, ALU.mult, ALU.add)
        tt("dve", aa[:], lic[:], nbt[:], ALU.add)
        nb3 = nbt[:, :].rearrange("p (c s) -> p c s", c=8)
        for c in range(8):
            ts("dve", ag[:, c * 64:(c + 1) * 64], aa[:, c * 64:(c + 1) * 64], nbt[:, c * 64 + 63:c * 64 + 64], None,
               ALU.subtract)
        if KSTOP <= 1:
            ph.close()
            return
        hl = {}
        for nm, src in (("aa", aa), ("ag", ag), ("nb", nbt)):
            hi = sbt(ph, nm + "hi", [4, TB], BF16)
            lo_ = sbt(ph, nm + "lo", [4, TB], BF16)
            hf = xc[0][0:4, 0:TB]
            cp("dve", hi[:], src[:])
            cp("dve", hf, hi[:])
            tt("dve", hf, src[:], hf, ALU.subtract)
            cp("dve", lo_[:], hf)
            hl[nm] = (hi, lo_)
        i4 = cm[0:4, CM["I4"]:CM["I4"] + 4]
        psg = bank()
        for c in range(8):
            for q_, nm in ((0, "aa"), (4, "ag")):
                hi, lo_ = hl[nm]
                mm(psg[0:64, c * 8 + q_:c * 8 + q_ + 4], hi[:, c * 64:(c + 1) * 64], i4, True, False)
                mm(psg[0:64, c * 8 + q_:c * 8 + q_ + 4], lo_[:, c * 64:(c + 1) * 64], i4, False, True)
        act(eaeg[:, :, :].rearrange("p c n -> p (c n)"), psg[0:64, 0:64], AF.Exp)
        for hd in range(4):
            ps = bank()
            sel = cm[0:4, CM["SEL"] + hd * 128:CM["SEL"] + (hd + 1) * 128]
            mm(ps[:], sel, hl["nb"][0][:], True, False)
            mm(ps[:], sel, hl["nb"][1][:], False, True)
            act(EB[hd][:], ps[:], AF.Exp, scale=-1.0)
            act(EBL[:, hd, :], ps[:, 63:512:64], AF.Exp, scale=-1.0)
        if b == 0 and l == 0 and j == 0:
            dump("lic", lic[:])
            dump("nbt", nbt[:])
            dump("eaeg", eaeg[:])
            dump("EB0", EB[0][:])

        if KSTOP <= 2:
            ph.close()
            return
        for which in range(2):
            wbb = wbuf()
            w = wbb[:, :].rearrange("p (c n) -> p c n", c=8)
            c0 = 584 + which * 512
            wload(w, w_in_d[l, :, c0:c0 + 512].rearrange("(c p) n -> p c n", p=128))
            for cc in range(4):
                ch = which * 4 + cc
                ps = bank()
                for kc in range(8):
                    mm(ps[:], w[:, kc, cc * 128:(cc + 1) * 128], xnb[:, kc, :], kc == 0, kc == 7)
                x_ = xc[cc % 2]
                ca = cacc[cc % 2]
                cp("pool", x_[:, 0:4], halo[:, ch, :])
                cp("act", x_[:, 4:4 + TB], ps[:])
                if KSUB <= 1:
                    continue
                cw = CL.off[("conv_w", l)]
                act(ca[:], x_[:, 1:1 + TB], AF.Identity, bias=ccol("conv_b", l, ch), scale=cst[:, cw + ch:cw + ch + 1])
                for jt in range(1, 4):
                    stt("dve", ca[:], x_[:, 1 + jt:1 + jt + TB],
                        cst[:, cw + jt * 8 + ch:cw + jt * 8 + ch + 1], ca[:], ALU.mult, ALU.add)
                if KSUB <= 2:
                    continue
                cp("pool", halo[:, ch, :], x_[:, TB:TB + 4])
                if KSUB <= 3:
                    continue
                sg = x_[:, 4:4 + TB]
                act(sg, ca[:], AF.Sigmoid)
                if which == 0:
                    tt("dve", ca[:], ca[:], sg, ALU.mult)
                    ts("dve", qf[:, cc, :], ca[:], float(128 ** -0.5), None, ALU.mult)
                    tt("dve", qf[:, cc, :], qf[:, cc, :], EB[cc][:], ALU.mult)
                else:
                    tt("dve", kf[:, cc, :], ca[:], sg, ALU.mult)
        if KSTOP <= 3:
            ph.close()
            return
        wbb = wbuf()
        wv = wbb[:, :].rearrange("p (c n) -> p c n", c=8)
        wload(wv, w_in_d[l, :, 1608:2120].rearrange("(c p) n -> p c n", p=128))
        for c in range(8):
            ps = bank()
            for kc in range(8):
                mm(ps[0:64, :], xnb[:, kc, c * 64:(c + 1) * 64], wv[:, kc, :], kc == 0, kc == 7)
            cp("act" if c % 2 == 0 else "dve", vt[:, c, :], ps[0:64, :])
        if KSUB2 <= 1:
            ph.close()
            return
        for c in range(8):
            ps = bank()
            for hd in range(4):
                mm(ps[0:64, hd * 128:(hd + 1) * 128], kf[:, hd, c * 64:(c + 1) * 64], cmat("IDENT", 128, 128))
            for hd in range(4):
                ts("dve", kg[:, c, hd, :], ps[0:64, hd * 128:(hd + 1) * 128], eaeg[:, c, 4 + hd:5 + hd], None, ALU.mult)
        if KSUB2 <= 2:
            ph.close()
            return
        wbb = wbuf()
        w = wbb[:, :].rearrange("p (c n) -> p c n", c=8)
        wload(w, w_in_d[l, :, 2120:2632].rearrange("(c p) n -> p c n", p=128))
        for cc in range(4):
            ps = bank()
            for kc in range(8):
                mm(ps[:], w[:, kc, cc * 128:(cc + 1) * 128], xnb[:, kc, :], kc == 0, kc == 7)
            act(og[:, cc, :], ps[:], AF.Sigmoid)
        if b == 0 and l == 0 and j == 0:
            dump("qf", qf[:])
            dump("kf", kf[:])
            dump("vt", vt[:])
            dump("kg", kg[:])
            dump("og", og[:])

        if KSTOP <= 4:
            ph.close()
            return
        nbank[0] = 4
        for pair in range(2):
            for c in range(8):
                csl = slice(c * 64, (c + 1) * 64)
                for hh in range(2):
                    hd = pair * 2 + hh
                    psN = acc_banks[hh * 2]
                    psDn = acc_banks[hh * 2 + 1]
                    psS = bank()
                    mm(psS[0:64, 0:64], kf[:, hd, csl], qf[:, hd, csl])
                    Sb = Ssb[(c * 2 + hh) % 4]
                    stt("dve", Sb[:], psS[0:64, 0:64], eaeg[:, c, hd:hd + 1], cf[0:64, CF["TRIU"]:CF["TRIU"] + 64], ALU.mult, ALU.mult)
                    mm(psN[:, csl], Cbf[:, hd, 0:128], qf[:, hd, csl], True, False)
                    mm(psN[:, csl], vt[:, c, hd * 128:(hd + 1) * 128], Sb[:], False, True)
                    mm(psDn[:, csl], Cbf[:, hd, 128:256], qf[:, hd, csl], True, False)
                    mm(psDn[:, csl], cmat("ONES", 64, 128), Sb[:], False, True)
                    psD = bank()
                    mm(psD[:, 0:128], kg[:, c, hd, :], vt[:, c, hd * 128:(hd + 1) * 128])
                    mm(psD[:, 128:256], kg[:, c, hd, :], cmat("ONES", 64, 128))
                    stt("dve", Cst[:, hd, :], Cst[:, hd, :], EBL[:, hd, c:c + 1], psD[:, 0:256],
                        ALU.mult, ALU.add)
                    cp("act", Cbf[:, hd, :], Cst[:, hd, :])
            for hh in range(2):
                hd = pair * 2 + hh
                psN = acc_banks[hh * 2]
                psDn = acc_banks[hh * 2 + 1]
                act(dn[:], psDn[:], AF.Abs)
                ts("dve", dn[:], dn[:], 1.0, None, ALU.max)
                recip_act(dn[:], dn[:])
                tt("dve", hm[:], psN[:], dn[:], ALU.mult)
                if b == 0 and l == 0 and j == 0 and hd == 0:
                    dump("hm0", hm[:])
                s = sq[hh]
                act(s[:], hm[:], AF.Square)
                psm = bank()
                mm(psm[:], cmat("MEAN128", 128, 128), s[:])
                r = rstd[hh]
                rstd_from_mean(r[:], psm[:])
                g_o = CL.off[("g_mh", l)] + hd
                hmb = sq[hh]
                stt("dve", hmb[:], hm[:], cst[:, g_o:g_o + 1], r[:], ALU.mult, ALU.mult)
                tt("dve", mixM[:, hd, :], hmb[:], og[:, hd, :], ALU.mult)
        if b == 0 and l == 0 and j == 0:
            dump("mixM", mixM[:])
        nbank[0] = 8
        wbb = wbuf()
        wo = wbb[:, :].rearrange("p (c n) -> p c n", c=4)
        wload(wo, w_out_d[l, 512:1024, :].rearrange("(c p) n -> p c n", p=128))
        for dc in range(8):
            ps = bank()
            for kc in range(4):
                mm(ps[:], wo[:, kc, dc * 128:(dc + 1) * 128], mixM[:, kc, :], kc == 0, kc == 3)
            tt("dve", h[:, dc, tsl], h[:, dc, tsl], ps[:], ALU.add)
        ph.close()

    final_ops = []

    for b in range(nb):
        for c in range(8):
            dma("sp", h[:, c, :], x_d[b, c * 128:(c + 1) * 128, :], wr=h[:, c, :])
        if "mix" in stages:
            ph = contextlib.ExitStack()
            posi = sbt(ph, "posi", [64, S], I32)
            ang = sbt(ph, "ang", [64, S], F32)
            kf = sbt(ph, "kf", [64, S], F32)
            ki = sbt(ph, "ki", [64, S], I32)
            msk = sbt(ph, "msk", [64, S], F32)
            dma("sp", posi[:], pos_d[b], wr=posi[:])
            cp("dve", ang[:], posi[:])
            ts("dve", ang[:], ang[:], cf[0:64, CF["INV"]:CF["INV"] + 1], None, ALU.mult)
            for (dstT, shift) in ((sinF, 0.0), (cosF, np.pi / 2)):
                ts("dve", kf[:], ang[:], shift, 1.0 / TWO_PI, ALU.add, ALU.mult)
                cp("dve", ki[:], kf[:])
                cp("dve", kf[:], ki[:])
                ts("dve", msk[:], ang[:], shift, None, ALU.add)
                stt("dve", kf[:], kf[:], -TWO_PI, msk[:], ALU.mult, ALU.add)
                ts("dve", msk[:], kf[:], float(np.pi), None, ALU.is_gt)
                stt("dve", kf[:], msk[:], -TWO_PI, kf[:], ALU.mult, ALU.add)
                ts("dve", msk[:], kf[:], float(-np.pi), None, ALU.is_lt)
                stt("dve", kf[:], msk[:], TWO_PI, kf[:], ALU.mult, ALU.add)
                ts("dve", kf[:], kf[:], float(np.pi), float(-np.pi), ALU.min, ALU.max)
                act(dstT[:], kf[:], AF.Sin)
            dump("cosF", cosF[:])
            dump("sinF", sinF[:])
            ph.close()
            P.barrier()

        for l in range(nl):
            if "mix" in stages:
                mixer_layer(b, l)
            ph = contextlib.ExitStack()
            xn = sbt(ph, "xn", [128, 8, S], BF16)
            ubuf = sbt(ph, "ubuf", [128, 4, S], BF16)
            relu_t = [sbt(ph, "relu%d" % i, [128, TB], F32) for i in range(2)]
            if "ffn" in stages:
                for tb in range(NTB):
                    tsl = slice(tb * TB, (tb + 1) * TB)
                    rmsnorm_block(l, "g_mlp", tsl, xn, tsl)
                for j in range(8):
                    w1b = wbuf()
                    w2b = wbuf()
                    w1 = w1b[:, :].rearrange("p (c n) -> p c n", c=8)
                    w2 = w2b[:, :].rearrange("p (c n) -> p c n", c=4)
                    wload(w1, w_ff1_d[l, :, j * 512:(j + 1) * 512].rearrange("(c p) n -> p c n", p=128))
                    wload(w2, w_ff2_d[l, j * 512:(j + 1) * 512, :].rearrange("(c p) n -> p c n", p=128))
                    for tb in range(NTB):
                        tsl = slice(tb * TB, (tb + 1) * TB)
                        for fc in range(4):
                            ps = bank()
                            for kc in range(8):
                                mm(ps[:], w1[:, kc, fc * 128:(fc + 1) * 128], xn[:, kc, tsl], kc == 0, kc == 7)
                            rl = relu_t[fc % 2]
                            act(rl[:], ps[:], AF.Relu)
                            tt("dve", ubuf[:, fc, tsl], rl[:], rl[:], ALU.mult)
                    for tb in range(NTB):
                        tsl = slice(tb * TB, (tb + 1) * TB)
                        for dc in range(8):
                            ps = bank()
                            for fc in range(4):
                                mm(ps[:], w2[:, fc, dc * 128:(dc + 1) * 128], ubuf[:, fc, tsl], fc == 0, fc == 3)
                            tt("dve", h[:, dc, tsl], h[:, dc, tsl], ps[:], ALU.add)
            if "ple" in stages:
                for tb in range(NTB):
                    tsl = slice(tb * TB, (tb + 1) * TB)
                    rmsnorm_block(l, "g_ple", tsl, xn, tsl)
                pb = ubuf
                for kc in range(2):
                    wload(pb[:, kc, :], p_d[l, b, kc * 128:(kc + 1) * 128, :])
                for half in range(2):
                    wgb = wbuf()
                    wpb = wbuf()
                    wg = wgb[:, :].rearrange("p (c n) -> p c n", c=8)
                    wp = wpb[:, 0:1024].rearrange("p (c n) -> p c n", c=2)
                    wload(wg, w_pg_d[l, :, half * 512:(half + 1) * 512].rearrange("(c p) n -> p c n", p=128))
                    wload(wp, w_pl_d[l, :, half * 512:(half + 1) * 512].rearrange("(c p) n -> p c n", p=128))
                    for tb in range(NTB):
                        tsl = slice(tb * TB, (tb + 1) * TB)
                        for dcl in range(4):
                            dc = half * 4 + dcl
                            ps = bank()
                            for kc in range(8):
                                mm(ps[:], wg[:, kc, dcl * 128:(dcl + 1) * 128], xn[:, kc, tsl], kc == 0, kc == 7)
                            g = relu_t[dcl % 2]
                            act(g[:], ps[:], AF.Sigmoid, bias=ccol("b_ple", l, dc))
                            ps2 = bank()
                            for kc in range(2):
                                mm(ps2[:], wp[:, kc, dcl * 128:(dcl + 1) * 128], pb[:, kc, tsl], kc == 0, kc == 1)
                            tt("dve", g[:], g[:], ps2[:], ALU.mult)
                            tt("dve", h[:, dc, tsl], h[:, dc, tsl], g[:], ALU.add)
            ph.close()
            P.barrier()
        for c in range(8):
            final_ops.append(dma("sp", out_d[b, c * 128:(c + 1) * 128, :], h[:, c, :], rd=h[:, c, :]))

    final_ops += list(dumps.values())
    P.emit(final_wait_ops=final_ops)
    es.close()
    return nc


def make_in_maps(inp, nb, ncores):
    cst = build_cst(inp)
    cm = build_cm()
    cf = build_cf()
    x = inp["x"]
    p = inp["p"]
    pos = inp["positions"].astype(np.int32)
    in_maps = []
    for c in range(ncores):
        bs = slice(c * nb, (c + 1) * nb)
        m = {
            "x": np.ascontiguousarray(x[bs].transpose(0, 2, 1)),
            "p": np.ascontiguousarray(p[:, bs].transpose(0, 1, 3, 2)),
            "pos": np.ascontiguousarray(np.broadcast_to(pos[bs][:, None, :], (nb, 64, S))),
            "cst": cst, "cm": cm, "cf": cf,
        }
        for k in ("w_in", "w_q_up", "w_iq_up", "w_out", "w_ff1", "w_ff2", "w_ple_gate", "w_ple"):
            m[k] = np.ascontiguousarray(inp[k], dtype=np.float32)
        in_maps.append(m)
    return in_maps


def kernel(**inputs):
    inp = {k: np.asarray(v) for k, v in inputs.items()}
    nb = 2
    nc = build_nc(nb=nb, stages=RUN_STAGES)
    in_maps = make_in_maps(inp, nb, NCORES)
    res = run_bass_kernel_spmd(nc, in_maps, core_ids=list(range(NCORES)))
    out = np.concatenate([r["out"] for r in res.results], axis=0)
    return np.ascontiguousarray(out.transpose(0, 2, 1)).astype(np.float32)
```
